# Optimizing a Trainium2 kernel written in Bass

```python
import math
import jax, jax.numpy as jnp
from jax import lax
import numpy as np

D_MODEL = 1024
BATCH = 8
SEQ = 4096
DEPTH = 2

N_META = 16
GRID_W = 64
N_MIXERS = 2
N_LRU_LAYERS = (DEPTH + 1) // 2
N_NA_LAYERS = DEPTH // 2

D_RNN = D_MODEL
LRU_BLOCKS = 4
LRU_BLOCK = D_RNN // LRU_BLOCKS
CONV_W = 4
LRU_C = 8.0

NA_HEADS = 16
NA_HEAD_DIM = D_MODEL // NA_HEADS
NA_MAX_KH = 8
NA_KW = 16

N_EXPERTS = 32
TOP_K = 4
D_EXPERT = D_MODEL
SWIGLU_LIMIT = 7.0
SWIGLU_ALPHA = 1.702
MOE_BLOCK = 512

DN_ALPHA = (2.0 * DEPTH) ** 0.25
DN_BETA = (8.0 * DEPTH) ** -0.25
LN_EPS = 1e-5

kernel_name = "hybrid_rglru_natten_moe_encoder"


def layer_norm(x, g, b):
    xf = x.astype(jnp.float32)
    mu = jnp.mean(xf, axis=-1, keepdims=True)
    var = jnp.mean(jnp.square(xf - mu), axis=-1, keepdims=True)
    y = (xf - mu) * lax.rsqrt(var + LN_EPS)
    return (y * g.astype(jnp.float32) + b.astype(jnp.float32)).astype(x.dtype)


def _lin_combine(c1, c2):
    a1, b1 = c1
    a2, b2 = c2
    return a1 * a2, a2 * b1 + b2


def linear_scan(a, b, reverse):
    _, h = lax.associative_scan(_lin_combine, (a, b), axis=1, reverse=reverse)
    return h


def rg_lru_direction(xc, wa, ba, wx, bx, lam, reverse):
    B_, L, _ = xc.shape
    xb = xc.reshape(B_, L, LRU_BLOCKS, LRU_BLOCK)
    gate_a = jax.nn.sigmoid(jnp.einsum('blni,nij->blnj', xb, wa).reshape(B_, L, D_RNN) + ba)
    gate_x = jax.nn.sigmoid(jnp.einsum('blni,nij->blnj', xb, wx).reshape(B_, L, D_RNN) + bx)
    log_a = (-LRU_C * gate_a.astype(jnp.float32)) * jax.nn.softplus(-lam.astype(jnp.float32))
    a = jnp.exp(log_a)
    mult = jnp.sqrt(-jnp.expm1(2.0 * log_a))
    start = L - 1 if reverse else 0
    is_start = (jnp.arange(L) == start)[None, :, None]
    mult = jnp.where(is_start, 1.0, mult)
    b = mult * (gate_x * xc).astype(jnp.float32)
    return linear_scan(a, b, reverse)


def rglru_mixer(x, w_in, conv_w, conv_b, wa, ba, wx, bx, lam, w_out):
    u = x @ w_in
    xr, y = u[..., :D_RNN], u[..., D_RNN:]
    xc = lax.conv_general_dilated(
        xr, conv_w[:, None, :], window_strides=(1,),
        padding=[(CONV_W // 2, CONV_W - 1 - CONV_W // 2)],
        dimension_numbers=('NWC', 'WIO', 'NWC'),
        feature_group_count=D_RNN) + conv_b
    h = (rg_lru_direction(xc, wa[0], ba[0], wx[0], bx[0], lam[0], False)
         + rg_lru_direction(xc, wa[1], ba[1], wx[1], bx[1], lam[1], True))
    return (h.astype(x.dtype) * jax.nn.gelu(y)) @ w_out


def na_mixer(x, w_qkv, rpb, meta_bias, w_out):
    B_, L, D = x.shape
    n_tok = L - N_META
    rows = n_tok // GRID_W
    kh = min(NA_MAX_KH, rows)
    kw = min(NA_KW, GRID_W)
    qkv = (x @ w_qkv).reshape(B_, L, 3, NA_HEADS, NA_HEAD_DIM)
    q = qkv[:, :, 0] * (NA_HEAD_DIM ** -0.5)
    k = qkv[:, :, 1]
    v = qkv[:, :, 2]
    qm, km, vm = q[:, :N_META], k[:, :N_META], v[:, :N_META]
    grid_shape = (B_, rows, GRID_W, NA_HEADS, NA_HEAD_DIM)
    qg = q[:, N_META:].reshape(grid_shape)
    kg = k[:, N_META:].reshape(grid_shape)
    vg = v[:, N_META:].reshape(grid_shape)

    row_start = jnp.clip(jnp.arange(rows) - kh // 2, 0, rows - kh)
    col_start = jnp.clip(jnp.arange(GRID_W) - kw // 2, 0, GRID_W - kw)
    col_idx = col_start[:, None] + jnp.arange(kw)[None, :]
    col_off = col_idx - jnp.arange(GRID_W)[:, None] + (NA_KW - 1)
    rpb_f = rpb.astype(jnp.float32)
    meta_b = meta_bias.astype(jnp.float32)

    def row_block(r):
        rs = row_start[r]
        q_r = lax.dynamic_index_in_dim(qg, r, axis=1, keepdims=False)
        k_rows = lax.dynamic_slice_in_dim(kg, rs, kh, axis=1)
        v_rows = lax.dynamic_slice_in_dim(vg, rs, kh, axis=1)
        k_win = k_rows[:, :, col_idx]
        v_win = v_rows[:, :, col_idx]
        row_off = rs + jnp.arange(kh) - r + (NA_MAX_KH - 1)
        bias = rpb_f[:, row_off[None, :, None], col_off[:, None, :]]
        s_win = jnp.einsum('bqhd,bjqkhd->bhqjk', q_r, k_win).astype(jnp.float32) + bias
        s_win = s_win.reshape(B_, NA_HEADS, GRID_W, kh * kw)
        s_meta = jnp.einsum('bqhd,bmhd->bhqm', q_r, km).astype(jnp.float32) + meta_b[:, None, :]
        p = jax.nn.softmax(jnp.concatenate([s_win, s_meta], axis=-1), axis=-1).astype(x.dtype)
        p_win = p[..., :kh * kw].reshape(B_, NA_HEADS, GRID_W, kh, kw)
        p_meta = p[..., kh * kw:]
        return (jnp.einsum('bhqjk,bjqkhd->bqhd', p_win, v_win)
                + jnp.einsum('bhqm,bmhd->bqhd', p_meta, vm))

    og = lax.map(row_block, jnp.arange(rows))
    og = jnp.moveaxis(og, 0, 1).reshape(B_, n_tok, D)
    s_mm = jnp.einsum('bqhd,bmhd->bhqm', qm, km).astype(jnp.float32) + meta_b[:, None, :]
    p_mm = jax.nn.softmax(s_mm, axis=-1).astype(x.dtype)
    om = jnp.einsum('bhqm,bmhd->bqhd', p_mm, vm).reshape(B_, N_META, D)
    return jnp.concatenate([om, og], axis=1) @ w_out


def moe_ffn(x, router_w, router_b, w_gu, b_gu, w_down, b_down):
    B_, L, D = x.shape
    xt = x.reshape(-1, D)
    n = xt.shape[0]
    logits = (xt @ router_w).astype(jnp.float32) + router_b.astype(jnp.float32)
    top_val, top_idx = lax.top_k(logits, TOP_K)
    gates = jax.nn.softmax(top_val, axis=-1).astype(x.dtype)
    n_slots = n * TOP_K
    flat_e = top_idx.reshape(-1).astype(jnp.int32)
    flat_tok = (jnp.arange(n_slots) // TOP_K).astype(jnp.int32)
    flat_gate = gates.reshape(-1)
    order = jnp.argsort(flat_e)
    sorted_e = flat_e[order]
    counts = jax.ops.segment_sum(jnp.ones((n_slots,), jnp.int32), flat_e, num_segments=N_EXPERTS)
    padded = (counts + MOE_BLOCK - 1) // MOE_BLOCK * MOE_BLOCK
    pad_end = jnp.cumsum(padded)
    pad_start = pad_end - padded
    start = jnp.cumsum(counts) - counts
    dest = pad_start[sorted_e] + (jnp.arange(n_slots, dtype=jnp.int32) - start[sorted_e])
    n_blocks = -(-(n_slots + N_EXPERTS * (MOE_BLOCK - 1)) // MOE_BLOCK)
    cap = n_blocks * MOE_BLOCK
    buf_tok = jnp.full((cap,), n, jnp.int32).at[dest].set(flat_tok[order])
    buf_gate = jnp.zeros((cap,), x.dtype).at[dest].set(flat_gate[order])
    block_e = jnp.minimum(
        jnp.searchsorted(pad_end, jnp.arange(n_blocks, dtype=jnp.int32) * MOE_BLOCK, side='right'),
        N_EXPERTS - 1)
    x_pad = jnp.concatenate([xt, jnp.zeros((1, D), x.dtype)], axis=0)

    def expert_block(args):
        tok, g, e = args
        xb = x_pad[tok]
        h = xb @ w_gu[e] + b_gu[e]
        glu = jnp.minimum(h[:, :D_EXPERT], SWIGLU_LIMIT)
        lin = jnp.clip(h[:, D_EXPERT:], -SWIGLU_LIMIT, SWIGLU_LIMIT)
        act = glu * jax.nn.sigmoid(SWIGLU_ALPHA * glu) * (lin + 1.0)
        return (act @ w_down[e] + b_down[e]) * g[:, None]

    y_blocks = lax.map(expert_block, (buf_tok.reshape(n_blocks, MOE_BLOCK),
                                      buf_gate.reshape(n_blocks, MOE_BLOCK), block_e))
    y = jnp.zeros((n + 1, D), x.dtype).at[buf_tok].add(y_blocks.reshape(cap, D))
    return y[:n].reshape(B_, L, D)


def setup_inputs(seed: int = 0) -> dict:
    key = jax.random.key(seed)
    ks = jax.random.split(key, 32)
    f32 = jnp.float32
    nrm = lambda k, shape, s: (jax.random.normal(k, shape, f32) * s).astype(f32)
    D = D_MODEL
    u = jax.random.uniform(ks[9], (N_LRU_LAYERS, 2, D_RNN), f32, 0.9, 0.999)
    sig = u ** (1.0 / LRU_C)
    lam = jnp.log(sig) - jnp.log1p(-sig)
    return {
        "x": nrm(ks[0], (BATCH, SEQ, D), 1.0),
        "meta_tokens": nrm(ks[1], (N_META, D), 1.0),
        "lru_w_in": nrm(ks[2], (N_LRU_LAYERS, D, 2 * D_RNN), D ** -0.5),
        "lru_conv_w": nrm(ks[3], (N_LRU_LAYERS, CONV_W, D_RNN), CONV_W ** -0.5),
        "lru_conv_b": nrm(ks[4], (N_LRU_LAYERS, D_RNN), 0.01),
        "lru_wa": nrm(ks[5], (N_LRU_LAYERS, 2, LRU_BLOCKS, LRU_BLOCK, LRU_BLOCK), LRU_BLOCK ** -0.5),
        "lru_ba": nrm(ks[6], (N_LRU_LAYERS, 2, D_RNN), 0.01),
        "lru_wx": nrm(ks[7], (N_LRU_LAYERS, 2, LRU_BLOCKS, LRU_BLOCK, LRU_BLOCK), LRU_BLOCK ** -0.5),
        "lru_bx": nrm(ks[8], (N_LRU_LAYERS, 2, D_RNN), 0.01),
        "lru_lambda": lam.astype(f32),
        "lru_w_out": nrm(ks[10], (N_LRU_LAYERS, D_RNN, D), DN_BETA * D_RNN ** -0.5),
        "na_w_qkv": nrm(ks[11], (N_NA_LAYERS, D, 3 * D), D ** -0.5),
        "na_rpb": nrm(ks[12], (N_NA_LAYERS, NA_HEADS, 2 * NA_MAX_KH - 1, 2 * NA_KW - 1), 0.02),
        "na_meta_bias": nrm(ks[13], (N_NA_LAYERS, NA_HEADS, N_META), 0.02),
        "na_w_out": nrm(ks[14], (N_NA_LAYERS, D, D), DN_BETA * D ** -0.5),
        "ln_mix_g": 1.0 + nrm(ks[15], (DEPTH, D), 0.01),
        "ln_mix_b": nrm(ks[16], (DEPTH, D), 0.01),
        "router_w": nrm(ks[17], (DEPTH, D, N_EXPERTS), D ** -0.5),
        "router_b": nrm(ks[18], (DEPTH, N_EXPERTS), 0.01),
        "moe_w_gu": nrm(ks[19], (DEPTH, N_EXPERTS, D, 2 * D_EXPERT), D ** -0.5),
        "moe_b_gu": nrm(ks[20], (DEPTH, N_EXPERTS, 2 * D_EXPERT), 0.01),
        "moe_w_down": nrm(ks[21], (DEPTH, N_EXPERTS, D_EXPERT, D), DN_BETA * D_EXPERT ** -0.5),
        "moe_b_down": nrm(ks[22], (DEPTH, N_EXPERTS, D), 0.01),
        "ln_ffn_g": 1.0 + nrm(ks[23], (DEPTH, D), 0.01),
        "ln_ffn_b": nrm(ks[24], (DEPTH, D), 0.01),
    }


def reference(x, meta_tokens, lru_w_in, lru_conv_w, lru_conv_b, lru_wa, lru_ba, lru_wx, lru_bx,
              lru_lambda, lru_w_out, na_w_qkv, na_rpb, na_meta_bias, na_w_out, ln_mix_g, ln_mix_b,
              router_w, router_b, moe_w_gu, moe_b_gu, moe_w_down, moe_b_down, ln_ffn_g, ln_ffn_b):
    B_ = x.shape[0]
    meta = jnp.broadcast_to(meta_tokens[None].astype(x.dtype), (B_, N_META, D_MODEL))
    h = jnp.concatenate([meta, x], axis=1)
    for i in range(DEPTH):
        j = i // N_MIXERS
        if i % N_MIXERS == 0:
            mix = rglru_mixer(h, lru_w_in[j], lru_conv_w[j], lru_conv_b[j], lru_wa[j], lru_ba[j],
                              lru_wx[j], lru_bx[j], lru_lambda[j], lru_w_out[j])
        else:
            mix = na_mixer(h, na_w_qkv[j], na_rpb[j], na_meta_bias[j], na_w_out[j])
        h = layer_norm(DN_ALPHA * h + mix, ln_mix_g[i], ln_mix_b[i])
        ffn = moe_ffn(h, router_w[i], router_b[i], moe_w_gu[i], moe_b_gu[i], moe_w_down[i], moe_b_down[i])
        h = layer_norm(DN_ALPHA * h + ffn, ln_ffn_g[i], ln_ffn_b[i])
    return h[:, N_META:]
```

```python
import contextlib
import numpy as np
import ml_dtypes
import concourse.bass as bass
import concourse.mybir as mybir
from concourse.bass_utils import run_bass_kernel_spmd

F32 = mybir.dt.float32
BF16 = mybir.dt.bfloat16
I32 = mybir.dt.int32
ALU = mybir.AluOpType
AF = mybir.ActivationFunctionType
AX = mybir.AxisListType

D = 1024
KC = 8
NMETA = 16
SEQ = 4096
L = NMETA + SEQ
NE = 32
CAP = 704
SLOTCH = [(i * 128, min(128, CAP - i * 128)) for i in range((CAP + 127) // 128)]
NSC = len(SLOTCH)
NB = 3
DUMP = NE * CAP
ALPHA = 4.0 ** 0.25
EPS = 1e-5
GROUPS = [(0, 16)] + [(16 + 512 * g, 512) for g in range(8)]
CHUNKS = [(0, 16)] + [(16 + 128 * c, 128) for c in range(32)]

P_CW, P_CB, P_BA, P_BX, P_LAM, P_BGU0, P_BGU1, P_MB, NPAR = 0, 32, 40, 56, 72, 88, 600, 1112, 1128


class Buf:
    def __init__(self, name, space, rng=None):
        self.name, self.space, self.rng, self.aliases = name, space, rng, []


class Tile:
    def __init__(self, buf, ap):
        self.buf, self.ap = buf, ap


def _key(x):
    if isinstance(x, Tile):
        return (x.buf, None)
    t, i = x
    return (t.buf, i)


class Sched:
    CE = ("pe", "act", "dve", "pool")

    def __init__(self, nc, es):
        self.nc, self.es = nc, es
        self.q = {e: [] for e in self.CE + ("sp",)}
        self.cnt = {e: 0 for e in self.CE}
        self.semh = {e: es.enter_context(nc.semaphore("c_" + e)) for e in self.CE}
        self.seen = {e: {} for e in self.q}
        self.lastw, self.rd, self.keys_of, self.chans = {}, {}, {}, {}

    def _match(self, b, i):
        ks = self.keys_of.get(b, ())
        if i is None:
            return [(b, j) for j in ks]
        return [(b, j) for j in (i, None) if j in ks]

    def _deps(self, reads, writes):
        ev = {}

        def add(e):
            if e is not None and ev.get(e[0], 0) < e[1]:
                ev[e[0]] = e[1]

        for (b, i) in reads:
            for k in self._match(b, i):
                add(self.lastw.get(k))
        for (b, i) in writes:
            ks = self._match(b, i)
            for a in b.aliases:
                ks = ks + self._match(a, None)
            for k in ks:
                add(self.lastw.get(k))
                for sk, v in self.rd.get(k, {}).items():
                    add((sk, v))
        return ev

    def _record(self, reads, writes, e):
        for (b, i) in writes:
            if i is None:
                for k in self._match(b, None):
                    self.lastw[k] = e
                    self.rd[k] = {}
            self.keys_of.setdefault(b, set()).add(i)
            self.lastw[(b, i)] = e
            self.rd[(b, i)] = {}
        for (b, i) in reads:
            self.keys_of.setdefault(b, set()).add(i)
            d = self.rd.setdefault((b, i), {})
            if d.get(e[0], 0) < e[1]:
                d[e[0]] = e[1]

    def _waits(self, eng, ev):
        w = []
        for sk, v in ev.items():
            if eng == "pe" and sk == "pe":
                continue
            if self.seen[eng].get(sk, 0) >= v:
                continue
            self.seen[eng][sk] = v
            w.append((sk, v))
        return w

    def emit(self, eng, fn, reads=(), writes=(), inc=True):
        reads = [_key(x) for x in reads]
        writes = [_key(x) for x in writes]
        assert inc or eng == "pe"
        w = self._waits(eng, self._deps(reads, writes))
        if inc:
            self.cnt[eng] += 1
            e = (eng, self.cnt[eng])
        else:
            e = (eng, self.cnt[eng] + 1)
        self._record(reads, writes, e)
        self.q[eng].append((w, fn, eng if inc else None))

    def dma(self, q, fn, reads=(), writes=(), chan="d", K=4):
        reads = [_key(x) for x in reads]
        writes = [_key(x) for x in writes]
        st = self.chans.get(chan)
        if st is None:
            st = {"i": 0, "K": K}
            for s in range(K):
                self.semh[("dma", chan, s)] = self.es.enter_context(self.nc.semaphore("d_%s%d" % (chan, s)))
            self.chans[chan] = st
        i, K = st["i"], st["K"]
        st["i"] += 1
        sk = ("dma", chan, i % K)
        ev = self._deps(reads, writes)
        if i >= K:
            ev[sk] = max(ev.get(sk, 0), 16 * (i // K))
        w = self._waits(q, ev)
        self._record(reads, writes, (sk, 16 * (i // K + 1)))
        self.q[q].append((w, fn, sk))

    def final_wait(self, q, tiles):
        ev = self._deps([_key(t) for t in tiles], [])
        self.q[q].append((self._waits(q, ev), None, None))


    def check(self):
        sem = {}
        pos = {e: 0 for e in self.q}
        progress = True
        while progress:
            progress = False
            for e, lst in self.q.items():
                while pos[e] < len(lst):
                    waits, fn, inc = lst[pos[e]]
                    if any(sem.get(sk, 0) < v for sk, v in waits):
                        break
                    if fn is not None and inc is not None:
                        sem[inc] = sem.get(inc, 0) + (16 if isinstance(inc, tuple) else 1)
                    pos[e] += 1
                    progress = True
        stuck = {e: (pos[e], len(lst)) for e, lst in self.q.items() if pos[e] < len(lst)}
        if stuck:
            msg = []
            for e, (p, n) in stuck.items():
                waits = self.q[e][p][0]
                msg.append("%s stuck at %d/%d waiting %s" % (e, p, n, [(sk, v, sem.get(sk, 0)) for sk, v in waits if sem.get(sk, 0) < v]))
            raise RuntimeError("sync deadlock: " + "; ".join(msg))

    def build(self, block):
        for eng, deco in (("pe", block.tensor), ("act", block.scalar), ("dve", block.vector),
                          ("pool", block.gpsimd), ("sp", block.sync)):
            def body(e, lst=self.q[eng]):
                for waits, fn, inc in lst:
                    for sk, v in waits:
                        e.wait_ge(self.semh[sk], v)
                    if fn is None:
                        continue
                    ins = fn(e)
                    if inc is None:
                        continue
                    if isinstance(inc, tuple):
                        ins.then_inc(self.semh[inc], 16)
                    else:
                        ins.then_inc(self.semh[inc], 1)
            deco(body)


class Arena:
    def __init__(self, nc, es, nbytes):
        self.cap = nbytes
        self.t = es.enter_context(nc.sbuf_tensor("arena", [128, nbytes // 2], BF16))
        self.pos = 0
        self.bufs = []

    def alloc(self, name, free, dtype, parts=128):
        esz = 2 if dtype == BF16 else 4
        n = int(np.prod(free))
        nb = n * esz
        nba = (nb + 63) // 64 * 64
        off = self.pos
        self.pos += nba
        assert self.pos <= self.cap, ("SBUF arena overflow", name, self.pos)
        ap = self.t[0:parts, off // 2: off // 2 + nb // 2]
        if dtype != BF16:
            ap = ap.bitcast(dtype)
        if len(free) == 2:
            ap = ap.rearrange("p (a b) -> p a b", a=free[0])
        elif len(free) == 3:
            ap = ap.rearrange("p (a b c) -> p a b c", a=free[0], b=free[1])
        b = Buf(name, "sb", (off, off + nba))
        for o in self.bufs:
            if o.rng[0] < b.rng[1] and b.rng[0] < o.rng[1]:
                b.aliases.append(o)
                o.aliases.append(b)
        self.bufs.append(b)
        return Tile(b, ap)

    def mark(self):
        return self.pos

    def reset(self, m):
        self.pos = m


def build_program(debug=False, stop=None):
    nc = bass.Bass("TRN2", target_bir_lowering=False)
    es = contextlib.ExitStack()

    in_names = []

    def din(name, shape, dt=F32):
        in_names.append(name)
        return Tile(Buf(name, "dram"), nc.dram_tensor(name, list(shape), dt, kind="ExternalInput").ap())

    def dscr(name, shape, dt=F32, out=False):
        kind = "ExternalOutput" if (out or debug) else "Internal"
        return Tile(Buf(name, "dram"), nc.dram_tensor(name, list(shape), dt, kind=kind).ap())

    H0 = din("h0", [L, D])
    H0T = din("h0T", [D, L])
    PARd = din("par", [128, NPAR])
    ROWSd = din("rows", [8, D])
    RBd = din("rb", [2, NE])
    CSTF = din("cstf", [128, 224])
    CSTB = din("cstb", [128, 512], BF16)
    ZROWS = din("zrows", [1024, D], BF16)
    W_IN = din("lru_w_in", [D, 2 * D])
    W_A = din("lru_wa", [2, 4, 256, 256])
    W_X = din("lru_wx", [2, 4, 256, 256])
    W_LO = din("lru_w_out", [D, D])
    W_QKV = din("na_w_qkv", [D, 3 * D])
    W_NO = din("na_w_out", [D, D])
    RPd = din("rp", [16, 19, 127])
    W_R = din("router_w", [2, D, NE])
    W_GU = din("moe_w_gu", [2, NE, D, 2 * D])
    W_DN = din("moe_w_down", [2, NE, D, D])
    B_DN = din("moe_b_down", [2, NE, D])
    OUT = dscr("out", [SEQ, D], out=True)

    XRd = dscr("xr_s", [8, 128, L])
    GYd = dscr("gy_s", [8, 128, L], BF16)
    ZTd = dscr("zt_s", [8, 128, L], BF16)
    H1 = dscr("h1_s", [L, D])
    H2 = dscr("h2_s", [L, D])
    H3 = dscr("h3_s", [L, D])
    XG = dscr("xg_s", [DUMP + 1, D], BF16)
    YG = dscr("yg_s", [DUMP + 1, D])
    ATTD = dscr("att_s", [SEQ, D], BF16)

    S = Sched(nc, es)
    A = Arena(nc, es, 207 * 1024)
    PS = []
    for i in range(8):
        t = es.enter_context(nc.psum_tensor("ps%d" % i, [128, 512], F32))
        PS.append(Tile(Buf("ps%d" % i, "ps"), t[:]))
    psi = [0]

    def next_ps():
        p = PS[psi[0] % 8]
        psi[0] += 1
        return p

    ps_free = list(range(8))

    def ps_get():
        while not ps_free:
            yield
        return PS[ps_free.pop(0)]

    def ps_put(*ps):
        for p in ps:
            ps_free.append(PS.index(p))

    def mm(out, lhsT, rhs, start, stop, reads, writes, inc):
        S.emit("pe", lambda e: e.matmul(out, lhsT, rhs, start=start, stop=stop), reads, writes, inc)

    PAR = A.alloc("par", [NPAR], F32)
    CF = A.alloc("cstf", [224], F32)
    CB = A.alloc("cstb", [512], BF16)
    IDF = CF.ap[:, 0:128]
    EOFFM = CF.ap[:, 128:160]
    CMASK = CF.ap[:, 160:224]
    IDB = CB.ap[:, 0:128]
    UTRI = CB.ap[:, 128:256]
    ONESB = CB.ap[:, 256:384]
    J2 = CB.ap[:, 384:512]
    SP = A.alloc("sp", [32], F32)
    BL1 = A.alloc("bl1", [2, NE, 8], F32)
    HBIAS = A.alloc("hbias", [32], F32)
    SPH = A.alloc("sph", [16], F32)
    GALL = A.alloc("gall", [33, NE], F32)
    GK = A.alloc("gk", [33, 4], F32)
    DI = A.alloc("di", [33, 4], I32)
    CNT = A.alloc("cnt", [NE], F32)
    S.dma("sp", lambda e: e.dma_start(out=PAR.ap, in_=PARd.ap), [], [PAR], "ld")
    S.dma("sp", lambda e: e.dma_start(out=CF.ap, in_=CSTF.ap), [], [CF], "ld")
    S.dma("sp", lambda e: e.dma_start(out=CB.ap, in_=CSTB.ap), [], [CB], "ld")
    S.emit("act", lambda e: e.activation(out=SP.ap[:, 0:16], in_=PAR.ap[:, P_LAM:P_LAM + 16], func=AF.Exp, scale=-1.0), [PAR], [SP])
    S.emit("act", lambda e: e.activation(out=SP.ap[:, 0:16], in_=SP.ap[:, 0:16], func=AF.Ln, bias=1.0, scale=1.0), [SP], [SP])
    S.emit("dve", lambda e: e.tensor_scalar(out=SP.ap[:, 16:32], in0=SP.ap[:, 0:16], scalar1=-16.0, scalar2=None, op0=ALU.mult), [SP], [SP])
    S.emit("dve", lambda e: e.tensor_scalar(out=SP.ap[:, 0:16], in0=SP.ap[:, 0:16], scalar1=-8.0, scalar2=None, op0=ALU.mult), [SP], [SP])
    S.emit("dve", lambda e: e.tensor_scalar(out=SPH.ap, in0=SP.ap[:, 0:16], scalar1=0.5, scalar2=None, op0=ALU.mult), [SP], [SPH])
    S.emit("dve", lambda e: e.tensor_scalar(out=HBIAS.ap, in0=PAR.ap[:, P_BA:P_BA + 32], scalar1=0.5, scalar2=None, op0=ALU.mult), [PAR], [HBIAS])
    for li in range(2):
        c0 = (P_BGU0, P_BGU1)[li]
        src = PAR.ap[:, c0:c0 + 512].rearrange("p (e f) -> p e f", e=NE)[:, :, 8:16]
        S.emit("dve", lambda e, src=src, li=li: e.tensor_scalar(out=BL1.ap[:, li], in0=src, scalar1=1.0, scalar2=None, op0=ALU.add), [PAR], [BL1])
    base_mark = A.mark()

    def phase_1a():
        m = A.mark()
        h0T = A.alloc("h0T", [KC, L], BF16)
        win = A.alloc("win", [KC, 2 * D], BF16)
        ms = A.mark()
        stg = [A.alloc("stg%d" % i, [L], F32) for i in range(2)]
        A.reset(ms)
        stb = [A.alloc("stb%d" % i, [L], BF16) for i in range(2)]
        for kc in range(KC):
            for (a, b) in ((0, 2048), (2048, 4096), (4096, L)):
                S.dma("pool", lambda e, kc=kc, a=a, b=b: e.dma_start(out=h0T.ap[:, kc, a:b], in_=H0T.ap[kc * 128:(kc + 1) * 128, a:b]),
                      [], [(h0T, kc)], "ldc", 8)
            S.dma("pool", lambda e, kc=kc: e.dma_start(out=win.ap[:, kc, :], in_=W_IN.ap[kc * 128:(kc + 1) * 128, :]),
                  [], [(win, kc)], "ldc", 8)
        for fc in range(16):
            st = (stg if fc < 8 else stb)[fc % 2]
            for gi, (t0, n) in enumerate(GROUPS):
                ps = next_ps()
                for kc in range(KC):
                    mm(ps.ap[:, 0:n], win.ap[:, kc, fc * 128:(fc + 1) * 128], h0T.ap[:, kc, t0:t0 + n], kc == 0, kc == KC - 1,
                       [(win, kc), (h0T, kc)], [ps], kc == KC - 1)
                if fc < 8:
                    if gi % 2 == 0:
                        S.emit("act", lambda e, st=st, ps=ps, t0=t0, n=n: e.copy(out=st.ap[:, t0:t0 + n], in_=ps.ap[:, 0:n]), [ps], [(st, gi)])
                    else:
                        S.emit("dve", lambda e, st=st, ps=ps, t0=t0, n=n: e.tensor_copy(out=st.ap[:, t0:t0 + n], in_=ps.ap[:, 0:n]), [ps], [(st, gi)])
                else:
                    S.emit("act", lambda e, st=st, ps=ps, t0=t0, n=n: e.activation(out=st.ap[:, t0:t0 + n], in_=ps.ap[:, 0:n], func=AF.Gelu_apprx_tanh),
                           [ps], [(st, gi)])
            if fc < 8:
                S.dma("sp", lambda e, st=st, fc=fc: e.dma_start(out=XRd.ap[fc], in_=st.ap), [st], [(XRd, fc)], "st")
            else:
                S.dma("sp", lambda e, st=st, fc=fc: e.dma_start(out=GYd.ap[fc - 8], in_=st.ap), [st], [(GYd, fc - 8)], "st")
        A.reset(m)

    def phase_1b():
        m = A.mark()
        WA = A.alloc("wa", [16, 256], BF16)
        WX = A.alloc("wx", [16, 256], BF16)
        S.dma("pool", lambda e: e.dma_start(out=WA.ap, in_=W_A.ap.rearrange("d n (i p) j -> p (d n i) j", p=128)), [], [WA], "ldc", 8)
        S.dma("pool", lambda e: e.dma_start(out=WX.ap, in_=W_X.ap.rearrange("d n (i p) j -> p (d n i) j", p=128)), [], [WX], "ldc", 8)
        XC = A.alloc("xc", [2, L], F32)
        XCB = A.alloc("xcb", [2, L], BF16)
        HFB = [[A.alloc("h%d_%d" % (d, i), [L], F32) for d in range(2)] for i in range(2)]
        GYt = A.alloc("gyt", [L], BF16)
        Zt = A.alloc("zt", [L], BF16)
        HB0 = 2048
        HLMAX = L - HB0
        m2 = A.mark()
        XRP = A.alloc("xrp", [2, L + 4], F32)
        A.reset(m2)
        GA = [A.alloc("ga%d" % d, [HLMAX], F32) for d in range(2)]
        GX = [A.alloc("gx%d" % d, [HLMAX], F32) for d in range(2)]
        MMt = [A.alloc("mm%d" % d, [HLMAX], F32) for d in range(2)]
        HGROUPS = [[(512 * g, 512) for g in range(4)], [(512 * g, 512) for g in range(4)] + [(2048, L - HB0 - 2048)]]
        done = {}

        def chain(nb, jc, d):
            c = 2 * nb + jc
            H = HFB[jc][d]
            ga, gx, mmt = GA[d], GX[d], MMt[d]
            shcol = SPH.ap[:, d * 8 + c:d * 8 + c + 1]
            for hi, half in enumerate((0, 1) if d == 0 else (1, 0)):
                h0 = half * HB0
                hl = HB0 if half == 0 else L - HB0
                for gi, (g0, n) in enumerate(HGROUPS[half]):
                    t0 = h0 + g0
                    for (Wt, Gt, pcol) in ((WA, ga, 0), (WX, gx, 16)):
                        ps = yield from ps_get()
                        for ic in range(2):
                            mm(ps.ap[:, 0:n], Wt.ap[:, d * 8 + nb * 2 + ic, jc * 128:(jc + 1) * 128], XCB.ap[:, ic, t0:t0 + n], ic == 0, ic == 1,
                               [Wt, (XCB, ic)], [ps], ic == 1)
                        bcol = HBIAS.ap[:, pcol + d * 8 + c:pcol + d * 8 + c + 1]
                        S.emit("act", lambda e, Gt=Gt, ps=ps, g0=g0, n=n, bcol=bcol: e.activation(out=Gt.ap[:, g0:g0 + n], in_=ps.ap[:, 0:n], func=AF.Tanh,
                                                                                                 bias=bcol, scale=0.5), [ps, HBIAS], [(Gt, gi)])
                        ps_put(ps)
                        yield
                S.emit("act", lambda e, hl=hl: e.activation(out=ga.ap[:, 0:hl], in_=ga.ap[:, 0:hl], func=AF.Exp, scale=shcol, bias=shcol), [ga, SPH], [ga])
                for _ in range(3):
                    yield
                S.emit("dve", lambda e, hl=hl: e.tensor_tensor(out=mmt.ap[:, 0:hl], in0=ga.ap[:, 0:hl], in1=ga.ap[:, 0:hl], op=ALU.mult), [ga], [mmt])
                for _ in range(3):
                    yield
                S.emit("act", lambda e, hl=hl: e.activation(out=mmt.ap[:, 0:hl], in_=mmt.ap[:, 0:hl], func=AF.Sqrt, bias=0.25, scale=-0.25), [mmt], [mmt])
                yield
                if hi == 0:
                    sc = 0 if d == 0 else hl - 1
                    S.emit("pool", lambda e, sc=sc: e.memset(mmt.ap[:, sc:sc + 1], 0.5), [mmt], [mmt])
                    yield
                S.emit("dve", lambda e, h0=h0, hl=hl: e.scalar_tensor_tensor(out=gx.ap[:, 0:hl], in0=gx.ap[:, 0:hl], scalar=1.0, in1=XC.ap[:, jc, h0:h0 + hl],
                                                                          op0=ALU.add, op1=ALU.mult), [gx, (XC, jc)], [gx])
                yield
                S.emit("pool", lambda e, hl=hl: e.tensor_tensor(out=gx.ap[:, 0:hl], in0=gx.ap[:, 0:hl], in1=mmt.ap[:, 0:hl], op=ALU.mult), [gx, mmt], [gx])
                for _ in range(5):
                    yield
                if hi == 1:
                    fi = 0 if d == 0 else hl - 1
                    prev = H.ap[:, HB0 - 1:HB0] if d == 0 else H.ap[:, HB0:HB0 + 1]
                    S.emit("dve", lambda e, fi=fi, prev=prev: e.scalar_tensor_tensor(out=gx.ap[:, fi:fi + 1], in0=ga.ap[:, fi:fi + 1], scalar=prev, in1=gx.ap[:, fi:fi + 1],
                                                                                  op0=ALU.mult, op1=ALU.add), [ga, gx, (H, 1 - half)], [gx])
                    yield
                if d == 0:
                    S.emit("dve", lambda e, h0=h0, hl=hl: e.tensor_tensor_scan(out=H.ap[:, h0:h0 + hl], data0=ga.ap[:, 0:hl], data1=gx.ap[:, 0:hl], initial=0.0,
                                                                            op0=ALU.mult, op1=ALU.add), [ga, gx], [(H, half)])
                else:
                    S.emit("dve", lambda e, h0=h0, hl=hl: e.tensor_tensor_scan(out=H.ap[:, h0:h0 + hl][:, ::-1], data0=ga.ap[:, 0:hl][:, ::-1], data1=gx.ap[:, 0:hl][:, ::-1],
                                                                            initial=0.0, op0=ALU.mult, op1=ALU.add), [ga, gx], [(H, half)])
                yield
            done[(nb, jc)] = done.get((nb, jc), 0) + 1

        def tail(nb, jc):
            c = 2 * nb + jc
            while done.get((nb, jc), 0) < 2:
                yield
            Hf, Hb = HFB[jc]
            S.dma("sp", lambda e: e.dma_start(out=GYt.ap, in_=GYd.ap[c]), [(GYd, c)], [GYt], "ld")
            yield
            S.emit("pool", lambda e: e.tensor_tensor(out=Hf.ap, in0=Hf.ap, in1=Hb.ap, op=ALU.add), [Hf, Hb], [Hf])
            for _ in range(16):
                yield
            S.emit("dve", lambda e: e.tensor_tensor(out=Zt.ap, in0=Hf.ap, in1=GYt.ap, op=ALU.mult), [Hf, GYt], [Zt])
            yield
            S.dma("sp", lambda e: e.dma_start(out=ZTd.ap[c], in_=Zt.ap), [Zt], [(ZTd, c)], "st")
            yield

        for nb in range(4):
            S.emit("pool", lambda e: e.memset(XRP.ap[:, :, 0:2], 0.0), [], [XRP])
            S.emit("pool", lambda e: e.memset(XRP.ap[:, :, L + 2:L + 4], 0.0), [], [XRP])
            for jc in range(2):
                c = 2 * nb + jc
                S.dma("sp", lambda e, jc=jc, c=c: e.dma_start(out=XRP.ap[:, jc, 2:L + 2], in_=XRd.ap[c]), [(XRd, c)], [(XRP, jc)], "ld")
                cw = lambda j, c=c: PAR.ap[:, P_CW + c * 4 + j:P_CW + c * 4 + j + 1]
                S.emit("dve", lambda e, jc=jc, c=c, cw=cw: e.tensor_scalar(out=XC.ap[:, jc, :], in0=XRP.ap[:, jc, 0:L], scalar1=cw(0),
                                                                          scalar2=PAR.ap[:, P_CB + c:P_CB + c + 1], op0=ALU.mult, op1=ALU.add),
                       [(XRP, jc), PAR], [(XC, jc)])
                for j in range(1, 4):
                    S.emit("dve", lambda e, jc=jc, j=j, cw=cw: e.scalar_tensor_tensor(out=XC.ap[:, jc, :], in0=XRP.ap[:, jc, j:j + L], scalar=cw(j),
                                                                                     in1=XC.ap[:, jc, :], op0=ALU.mult, op1=ALU.add),
                           [(XRP, jc), (XC, jc), PAR], [(XC, jc)])
                S.emit("act", lambda e, jc=jc: e.copy(out=XCB.ap[:, jc, :], in_=XC.ap[:, jc, :]), [(XC, jc)], [(XCB, jc)])
            zero_fill_xg(nb, 4)
            gens = []
            for jc in range(2):
                gens += [chain(nb, jc, 0), chain(nb, jc, 1), tail(nb, jc)]
            interleave(gens, 3, 6)
        A.reset(m)

    EPT = {}
    regs = {}

    def bcreg(e):
        if "bc" not in regs:
            regs["bc"] = e.to_reg(DUMP)
        return regs["bc"]

    def alloc_epilogue():
        LNG = A.alloc("lng", [D], F32)
        LNB = A.alloc("lnb", [D], F32)
        RBT = A.alloc("rbt", [NE], F32)
        RW = A.alloc("rw", [KC, NE], F32)
        Rt = [A.alloc("r%d" % i, [D], F32) for i in range(NB)]
        Yt = [A.alloc("y%d" % i, [D], F32) for i in range(NB)]
        XBt = [A.alloc("xb%d" % i, [D], BF16) for i in range(NB)]
        YTt = [A.alloc("ytt%d" % i, [KC, 128], F32) for i in range(NB)]
        SMALL = [A.alloc("sm%d" % i, [232 + 256], F32) for i in range(NB)]
        MSKB = [A.alloc("mskb%d" % i, [NE], BF16) for i in range(NB)]
        EPT["end"] = A.mark()
        EPT.update(LNG=LNG, LNB=LNB, RBT=RBT, RW=RW, Rt=Rt, Yt=Yt, XBt=XBt, YTt=YTt, SMALL=SMALL, MSKB=MSKB)

    ep_mark = A.mark()
    epi = [0]

    def load_ln_params(ln_idx, li=None):
        LNG, LNB, RBT, RW = EPT["LNG"], EPT["LNB"], EPT["RBT"], EPT["RW"]
        S.dma("sp", lambda e: e.dma_start(out=LNG.ap, in_=ROWSd.ap[2 * ln_idx:2 * ln_idx + 1, :].to_broadcast([128, D])), [], [LNG], "ld")
        S.dma("sp", lambda e: e.dma_start(out=LNB.ap, in_=ROWSd.ap[2 * ln_idx + 1:2 * ln_idx + 2, :].to_broadcast([128, D])), [], [LNB], "ld")
        if li is not None:
            S.dma("sp", lambda e: e.dma_start(out=RBT.ap, in_=RBd.ap[li:li + 1, :].to_broadcast([128, NE])), [], [RBT], "ld")
            S.dma("sp", lambda e: e.dma_start(out=RW.ap, in_=W_R.ap[li].rearrange("(k p) e -> p k e", p=128)), [], [RW], "ld")
            S.emit("pool", lambda e: e.memset(CNT.ap, 0.0), [], [CNT])

    def epilogue(ci, ps_pair, extra, hprev, hnext, route_li=None, h2t=None, out_rows=None):
        LNG, LNB, RBT, RW, Rt, Yt, XBt, YTt, SMALL, MSKB = (EPT[x] for x in ("LNG", "LNB", "RBT", "RW", "Rt", "Yt", "XBt", "YTt", "SMALL", "MSKB"))
        t0, n = CHUNKS[ci]
        k = epi[0] % NB
        epi[0] += 1
        R, Y, XB, SM = Rt[k], Yt[k], XBt[k], SMALL[k]
        YTt, MSKB = YTt[k], MSKB[k]
        ST = SM.ap[:, 0:12].rearrange("p (a b) -> p a b", a=2)
        MV = SM.ap[:, 12:14]
        RS = SM.ap[:, 14:15]
        NMX = SM.ap[:, 15:16]
        o = 16
        LG = SM.ap[:, o:o + 32]
        MSK = SM.ap[:, o + 32:o + 64]
        EX = SM.ap[:, o + 64:o + 96]
        POS = SM.ap[:, o + 96:o + 128]
        V1 = SM.ap[:, o + 128:o + 160]
        OH = SM.ap[:, o + 160:o + 192]
        MX = SM.ap[:, o + 192:o + 200]
        SS = SM.ap[:, o + 200:o + 201]
        DK = SM.ap[:, o + 208:o + 212]
        JNK = SM.ap[:, 232:488]
        S.dma("sp", lambda e: e.dma_start(out=R.ap[0:n], in_=hprev.ap[t0:t0 + n, :]), [(hprev, ci)], [R], "ld")
        yield
        for h in range(2):
            S.emit("dve", lambda e, h=h: e.scalar_tensor_tensor(out=R.ap[0:n, h * 512:(h + 1) * 512], in0=R.ap[0:n, h * 512:(h + 1) * 512], scalar=ALPHA,
                                                               in1=ps_pair[h].ap[0:n, :], op0=ALU.mult, op1=ALU.add), [R, ps_pair[h]], [R])
            yield
        ps_put(*ps_pair)
        if extra is not None:
            for kk in range(4):
                S.emit("dve", lambda e, kk=kk: e.scalar_tensor_tensor(out=R.ap[0:n], in0=extra.ap[0:n, kk, :], scalar=GK.ap[0:n, ci, kk:kk + 1], in1=R.ap[0:n],
                                                                     op0=ALU.mult, op1=ALU.add), [R, (extra, kk), (GK, ci)], [R])
                yield
        for h in range(2):
            S.emit("dve", lambda e, h=h: e.bn_stats(out=ST[0:n, h, :], in_=R.ap[0:n, h * 512:(h + 1) * 512]), [R], [(SM, "st%d" % h)])
            yield
        S.emit("dve", lambda e: e.bn_aggr(out=MV[0:n], in_=SM.ap[0:n, 0:12]), [(SM, "st0"), (SM, "st1")], [(SM, "mv")])
        yield
        S.emit("act", lambda e: e.activation(out=RS[0:n], in_=MV[0:n, 1:2], func=AF.Ln, bias=EPS, scale=1.0), [(SM, "mv")], [(SM, "rs")])
        yield
        S.emit("act", lambda e: e.activation(out=RS[0:n], in_=RS[0:n], func=AF.Exp, scale=-0.5), [(SM, "rs")], [(SM, "rs")])
        yield
        S.emit("dve", lambda e: e.scalar_tensor_tensor(out=Y.ap[0:n], in0=R.ap[0:n], scalar=MV[0:n, 0:1], in1=LNG.ap[0:n], op0=ALU.subtract, op1=ALU.mult),
               [R, (SM, "mv"), LNG], [Y])
        yield
        S.emit("dve", lambda e: e.scalar_tensor_tensor(out=Y.ap[0:n], in0=Y.ap[0:n], scalar=RS[0:n], in1=LNB.ap[0:n], op0=ALU.mult, op1=ALU.add),
               [Y, (SM, "rs"), LNB], [Y])
        yield
        if hnext is not None:
            S.dma("pool", lambda e: e.dma_start(out=hnext.ap[t0:t0 + n, :], in_=Y.ap[0:n]), [Y], [(hnext, ci)], "stp")
            yield
        if out_rows is not None:
            S.dma("pool", lambda e: e.dma_start(out=OUT.ap[out_rows:out_rows + n, :], in_=Y.ap[0:n]), [Y], [(OUT, ci)], "stp")
            yield
        if route_li is None and h2t is None:
            return
        pt = ((yield from ps_get()), (yield from ps_get()))
        for kk in range(KC):
            p = pt[kk // 4]
            S.emit("pe", lambda e, p=p, kk=kk: e.transpose(p.ap[:, (kk % 4) * 128:(kk % 4) * 128 + n], Y.ap[0:n, kk * 128:(kk + 1) * 128], IDF[0:n, 0:n]),
                   [Y, CF], [p], kk % 4 == 3)
            yield
        if h2t is not None:
            for hh in range(2):
                S.emit("act", lambda e, hh=hh: e.copy(out=h2t.ap[:, hh * 4:(hh + 1) * 4, t0:t0 + n],
                                                       in_=pt[hh].ap.rearrange("p (a b) -> p a b", a=4)[:, :, 0:n]), [pt[hh]], [(h2t, ci)])
                yield
        if route_li is None:
            ps_put(*pt)
            return
        li = route_li
        for hh in range(2):
            S.emit("act", lambda e, hh=hh: e.copy(out=YTt.ap[:, hh * 4:(hh + 1) * 4, 0:n], in_=pt[hh].ap.rearrange("p (a b) -> p a b", a=4)[:, :, 0:n]),
                   [pt[hh]], [(YTt, hh)])
            yield
        ps_put(*pt)
        S.emit("act", lambda e: e.copy(out=XB.ap[0:n], in_=Y.ap[0:n]), [Y], [XB])
        yield
        pl = yield from ps_get()
        for kk in range(KC):
            mm(pl.ap[0:n, 0:NE], YTt.ap[:, kk, 0:n], RW.ap[:, kk, :], kk == 0, kk == KC - 1, [YTt, RW], [pl], kk == KC - 1)
            yield
        S.emit("dve", lambda e: e.tensor_tensor(out=LG[0:n], in0=pl.ap[0:n, 0:NE], in1=RBT.ap[0:n], op=ALU.add), [pl, RBT], [(SM, "lg")])
        yield
        ps_put(pl)
        S.emit("dve", lambda e: e.max(out=MX[0:n], in_=LG[0:n]), [(SM, "lg")], [(SM, "mx")])
        yield
        S.emit("dve", lambda e: e.tensor_scalar(out=MSK[0:n], in0=LG[0:n], scalar1=MX[0:n, 3:4], scalar2=None, op0=ALU.is_ge), [(SM, "lg"), (SM, "mx")], [(SM, "msk")])
        yield
        S.emit("dve", lambda e: e.tensor_scalar(out=NMX[0:n], in0=MX[0:n, 0:1], scalar1=-1.0, scalar2=None, op0=ALU.mult), [(SM, "mx")], [(SM, "nmx")])
        yield
        S.emit("act", lambda e: e.activation(out=EX[0:n], in_=LG[0:n], func=AF.Exp, bias=NMX[0:n], scale=1.0), [(SM, "lg"), (SM, "nmx")], [(SM, "ex")])
        yield
        S.emit("dve", lambda e: e.tensor_tensor(out=EX[0:n], in0=EX[0:n], in1=MSK[0:n], op=ALU.mult), [(SM, "ex"), (SM, "msk")], [(SM, "ex")])
        yield
        S.emit("dve", lambda e: e.tensor_reduce(out=SS[0:n], in_=EX[0:n], axis=AX.X, op=ALU.add), [(SM, "ex")], [(SM, "ss")])
        yield
        S.emit("dve", lambda e: e.reciprocal(out=SS[0:n], in_=SS[0:n]), [(SM, "ss")], [(SM, "ss")])
        yield
        S.emit("dve", lambda e: e.tensor_scalar(out=GALL.ap[0:n, ci, :], in0=EX[0:n], scalar1=SS[0:n], scalar2=None, op0=ALU.mult),
               [(SM, "ex"), (SM, "ss")], [(GALL, ci)])
        yield
        S.emit("act", lambda e: e.copy(out=MSKB.ap[0:n], in_=MSK[0:n]), [(SM, "msk")], [MSKB])
        yield
        pp = yield from ps_get()
        mm(pp.ap[0:n, 0:NE], UTRI[0:n, 0:n], MSKB.ap[0:n], True, True, [MSKB, CB], [pp], False)
        yield
        mm(pp.ap[:, NE:2 * NE], ONESB[0:n, :], MSKB.ap[0:n], True, True, [MSKB, CB], [pp], True)
        yield
        S.emit("dve", lambda e: e.tensor_tensor(out=POS[0:n], in0=pp.ap[0:n, 0:NE], in1=CNT.ap[0:n], op=ALU.add), [pp, CNT], [(SM, "pos")])
        S.emit("dve", lambda e: e.tensor_tensor(out=CNT.ap, in0=pp.ap[:, NE:2 * NE], in1=CNT.ap, op=ALU.add), [pp, CNT, (SM, "pos")], [CNT])
        yield
        ps_put(pp)
        S.emit("dve", lambda e: e.tensor_tensor(out=V1[0:n], in0=POS[0:n], in1=EOFFM[0:n], op=ALU.add), [(SM, "pos"), CF], [(SM, "v1")])
        yield
        S.emit("dve", lambda e: e.tensor_scalar(out=POS[0:n], in0=POS[0:n], scalar1=float(CAP), scalar2=None, op0=ALU.is_lt), [(SM, "pos"), (SM, "v1")], [(SM, "pos")])
        yield
        S.emit("dve", lambda e: e.tensor_tensor(out=V1[0:n], in0=V1[0:n], in1=POS[0:n], op=ALU.mult), [(SM, "pos"), (SM, "v1")], [(SM, "v1")])
        yield
        for kk in range(4):
            j0 = JNK[:, 64 * kk:64 * kk + 32]
            j1 = JNK[:, 64 * kk + 32:64 * kk + 64]
            S.emit("dve", lambda e, kk=kk, j0=j0: e.scalar_tensor_tensor(out=j0[0:n], in0=LG[0:n], scalar=MX[0:n, kk:kk + 1], in1=V1[0:n], op0=ALU.is_equal, op1=ALU.mult,
                                                                      accum_out=DK[0:n, kk:kk + 1]), [(SM, "lg"), (SM, "mx"), (SM, "v1")], [(SM, "dk%d" % kk)])
            yield
            S.emit("dve", lambda e, kk=kk, j1=j1: e.scalar_tensor_tensor(out=j1[0:n], in0=LG[0:n], scalar=MX[0:n, kk:kk + 1], in1=GALL.ap[0:n, ci, :], op0=ALU.is_equal,
                                                                      op1=ALU.mult, accum_out=GK.ap[0:n, ci, kk:kk + 1]), [(SM, "lg"), (SM, "mx"), (GALL, ci)], [(GK, ci)])
            yield
        S.emit("dve", lambda e: e.tensor_scalar(out=DI.ap[0:n, ci, :], in0=DK[0:n], scalar1=float(DUMP), scalar2=None, op0=ALU.add), [(SM, "dk0"), (SM, "dk1"), (SM, "dk2"), (SM, "dk3")], [(DI, ci)])
        yield
        for kk in range(4):
            S.dma("pool", lambda e, kk=kk: e.indirect_dma_start(out=XG.ap, out_offset=bass.IndirectOffsetOnAxis(ap=DI.ap[0:n, ci, kk:kk + 1], axis=0),
                                                                in_=XB.ap[0:n], in_offset=None, bounds_check=bcreg(e), oob_is_err=False),
                  [XB, (DI, ci)], [(XG, "sc")], "ind", 8)
            yield


    def interleave(gens, width, stagger=0):
        gens = list(gens)
        active = []
        nxt = 0
        since = stagger
        while active or nxt < len(gens):
            if len(active) < width and nxt < len(gens) and (since >= stagger or not active):
                active.append(gens[nxt])
                nxt += 1
                since = 0
            since += 1
            for g in list(active):
                try:
                    next(g)
                except StopIteration:
                    active.remove(g)

    def phase_1c():
        m = A.mark()
        wo = A.alloc("wo", [KC, D], BF16)
        zg = [A.alloc("zg%d" % i, [KC, 512], BF16) for i in range(3)]
        for kc in range(KC):
            S.dma("pool", lambda e, kc=kc: e.dma_start(out=wo.ap[:, kc, :], in_=W_LO.ap[kc * 128:(kc + 1) * 128, :]), [], [(wo, kc)], "ldc", 8)
        load_ln_params(0, 0)
        def chunk_gen(ci, z, s_, n):
            pp = ((yield from ps_get()), (yield from ps_get()))
            for h in range(2):
                for kc in range(KC):
                    mm(pp[h].ap[0:n, :], z.ap[:, kc, s_ * 128:s_ * 128 + n], wo.ap[:, kc, h * 512:(h + 1) * 512], kc == 0, kc == KC - 1,
                       [z, (wo, kc)], [pp[h]], kc == KC - 1)
                yield
            yield from epilogue(ci, pp, None, H0, H1, route_li=0)

        gens = []
        ci = 0
        for gi, (g0, gn) in enumerate(GROUPS):
            z = zg[gi % 3]
            first = True
            for s_ in range(max(1, gn // 128)):
                def g_(ci=ci, z=z, s_=s_, n=min(128, gn), first=first, g0=g0, gn=gn):
                    if first:
                        S.dma("sp", lambda e: e.dma_start(out=z.ap[:, :, 0:gn], in_=ZTd.ap[:, :, g0:g0 + gn].rearrange("k p t -> p k t")), [ZTd], [z], "ld")
                    yield from chunk_gen(ci, z, s_, n)
                gens.append(g_())
                first = False
                ci += 1
        interleave(gens, NB, 30)
        A.reset(m)


    def phase_moe(li, hprev, hnext, ln_idx, chunk_ids, h2t=None, to_out=False):
        m = A.mark()
        A.reset(base_mark)
        WG = [A.alloc("wg%d" % i, [KC, 2 * D], BF16) for i in range(2)]
        WD = [A.alloc("wd%d" % i, [KC, D], BF16) for i in range(2)]
        XT = [A.alloc("xt%d" % i, [KC, CAP], BF16) for i in range(2)]
        ACTT = [A.alloc("actt%d" % i, [KC, CAP], BF16) for i in range(2)]
        XS = A.alloc("xs", [NSC, D], BF16)
        YS = [A.alloc("ys%d" % i, [D], F32) for i in range(2)]
        HN = CAP // 2
        NT = 3
        TT = [[A.alloc("t%d_%d" % (j, i), [HN], F32) for j in range(3)] for i in range(NT)]
        BG17 = A.alloc("bg17", [NE, 8], F32)
        bgu0 = (P_BGU0, P_BGU1)[li]
        SILU_C = 11.914 / (1.0 + float(np.exp(-11.914)))
        S.emit("dve", lambda en: en.tensor_scalar(out=BG17.ap, in0=PAR.ap[:, bgu0:bgu0 + 512].rearrange("p (e f) -> p e f", e=NE)[:, :, 0:8],
                                                  scalar1=1.702, scalar2=None, op0=ALU.mult), [PAR], [BG17])
        GUB = [PS[0:2], PS[2:4]]
        OTB = PS[4:8]
        oti = [0]

        def next_ot():
            p = OTB[oti[0] % 4]
            oti[0] += 1
            return p

        def load_wg(e):
            sl = e % 2
            for kc in range(KC):
                S.dma("pool", lambda en, kc=kc: en.dma_start(out=WG[sl].ap[:, kc, :], in_=W_GU.ap[li, e, kc * 128:(kc + 1) * 128, :]),
                      [], [(WG[sl], kc)], "ldw", 8)

        def load_wd(e):
            sl = e % 2
            for kc in range(KC):
                S.dma("pool", lambda en, kc=kc: en.dma_start(out=WD[sl].ap[:, kc, :], in_=W_DN.ap[li, e, kc * 128:(kc + 1) * 128, :]),
                      [], [(WD[sl], kc)], "ldw", 8)

        def load_xs(e):
            nf = CAP // 128
            S.dma("sp", lambda en: en.dma_start(out=XS.ap[:, 0:nf, :], in_=XG.ap[e * CAP:e * CAP + nf * 128, :].rearrange("(s p) d -> p s d", p=128)), [XG], [(XS, 0)], "ld")
            if CAP % 128:
                S.dma("sp", lambda en: en.dma_start(out=XS.ap[0:CAP % 128, nf, :], in_=XG.ap[e * CAP + nf * 128:(e + 1) * CAP, :]), [XG], [(XS, 1)], "ld")

        def transp(e):
            X = XT[e % 2]
            for k in range(KC):
                pt = next_ot()
                ptb = pt.ap.bitcast(BF16)
                for sc, (s0, sn) in enumerate(SLOTCH):
                    S.emit("pe", lambda en, ptb=ptb, sc=sc, k=k, s0=s0, sn=sn: en.transpose(ptb[:, s0:s0 + sn], XS.ap[0:sn, sc, k * 128:(k + 1) * 128], IDB[0:sn, 0:sn]),
                           [XS, CB], [pt], sc == NSC - 1)
                if k % 2 == 0:
                    S.emit("act", lambda en, ptb=ptb, k=k: en.copy(out=X.ap[:, k, :], in_=ptb[:, 0:CAP]), [pt], [(X, k)])
                else:
                    S.emit("dve", lambda en, ptb=ptb, k=k: en.tensor_copy(out=X.ap[:, k, :], in_=ptb[:, 0:CAP]), [pt], [(X, k)])

        tix = [0]

        def gate_up(e):
            sl = e % 2
            X, AC = XT[sl], ACTT[sl]
            for f in range(KC):
                for half in range(2):
                    hs = half * HN
                    SI, T3, SMt = TT[tix[0] % NT]
                    pg, pl = GUB[tix[0] % 2]
                    tix[0] += 1
                    for k in range(KC):
                        mm(pg.ap[:, 0:HN], WG[sl].ap[:, k, f * 128:(f + 1) * 128], X.ap[:, k, hs:hs + HN], k == 0, k == KC - 1,
                           [(WG[sl], k), (X, k)], [pg], k == KC - 1)
                    for k in range(KC):
                        mm(pl.ap[:, 0:HN], WG[sl].ap[:, k, D + f * 128:D + (f + 1) * 128], X.ap[:, k, hs:hs + HN], k == 0, k == KC - 1,
                           [(WG[sl], k), (X, k)], [pl], k == KC - 1)
                    bg = BG17.ap[:, e, f:f + 1]
                    bl = BL1.ap[:, li, e, f:f + 1]
                    S.emit("act", lambda en, SI=SI, pg=pg, bg=bg: en.activation(out=SI.ap, in_=pg.ap[:, 0:HN], func=AF.Silu, bias=bg, scale=1.702), [pg, BG17], [SI])
                    S.emit("dve", lambda en, T3=T3, pl=pl, bl=bl: en.tensor_scalar(out=T3.ap, in0=pl.ap[:, 0:HN], scalar1=bl, scalar2=-6.0, op0=ALU.add, op1=ALU.max),
                           [pl, BL1], [T3])
                    S.emit("dve", lambda en, SI=SI, SMt=SMt: en.tensor_scalar(out=SMt.ap, in0=SI.ap, scalar1=SILU_C, scalar2=1.0 / 1.702, op0=ALU.min, op1=ALU.mult),
                           [SI], [SMt])
                    S.emit("dve", lambda en, T3=T3, SMt=SMt, f=f, hs=hs: en.scalar_tensor_tensor(out=AC.ap[:, f, hs:hs + HN], in0=T3.ap, scalar=8.0, in1=SMt.ap,
                                                                                           op0=ALU.min, op1=ALU.mult), [T3, SMt], [(AC, (f, half))])

        def down(e):
            sl = e % 2
            AC = ACTT[sl]
            for sc, (s0, sn) in enumerate(SLOTCH):
                Y_ = YS[sc % 2]
                for dh in range(2):
                    pd = next_ot()
                    for f in range(KC):
                        mm(pd.ap[0:sn, :], AC.ap[:, f, s0:s0 + sn], WD[sl].ap[:, f, dh * 512:(dh + 1) * 512], f == 0, f == KC - 1,
                           [AC, (WD[sl], f)], [pd], f == KC - 1)
                    if dh == 0:
                        S.emit("act", lambda en, Y_=Y_, pd=pd, sn=sn: en.copy(out=Y_.ap[0:sn, 0:512], in_=pd.ap[0:sn, :]), [pd], [(Y_, 0)])
                    else:
                        S.emit("dve", lambda en, Y_=Y_, pd=pd, sn=sn: en.tensor_copy(out=Y_.ap[0:sn, 512:1024], in_=pd.ap[0:sn, :]), [pd], [(Y_, 1)])
                r0 = e * CAP + s0
                S.dma("sp", lambda en, Y_=Y_, r0=r0, sn=sn: en.dma_start(out=YG.ap[r0:r0 + sn, :], in_=Y_.ap[0:sn]), [Y_], [(YG, "y")], "st")

        load_wg(0)
        load_wd(0)
        load_xs(0)
        transp(0)
        load_xs(1)
        for e in range(NE):
            if e + 1 < NE:
                load_wg(e + 1)
                transp(e + 1)
                if e + 2 < NE:
                    load_xs(e + 2)
            gate_up(e)
            if e >= 1:
                down(e - 1)
            if e + 1 < NE:
                load_wd(e + 1)
        down(NE - 1)
        A.reset(m)
        pre = A.mark()
        if h2t:
            h2t = A.alloc("h2t", [KC, L], BF16)
        m = A.mark()
        YGa = [A.alloc("yga%d" % i, [4, D], F32) for i in range(NB)]
        BDN = A.alloc("bdn", [D], F32)
        GT = [A.alloc("gt%d" % i, [128], F32) for i in range(NB)]
        S.dma("sp", lambda en: en.dma_start(out=BDN.ap[0:NE], in_=B_DN.ap[li]), [], [BDN], "ld")
        load_ln_params(ln_idx, None)
        def comb_gen(it, ci):
            t0, n = CHUNKS[ci]
            Yg = YGa[it % NB]
            for kk in range(4):
                S.dma("pool", lambda en, kk=kk: en.indirect_dma_start(out=Yg.ap[0:n, kk, :], out_offset=None, in_=YG.ap,
                                                                   in_offset=bass.IndirectOffsetOnAxis(ap=DI.ap[0:n, ci, kk:kk + 1], axis=0),
                                                                   bounds_check=bcreg(en), oob_is_err=False),
                      [YG, (DI, ci)], [(Yg, kk)], "ind", 8)
            yield
            pg_ = yield from ps_get()
            S.emit("pe", lambda en: en.transpose(pg_.ap[0:NE, 0:n], GALL.ap[0:n, ci, :], IDF[0:n, 0:n]), [(GALL, ci), CF], [pg_], True)
            yield
            S.emit("act", lambda en: en.copy(out=GT[it % NB].ap[0:NE, 0:n], in_=pg_.ap[0:NE, 0:n]), [pg_], [GT[it % NB]])
            yield
            ps_put(pg_)
            pp = ((yield from ps_get()), (yield from ps_get()))
            for h in range(2):
                mm(pp[h].ap[0:n, :], GT[it % NB].ap[0:NE, 0:n], BDN.ap[0:NE, h * 512:(h + 1) * 512], True, True, [GT[it % NB], BDN], [pp[h]], True)
            yield
            yield from epilogue(ci, pp, Yg, hprev, hnext, route_li=None, h2t=(h2t or None), out_rows=((ci - 1) * 128 if to_out else None))

        interleave([comb_gen(it, ci) for it, ci in enumerate(chunk_ids)], NB, 10)
        A.reset(m)
        return h2t, pre


    def phase_na(H2T):
        m = A.mark()
        A.reset(base_mark)
        WQ = [A.alloc("wq%d" % i, [KC, 3, 128], BF16) for i in range(2)]
        QT = A.alloc("qt", [SEQ], BF16)
        KT = A.alloc("kt", [L], BF16)
        VA = A.alloc("va", [33, 2, 65], BF16)
        assert A.mark() <= EPT["end"]
        A.reset(m)
        ATT = [A.alloc("att%d" % i, [32, 128], BF16) for i in range(2)]
        E2 = A.alloc("e2", [18, 64], F32)
        E2b = A.alloc("e2b", [18, 64], BF16)
        E2c = A.alloc("e2c", [18, 64], BF16)
        EB = [[A.alloc("eb%d_%d" % (i, p), [5, 128], BF16) for p in range(5)] for i in range(2)]
        PT = [A.alloc("pt%d" % i, [6, 128], BF16) for i in range(8)]
        RC = [A.alloc("rc%d" % i, [1], F32) for i in range(8)]
        EMB = A.alloc("emb", [2], F32)
        S.emit("pool", lambda e: e.memset(VA.ap[:, :, :, 64:65], 1.0), [], [VA])

        def rs(qr):
            return min(max(qr - 4, 0), 56)

        def pat_of(rp):
            return {0: 0, 1: 1, 30: 3, 31: 4}.get(rp, 2)

        pat_rp = [0, 1, 2, 30, 31]

        def load_wq(hp):
            for j in range(3):
                S.dma("pool", lambda e, j=j, hp=hp: e.dma_start(out=WQ[hp % 2].ap[:, :, j, :],
                                                                in_=W_QKV.ap[:, j * D + hp * 128:j * D + (hp + 1) * 128].rearrange("(k p) c -> p k c", p=128)),
                      [], [(WQ[hp % 2], j)], "ldc", 8)

        load_wq(0)
        it = 0
        for hp in range(8):
            W = WQ[hp % 2]
            if hp + 1 < 8:
                load_wq(hp + 1)
            for g in range(8):
                ps = next_ps()
                for kc in range(KC):
                    mm(ps.ap, W.ap[:, kc, 0, :], H2T.ap[:, kc, 16 + 512 * g:16 + 512 * (g + 1)], kc == 0, kc == KC - 1, [(W, 0), H2T], [ps], kc == KC - 1)
                S.emit("act", lambda e, ps=ps, g=g: e.mul(QT.ap[:, 512 * g:512 * (g + 1)], ps.ap, 0.125), [ps], [(QT, g)])
            for gi, (t0, n) in enumerate(GROUPS):
                ps = next_ps()
                for kc in range(KC):
                    mm(ps.ap[:, 0:n], W.ap[:, kc, 1, :], H2T.ap[:, kc, t0:t0 + n], kc == 0, kc == KC - 1, [(W, 1), H2T], [ps], kc == KC - 1)
                S.emit("dve", lambda e, ps=ps, t0=t0, n=n: e.tensor_copy(out=KT.ap[:, t0:t0 + n], in_=ps.ap[:, 0:n]), [ps], [(KT, gi)])
            for ci, (t0, n) in enumerate(CHUNKS):
                ps = next_ps()
                for kc in range(KC):
                    mm(ps.ap[0:n, 0:128], H2T.ap[:, kc, t0:t0 + n], W.ap[:, kc, 2, :], kc == 0, kc == KC - 1, [(W, 2), H2T], [ps], kc == KC - 1)
                eng = ("act", "dve")[ci % 2]
                if eng == "act":
                    S.emit("act", lambda e, ps=ps, ci=ci, n=n: e.copy(out=VA.ap[0:n, ci, :, 0:64], in_=ps.ap[0:n, 0:128].rearrange("p (a b) -> p a b", a=2)),
                           [ps], [(VA, ci)])
                else:
                    S.emit("dve", lambda e, ps=ps, ci=ci, n=n: e.tensor_copy(out=VA.ap[0:n, ci, :, 0:64], in_=ps.ap[0:n, 0:128].rearrange("p (a b) -> p a b", a=2)),
                           [ps], [(VA, ci)])
            AT_ = ATT[hp % 2]
            for hh in range(2):
                h = 2 * hp + hh
                r0 = 64 * hh
                EBh = EB[hh]
                for rho in range(2):
                    src = bass.AP(RPd.ap.tensor, h * 19 * 127 + rho * 127, [[1, 64], [127, 18], [1, 64]])
                    S.dma("sp", lambda e, rho=rho, src=src: e.dma_start(out=E2.ap[64 * rho:64 * rho + 64], in_=src), [], [(E2, rho)], "ld")
                S.emit("act", lambda e: e.activation(out=E2.ap, in_=E2.ap, func=AF.Exp), [E2], [E2])
                S.emit("dve", lambda e: e.tensor_tensor(out=E2c.ap, in0=E2.ap, in1=CMASK.unsqueeze(1).to_broadcast([128, 18, 64]), op=ALU.mult), [E2, CF], [E2c])
                for j in range(3):
                    pf = next_ps()
                    mm(pf.ap[:, 0:384], J2, E2c.ap.rearrange("p a b -> p (a b)")[:, 384 * j:384 * (j + 1)], True, True, [E2c, CB], [pf], True)
                    S.emit(("act", "dve")[j % 2], lambda e, pf=pf, j=j: (e.copy if j % 2 == 0 else e.tensor_copy)(
                        out=E2b.ap.rearrange("p a b -> p (a b)")[:, 384 * j:384 * (j + 1)], in_=pf.ap[:, 0:384]), [pf], [(E2b, j)])
                for p_i, rp in enumerate(pat_rp):
                    jp0 = min(max(rp - 2, 0), 27)
                    T = EBh[p_i]
                    q = 0
                    for jpi in range(5):
                        for rho in range(2):
                            qr = 2 * rp + rho
                            kr0 = 2 * (jp0 + jpi)
                            di = kr0 - qr + 9
                            v0 = rs(qr) <= kr0 <= rs(qr) + 7
                            v1 = rs(qr) <= kr0 + 1 <= rs(qr) + 7
                            dst = T.ap[:, jpi, 64 * rho:64 * rho + 64]
                            eng = ("pool", "dve")[q % 2]
                            q += 1
                            if v0 or v1:
                                assert 0 <= di <= 17, (rp, jpi, rho, di)
                                S.emit(eng, lambda e, dst=dst, di=di: e.tensor_copy(out=dst, in_=E2b.ap[:, di, :]), [E2b], [(T, (jpi, rho))])
                                if not v0:
                                    S.emit(eng, lambda e, dst=dst: e.memset(dst[0:64], 0.0), [], [(T, (jpi, rho))])
                                if not v1:
                                    S.emit(eng, lambda e, dst=dst: e.memset(dst[64:128], 0.0), [], [(T, (jpi, rho))])
                            else:
                                S.emit(eng, lambda e, dst=dst: e.memset(dst, 0.0), [], [(T, (jpi, rho))])
                S.emit("act", lambda e, h=h, hh=hh: e.activation(out=EMB.ap[0:16, hh:hh + 1], in_=PAR.ap[0:16, P_MB + h:P_MB + h + 1], func=AF.Exp), [PAR], [(EMB, hh)])
                S.emit("pool", lambda e, hh=hh: e.memset(VA.ap[0:16, 0, hh, 64:65], 1.0), [(VA, 0)], [(VA, 0)])
                S.emit("dve", lambda e, hh=hh: e.tensor_scalar(out=VA.ap[0:16, 0, hh, :], in0=VA.ap[0:16, 0, hh, :], scalar1=EMB.ap[0:16, hh:hh + 1], scalar2=None,
                                                               op0=ALU.mult), [(VA, 0), (EMB, hh)], [(VA, 0)])

            def na_gen(hh, rp, slot, AT_):
                r0 = 64 * hh
                jp0 = min(max(rp - 2, 0), 27)
                T = EB[hh][pat_of(rp)]
                P_, Rc = PT[slot], RC[slot]
                psA = yield from ps_get()
                psB = yield from ps_get()
                qs = QT.ap[r0:r0 + 64, 128 * rp:128 * (rp + 1)]
                for jpi in range(5):
                    k0 = 16 + 128 * (jp0 + jpi)
                    o = psA.ap[:, 128 * jpi:128 * (jpi + 1)] if jpi < 4 else psB.ap[:, 0:128]
                    mm(o, KT.ap[r0:r0 + 64, k0:k0 + 128], qs, True, True, [KT, QT], [psA if jpi < 4 else psB], jpi == 3)
                mm(psB.ap[:, 128:256], KT.ap[r0:r0 + 64, 0:128], qs, True, True, [KT, QT], [psB], True)
                yield
                S.emit("act", lambda e: e.activation(out=P_.ap[:, 0:4, :], in_=psA.ap.rearrange("p (a b) -> p a b", a=4), func=AF.Exp), [psA], [(P_, 0)])
                S.emit("act", lambda e: e.activation(out=P_.ap[:, 4:6, :], in_=psB.ap[:, 0:256].rearrange("p (a b) -> p a b", a=2), func=AF.Exp), [psB], [(P_, 1)])
                ps_put(psA, psB)
                yield
                S.emit("dve", lambda e: e.tensor_tensor(out=P_.ap[:, 0:5, :], in0=P_.ap[:, 0:5, :], in1=T.ap, op=ALU.mult), [P_, T], [P_])
                yield
                po = yield from ps_get()
                for jpi in range(5):
                    mm(po.ap[:, 0:65], P_.ap[:, jpi, :], VA.ap[:, 1 + jp0 + jpi, hh, :], jpi == 0, False, [P_, VA], [po], False)
                mm(po.ap[:, 0:65], P_.ap[0:16, 5, :], VA.ap[0:16, 0, hh, :], False, True, [P_, VA], [po], True)
                yield
                S.emit("dve", lambda e: e.reciprocal(out=Rc.ap, in_=po.ap[:, 64:65]), [po], [Rc])
                yield
                S.emit("dve", lambda e: e.tensor_scalar(out=AT_.ap[:, rp, 64 * hh:64 * hh + 64], in0=po.ap[:, 0:64], scalar1=Rc.ap,
                                                        scalar2=None, op0=ALU.mult), [po, Rc], [(AT_, (rp, hh))])
                ps_put(po)
                yield

            def na_chain(hh, par, AT_=AT_):
                for j, rp in enumerate(range(par, 32, 2)):
                    yield from na_gen(hh, rp, (hh * 2 + par) * 2 + j % 2, AT_)

            interleave([na_chain(hh, par) for hh in range(2) for par in range(2)], 4, 1)
            S.dma("sp", lambda e, AT_=AT_, hp=hp: e.dma_start(out=ATTD.ap[:, hp * 128:(hp + 1) * 128].rearrange("(r p) c -> p r c", p=128), in_=AT_.ap),
                  [AT_], [(ATTD, hp)], "st")
        A.reset(m)
        m = A.mark()
        WNO = A.alloc("wno", [KC, D], BF16)
        ATt = [A.alloc("att_in%d" % i, [D], BF16) for i in range(NB)]
        ATk = [A.alloc("atk%d" % i, [KC, 128], BF16) for i in range(NB)]
        for kc in range(KC):
            S.dma("pool", lambda e, kc=kc: e.dma_start(out=WNO.ap[:, kc, :], in_=W_NO.ap[kc * 128:(kc + 1) * 128, :]), [], [(WNO, kc)], "ldc", 8)
        load_ln_params(2, 1)
        def no_gen(ci):
            a_in, a_k = ATt[ci % NB], ATk[ci % NB]
            S.dma("sp", lambda e: e.dma_start(out=a_in.ap, in_=ATTD.ap[(ci - 1) * 128:ci * 128, :]), [ATTD], [a_in], "ld")
            yield
            pt = yield from ps_get()
            ptb = pt.ap.bitcast(BF16)
            for k in range(KC):
                S.emit("pe", lambda e, k=k: e.transpose(ptb[:, k * 128:(k + 1) * 128], a_in.ap[:, k * 128:(k + 1) * 128], IDB), [a_in, CB], [pt], k == KC - 1)
            yield
            S.emit("act", lambda e: e.copy(out=a_k.ap, in_=ptb.rearrange("p (a b) -> p a b", a=KC)), [pt], [a_k])
            yield
            ps_put(pt)
            pp = ((yield from ps_get()), (yield from ps_get()))
            for h in range(2):
                for kc in range(KC):
                    mm(pp[h].ap, a_k.ap[:, kc, :], WNO.ap[:, kc, h * 512:(h + 1) * 512], kc == 0, kc == KC - 1, [a_k, (WNO, kc)], [pp[h]], kc == KC - 1)
                yield
            yield from epilogue(ci, pp, None, H2, H3, route_li=1)

        interleave([no_gen(ci) for ci in range(1, 33)], NB, 30)
        A.reset(m)

    def zero_fill_xg(part, nparts):
        nz = (DUMP + 1) // 1024
        for i in range(nz):
            if i % nparts == part:
                S.dma("sp", lambda e, i=i: e.dma_start(out=XG.ap[i * 1024:(i + 1) * 1024, :], in_=ZROWS.ap), [], [(XG, "sc")], "zf", 8)
        rem = DUMP + 1 - nz * 1024
        if rem and part == 0:
            S.dma("sp", lambda e: e.dma_start(out=XG.ap[nz * 1024:DUMP + 1, :], in_=ZROWS.ap[0:rem, :]), [], [(XG, "sc")], "zf", 8)

    zero_m = A.mark()
    ZR = A.alloc("zr", [D], F32)
    S.emit("pool", lambda e: e.memset(ZR.ap, 0.0), [], [ZR])
    S.dma("sp", lambda e: e.dma_start(out=YG.ap[DUMP:DUMP + 1, :], in_=ZR.ap[0:1, :]), [ZR], [(YG, "dump")], "st")
    A.reset(zero_m)

    phase_1a()
    phase_1b()
    if stop != "1b":
        alloc_epilogue()
        phase_1c()
        H2T_, pre_ = phase_moe(0, H1, H2, 1, list(range(33)), h2t=True)
        phase_na(H2T_)
        A.reset(pre_)
        phase_moe(1, H3, None, 3, list(range(1, 33)), to_out=True)

    outs = [OUT] if stop is None else [ZTd]
    if debug and stop is None:
        outs += [H1, H2, H3, ATTD]
    S.final_wait("sp", outs)
    S.check()
    with nc.Block() as block:
        S.build(block)
    es.close()
    return nc, in_names


def host_inputs(inp, b):
    f = np.float32
    x = np.asarray(inp["x"][b], f)
    h0 = np.ascontiguousarray(np.concatenate([np.asarray(inp["meta_tokens"], f), x], axis=0))
    d = {"h0": h0, "h0T": np.ascontiguousarray(h0.T)}
    return d


def shared_inputs(inp):
    f = np.float32
    par = np.zeros((128, NPAR), f)
    cw = np.asarray(inp["lru_conv_w"][0], f)
    par[:, P_CW:P_CW + 32] = cw.reshape(4, 8, 128).transpose(2, 1, 0).reshape(128, 32)
    par[:, P_CB:P_CB + 8] = np.asarray(inp["lru_conv_b"][0], f).reshape(8, 128).T
    for name, col in (("lru_ba", P_BA), ("lru_bx", P_BX), ("lru_lambda", P_LAM)):
        par[:, col:col + 16] = np.asarray(inp[name][0], f).reshape(2, 8, 128).transpose(2, 0, 1).reshape(128, 16)
    for li, col in ((0, P_BGU0), (1, P_BGU1)):
        par[:, col:col + 512] = np.asarray(inp["moe_b_gu"][li], f).reshape(NE, 16, 128).transpose(2, 0, 1).reshape(128, 512)
    par[0:16, P_MB:P_MB + 16] = np.asarray(inp["na_meta_bias"][0], f).T
    rows = np.stack([inp["ln_mix_g"][0], inp["ln_mix_b"][0], inp["ln_ffn_g"][0], inp["ln_ffn_b"][0],
                     inp["ln_mix_g"][1], inp["ln_mix_b"][1], inp["ln_ffn_g"][1], inp["ln_ffn_b"][1]]).astype(f)
    rpb = np.asarray(inp["na_rpb"][0], f)
    rp = np.zeros((16, 19, 127), f)
    rp[:, 2:17, 48:79] = rpb[:, :, ::-1]
    cstf = np.zeros((128, 224), f)
    cstf[:, 0:128] = np.eye(128, dtype=f)
    cstf[:, 128:160] = (np.arange(NE) * CAP - DUMP).astype(f)[None, :]
    cc = np.arange(64)
    cs = np.clip(cc - 8, 0, 48)
    cm = ((cc[:, None] >= cs[None, :]) & (cc[:, None] <= cs[None, :] + 15)).astype(f)
    cstf[:, 160:224] = np.concatenate([cm[::-1], cm[::-1]], axis=0)
    cstb = np.zeros((128, 512), f)
    jj = np.eye(64, dtype=f)[::-1]
    cstb[0:64, 384:448] = jj
    cstb[64:128, 448:512] = jj
    cstb[:, 0:128] = np.eye(128)
    cstb[:, 128:256] = np.triu(np.ones((128, 128)), 1)
    cstb[:, 256:384] = 1.0
    d = {"zrows": np.zeros((1024, D), ml_dtypes.bfloat16), "par": par, "rows": rows, "rb": np.asarray(inp["router_b"], f), "cstf": cstf, "cstb": cstb.astype(ml_dtypes.bfloat16),
         "lru_w_in": np.asarray(inp["lru_w_in"][0], f), "lru_wa": np.asarray(inp["lru_wa"][0], f), "lru_wx": np.asarray(inp["lru_wx"][0], f),
         "lru_w_out": np.asarray(inp["lru_w_out"][0], f), "na_w_qkv": np.asarray(inp["na_w_qkv"][0], f),
         "na_w_out": np.asarray(inp["na_w_out"][0], f), "rp": rp, "router_w": np.asarray(inp["router_w"], f),
         "moe_w_gu": np.asarray(inp["moe_w_gu"], f), "moe_w_down": np.asarray(inp["moe_w_down"], f),
         "moe_b_down": np.asarray(inp["moe_b_down"], f)}
    return d


def kernel(**inputs):
    nc, names = build_program()
    sh = shared_inputs(inputs)
    in_maps = []
    for b in range(8):
        m = dict(sh)
        m.update(host_inputs(inputs, b))
        in_maps.append({k: m[k] for k in names})
    res = run_bass_kernel_spmd(nc, in_maps, core_ids=list(range(8)))
    return np.stack([np.asarray(r["out"], np.float32) for r in res.results], axis=0)
```

```python
import contextlib
import numpy as np
import ml_dtypes
import concourse.bass as bass
import concourse.mybir as mybir
from concourse.bass_utils import run_bass_kernel_spmd

F32 = mybir.dt.float32
BF16 = mybir.dt.bfloat16
I32 = mybir.dt.int32
ALU = mybir.AluOpType
AF = mybir.ActivationFunctionType
AX = mybir.AxisListType

D = 1024
KC = 8
NMETA = 16
SEQ = 4096
L = NMETA + SEQ
NE = 32
CAP = 704
SLOTCH = [(i * 128, min(128, CAP - i * 128)) for i in range((CAP + 127) // 128)]
NSC = len(SLOTCH)
NB = 3
DUMP = NE * CAP
ALPHA = 4.0 ** 0.25
EPS = 1e-5
GROUPS = [(0, 16)] + [(16 + 512 * g, 512) for g in range(8)]
CHUNKS = [(0, 16)] + [(16 + 128 * c, 128) for c in range(32)]

P_CW, P_CB, P_BA, P_BX, P_LAM, P_BGU0, P_BGU1, P_MB, NPAR = 0, 32, 40, 56, 72, 88, 600, 1112, 1128


class Buf:
    def __init__(self, name, space, rng=None):
        self.name, self.space, self.rng, self.aliases = name, space, rng, []


class Tile:
    def __init__(self, buf, ap):
        self.buf, self.ap = buf, ap


def _key(x):
    if isinstance(x, Tile):
        return (x.buf, None)
    t, i = x
    return (t.buf, i)


class Sched:
    CE = ("pe", "act", "dve", "pool")

    def __init__(self, nc, es):
        self.nc, self.es = nc, es
        self.q = {e: [] for e in self.CE + ("sp",)}
        self.cnt = {e: 0 for e in self.CE}
        self.semh = {e: es.enter_context(nc.semaphore("c_" + e)) for e in self.CE}
        self.seen = {e: {} for e in self.q}
        self.lastw, self.rd, self.keys_of, self.chans = {}, {}, {}, {}

    def _match(self, b, i):
        ks = self.keys_of.get(b, ())
        if i is None:
            return [(b, j) for j in ks]
        return [(b, j) for j in (i, None) if j in ks]

    def _deps(self, reads, writes):
        ev = {}

        def add(e):
            if e is not None and ev.get(e[0], 0) < e[1]:
                ev[e[0]] = e[1]

        for (b, i) in reads:
            for k in self._match(b, i):
                add(self.lastw.get(k))
        for (b, i) in writes:
            ks = self._match(b, i)
            for a in b.aliases:
                ks = ks + self._match(a, None)
            for k in ks:
                add(self.lastw.get(k))
                for sk, v in self.rd.get(k, {}).items():
                    add((sk, v))
        return ev

    def _record(self, reads, writes, e):
        for (b, i) in writes:
            if i is None:
                for k in self._match(b, None):
                    self.lastw[k] = e
                    self.rd[k] = {}
            self.keys_of.setdefault(b, set()).add(i)
            self.lastw[(b, i)] = e
            self.rd[(b, i)] = {}
        for (b, i) in reads:
            self.keys_of.setdefault(b, set()).add(i)
            d = self.rd.setdefault((b, i), {})
            if d.get(e[0], 0) < e[1]:
                d[e[0]] = e[1]

    def _waits(self, eng, ev):
        w = []
        for sk, v in ev.items():
            if eng == "pe" and sk == "pe":
                continue
            if self.seen[eng].get(sk, 0) >= v:
                continue
            self.seen[eng][sk] = v
            w.append((sk, v))
        return w

    def emit(self, eng, fn, reads=(), writes=(), inc=True):
        reads = [_key(x) for x in reads]
        writes = [_key(x) for x in writes]
        assert inc or eng == "pe"
        w = self._waits(eng, self._deps(reads, writes))
        if inc:
            self.cnt[eng] += 1
            e = (eng, self.cnt[eng])
        else:
            e = (eng, self.cnt[eng] + 1)
        self._record(reads, writes, e)
        self.q[eng].append((w, fn, eng if inc else None))

    def dma(self, q, fn, reads=(), writes=(), chan="d", K=4):
        reads = [_key(x) for x in reads]
        writes = [_key(x) for x in writes]
        st = self.chans.get(chan)
        if st is None:
            st = {"i": 0, "K": K}
            for s in range(K):
                self.semh[("dma", chan, s)] = self.es.enter_context(self.nc.semaphore("d_%s%d" % (chan, s)))
            self.chans[chan] = st
        i, K = st["i"], st["K"]
        st["i"] += 1
        sk = ("dma", chan, i % K)
        ev = self._deps(reads, writes)
        if i >= K:
            ev[sk] = max(ev.get(sk, 0), 16 * (i // K))
        w = self._waits(q, ev)
        self._record(reads, writes, (sk, 16 * (i // K + 1)))
        self.q[q].append((w, fn, sk))

    def final_wait(self, q, tiles):
        ev = self._deps([_key(t) for t in tiles], [])
        self.q[q].append((self._waits(q, ev), None, None))


    def check(self):
        sem = {}
        pos = {e: 0 for e in self.q}
        progress = True
        while progress:
            progress = False
            for e, lst in self.q.items():
                while pos[e] < len(lst):
                    waits, fn, inc = lst[pos[e]]
                    if any(sem.get(sk, 0) < v for sk, v in waits):
                        break
                    if fn is not None and inc is not None:
                        sem[inc] = sem.get(inc, 0) + (16 if isinstance(inc, tuple) else 1)
                    pos[e] += 1
                    progress = True
        stuck = {e: (pos[e], len(lst)) for e, lst in self.q.items() if pos[e] < len(lst)}
        if stuck:
            msg = []
            for e, (p, n) in stuck.items():
                waits = self.q[e][p][0]
                msg.append("%s stuck at %d/%d waiting %s" % (e, p, n, [(sk, v, sem.get(sk, 0)) for sk, v in waits if sem.get(sk, 0) < v]))
            raise RuntimeError("sync deadlock: " + "; ".join(msg))

    def build(self, block):
        for eng, deco in (("pe", block.tensor), ("act", block.scalar), ("dve", block.vector),
                          ("pool", block.gpsimd), ("sp", block.sync)):
            def body(e, lst=self.q[eng]):
                for waits, fn, inc in lst:
                    for sk, v in waits:
                        e.wait_ge(self.semh[sk], v)
                    if fn is None:
                        continue
                    ins = fn(e)
                    if inc is None:
                        continue
                    if isinstance(inc, tuple):
                        ins.then_inc(self.semh[inc], 16)
                    else:
                        ins.then_inc(self.semh[inc], 1)
            deco(body)


class Arena:
    def __init__(self, nc, es, nbytes):
        self.cap = nbytes
        self.t = es.enter_context(nc.sbuf_tensor("arena", [128, nbytes // 2], BF16))
        self.pos = 0
        self.bufs = []

    def alloc(self, name, free, dtype, parts=128):
        esz = 2 if dtype == BF16 else 4
        n = int(np.prod(free))
        nb = n * esz
        nba = (nb + 63) // 64 * 64
        off = self.pos
        self.pos += nba
        assert self.pos <= self.cap, ("SBUF arena overflow", name, self.pos)
        ap = self.t[0:parts, off // 2: off // 2 + nb // 2]
        if dtype != BF16:
            ap = ap.bitcast(dtype)
        if len(free) == 2:
            ap = ap.rearrange("p (a b) -> p a b", a=free[0])
        elif len(free) == 3:
            ap = ap.rearrange("p (a b c) -> p a b c", a=free[0], b=free[1])
        b = Buf(name, "sb", (off, off + nba))
        for o in self.bufs:
            if o.rng[0] < b.rng[1] and b.rng[0] < o.rng[1]:
                b.aliases.append(o)
                o.aliases.append(b)
        self.bufs.append(b)
        return Tile(b, ap)

    def mark(self):
        return self.pos

    def reset(self, m):
        self.pos = m


def build_program(debug=False, stop=None):
    nc = bass.Bass("TRN2", target_bir_lowering=False)
    es = contextlib.ExitStack()

    in_names = []

    def din(name, shape, dt=F32):
        in_names.append(name)
        return Tile(Buf(name, "dram"), nc.dram_tensor(name, list(shape), dt, kind="ExternalInput").ap())

    def dscr(name, shape, dt=F32, out=False):
        kind = "ExternalOutput" if (out or debug) else "Internal"
        return Tile(Buf(name, "dram"), nc.dram_tensor(name, list(shape), dt, kind=kind).ap())

    H0 = din("h0", [L, D])
    H0T = din("h0T", [D, L])
    PARd = din("par", [128, NPAR])
    ROWSd = din("rows", [8, D])
    RBd = din("rb", [2, NE])
    CSTF = din("cstf", [128, 224])
    CSTB = din("cstb", [128, 512], BF16)
    ZROWS = din("zrows", [1024, D], BF16)
    W_IN = din("lru_w_in", [D, 2 * D])
    W_A = din("lru_wa", [2, 4, 256, 256])
    W_X = din("lru_wx", [2, 4, 256, 256])
    W_LO = din("lru_w_out", [D, D])
    W_QKV = din("na_w_qkv", [D, 3 * D])
    W_NO = din("na_w_out", [D, D])
    RPd = din("rp", [16, 19, 127])
    W_R = din("router_w", [2, D, NE])
    W_GU = din("moe_w_gu", [2, NE, D, 2 * D])
    W_DN = din("moe_w_down", [2, NE, D, D])
    B_DN = din("moe_b_down", [2, NE, D])
    OUT = dscr("out", [SEQ, D], out=True)

    XRd = dscr("xr_s", [8, 128, L])
    GYd = dscr("gy_s", [8, 128, L], BF16)
    ZTd = dscr("zt_s", [8, 128, L], BF16)
    H1 = dscr("h1_s", [L, D])
    H2 = dscr("h2_s", [L, D])
    H3 = dscr("h3_s", [L, D])
    XG = dscr("xg_s", [DUMP + 1, D], BF16)
    YG = dscr("yg_s", [DUMP + 1, D])
    ATTD = dscr("att_s", [SEQ, D], BF16)

    S = Sched(nc, es)
    A = Arena(nc, es, 207 * 1024)
    PS = []
    for i in range(8):
        t = es.enter_context(nc.psum_tensor("ps%d" % i, [128, 512], F32))
        PS.append(Tile(Buf("ps%d" % i, "ps"), t[:]))
    psi = [0]

    def next_ps():
        p = PS[psi[0] % 8]
        psi[0] += 1
        return p

    ps_free = list(range(8))

    def ps_get():
        while not ps_free:
            yield
        return PS[ps_free.pop(0)]

    def ps_put(*ps):
        for p in ps:
            ps_free.append(PS.index(p))

    def mm(out, lhsT, rhs, start, stop, reads, writes, inc):
        S.emit("pe", lambda e: e.matmul(out, lhsT, rhs, start=start, stop=stop), reads, writes, inc)

    PAR = A.alloc("par", [NPAR], F32)
    CF = A.alloc("cstf", [224], F32)
    CB = A.alloc("cstb", [512], BF16)
    IDF = CF.ap[:, 0:128]
    EOFFM = CF.ap[:, 128:160]
    CMASK = CF.ap[:, 160:224]
    IDB = CB.ap[:, 0:128]
    UTRI = CB.ap[:, 128:256]
    ONESB = CB.ap[:, 256:384]
    J2 = CB.ap[:, 384:512]
    SP = A.alloc("sp", [32], F32)
    BL1 = A.alloc("bl1", [2, NE, 8], F32)
    HBIAS = A.alloc("hbias", [32], F32)
    SPH = A.alloc("sph", [16], F32)
    GALL = A.alloc("gall", [33, NE], F32)
    GK = A.alloc("gk", [33, 4], F32)
    DI = A.alloc("di", [33, 4], I32)
    CNT = A.alloc("cnt", [NE], F32)
    S.dma("sp", lambda e: e.dma_start(out=PAR.ap, in_=PARd.ap), [], [PAR], "ld")
    S.dma("sp", lambda e: e.dma_start(out=CF.ap, in_=CSTF.ap), [], [CF], "ld")
    S.dma("sp", lambda e: e.dma_start(out=CB.ap, in_=CSTB.ap), [], [CB], "ld")
    S.emit("act", lambda e: e.activation(out=SP.ap[:, 0:16], in_=PAR.ap[:, P_LAM:P_LAM + 16], func=AF.Exp, scale=-1.0), [PAR], [SP])
    S.emit("act", lambda e: e.activation(out=SP.ap[:, 0:16], in_=SP.ap[:, 0:16], func=AF.Ln, bias=1.0, scale=1.0), [SP], [SP])
    S.emit("dve", lambda e: e.tensor_scalar(out=SP.ap[:, 16:32], in0=SP.ap[:, 0:16], scalar1=-16.0, scalar2=None, op0=ALU.mult), [SP], [SP])
    S.emit("dve", lambda e: e.tensor_scalar(out=SP.ap[:, 0:16], in0=SP.ap[:, 0:16], scalar1=-8.0, scalar2=None, op0=ALU.mult), [SP], [SP])
    S.emit("dve", lambda e: e.tensor_scalar(out=SPH.ap, in0=SP.ap[:, 0:16], scalar1=0.5, scalar2=None, op0=ALU.mult), [SP], [SPH])
    S.emit("dve", lambda e: e.tensor_scalar(out=HBIAS.ap, in0=PAR.ap[:, P_BA:P_BA + 32], scalar1=0.5, scalar2=None, op0=ALU.mult), [PAR], [HBIAS])
    for li in range(2):
        c0 = (P_BGU0, P_BGU1)[li]
        src = PAR.ap[:, c0:c0 + 512].rearrange("p (e f) -> p e f", e=NE)[:, :, 8:16]
        S.emit("dve", lambda e, src=src, li=li: e.tensor_scalar(out=BL1.ap[:, li], in0=src, scalar1=1.0, scalar2=None, op0=ALU.add), [PAR], [BL1])
    base_mark = A.mark()

    def phase_1a():
        m = A.mark()
        h0T = A.alloc("h0T", [KC, L], BF16)
        win = A.alloc("win", [KC, 2 * D], BF16)
        ms = A.mark()
        stg = [A.alloc("stg%d" % i, [L], F32) for i in range(2)]
        A.reset(ms)
        stb = [A.alloc("stb%d" % i, [L], BF16) for i in range(2)]
        for kc in range(KC):
            for (a, b) in ((0, 2048), (2048, 4096), (4096, L)):
                S.dma("pool", lambda e, kc=kc, a=a, b=b: e.dma_start(out=h0T.ap[:, kc, a:b], in_=H0T.ap[kc * 128:(kc + 1) * 128, a:b]),
                      [], [(h0T, kc)], "ldc", 8)
            S.dma("pool", lambda e, kc=kc: e.dma_start(out=win.ap[:, kc, :], in_=W_IN.ap[kc * 128:(kc + 1) * 128, :]),
                  [], [(win, kc)], "ldc", 8)
        for fc in range(16):
            st = (stg if fc < 8 else stb)[fc % 2]
            for gi, (t0, n) in enumerate(GROUPS):
                ps = next_ps()
                for kc in range(KC):
                    mm(ps.ap[:, 0:n], win.ap[:, kc, fc * 128:(fc + 1) * 128], h0T.ap[:, kc, t0:t0 + n], kc == 0, kc == KC - 1,
                       [(win, kc), (h0T, kc)], [ps], kc == KC - 1)
                if fc < 8:
                    if gi % 2 == 0:
                        S.emit("act", lambda e, st=st, ps=ps, t0=t0, n=n: e.copy(out=st.ap[:, t0:t0 + n], in_=ps.ap[:, 0:n]), [ps], [(st, gi)])
                    else:
                        S.emit("dve", lambda e, st=st, ps=ps, t0=t0, n=n: e.tensor_copy(out=st.ap[:, t0:t0 + n], in_=ps.ap[:, 0:n]), [ps], [(st, gi)])
                else:
                    S.emit("act", lambda e, st=st, ps=ps, t0=t0, n=n: e.activation(out=st.ap[:, t0:t0 + n], in_=ps.ap[:, 0:n], func=AF.Gelu_apprx_tanh),
                           [ps], [(st, gi)])
            if fc < 8:
                S.dma("sp", lambda e, st=st, fc=fc: e.dma_start(out=XRd.ap[fc], in_=st.ap), [st], [(XRd, fc)], "st")
            else:
                S.dma("sp", lambda e, st=st, fc=fc: e.dma_start(out=GYd.ap[fc - 8], in_=st.ap), [st], [(GYd, fc - 8)], "st")
        A.reset(m)

    def phase_1b():
        m = A.mark()
        WA = A.alloc("wa", [16, 256], BF16)
        WX = A.alloc("wx", [16, 256], BF16)
        S.dma("pool", lambda e: e.dma_start(out=WA.ap, in_=W_A.ap.rearrange("d n (i p) j -> p (d n i) j", p=128)), [], [WA], "ldc", 8)
        S.dma("pool", lambda e: e.dma_start(out=WX.ap, in_=W_X.ap.rearrange("d n (i p) j -> p (d n i) j", p=128)), [], [WX], "ldc", 8)
        XC = A.alloc("xc", [2, L], F32)
        XCB = A.alloc("xcb", [2, L], BF16)
        HFB = [[A.alloc("h%d_%d" % (d, i), [L], F32) for d in range(2)] for i in range(2)]
        GYt = A.alloc("gyt", [L], BF16)
        Zt = A.alloc("zt", [L], BF16)
        HB0 = 2048
        HLMAX = L - HB0
        m2 = A.mark()
        XRP = A.alloc("xrp", [2, L + 4], F32)
        A.reset(m2)
        GA = [A.alloc("ga%d" % d, [HLMAX], F32) for d in range(2)]
        GX = [A.alloc("gx%d" % d, [HLMAX], F32) for d in range(2)]
        MMt = [A.alloc("mm%d" % d, [HLMAX], F32) for d in range(2)]
        HGROUPS = [[(512 * g, 512) for g in range(4)], [(512 * g, 512) for g in range(4)] + [(2048, L - HB0 - 2048)]]
        done = {}

        def chain(nb, jc, d):
            c = 2 * nb + jc
            H = HFB[jc][d]
            ga, gx, mmt = GA[d], GX[d], MMt[d]
            shcol = SPH.ap[:, d * 8 + c:d * 8 + c + 1]
            for hi, half in enumerate((0, 1) if d == 0 else (1, 0)):
                h0 = half * HB0
                hl = HB0 if half == 0 else L - HB0
                for gi, (g0, n) in enumerate(HGROUPS[half]):
                    t0 = h0 + g0
                    for (Wt, Gt, pcol) in ((WA, ga, 0), (WX, gx, 16)):
                        ps = yield from ps_get()
                        for ic in range(2):
                            mm(ps.ap[:, 0:n], Wt.ap[:, d * 8 + nb * 2 + ic, jc * 128:(jc + 1) * 128], XCB.ap[:, ic, t0:t0 + n], ic == 0, ic == 1,
                               [Wt, (XCB, ic)], [ps], ic == 1)
                        bcol = HBIAS.ap[:, pcol + d * 8 + c:pcol + d * 8 + c + 1]
                        S.emit("act", lambda e, Gt=Gt, ps=ps, g0=g0, n=n, bcol=bcol: e.activation(out=Gt.ap[:, g0:g0 + n], in_=ps.ap[:, 0:n], func=AF.Tanh,
                                                                                                 bias=bcol, scale=0.5), [ps, HBIAS], [(Gt, gi)])
                        ps_put(ps)
                        yield
                S.emit("act", lambda e, hl=hl: e.activation(out=ga.ap[:, 0:hl], in_=ga.ap[:, 0:hl], func=AF.Exp, scale=shcol, bias=shcol), [ga, SPH], [ga])
                for _ in range(3):
                    yield
                S.emit("dve", lambda e, hl=hl: e.tensor_tensor(out=mmt.ap[:, 0:hl], in0=ga.ap[:, 0:hl], in1=ga.ap[:, 0:hl], op=ALU.mult), [ga], [mmt])
                for _ in range(3):
                    yield
                S.emit("act", lambda e, hl=hl: e.activation(out=mmt.ap[:, 0:hl], in_=mmt.ap[:, 0:hl], func=AF.Sqrt, bias=0.25, scale=-0.25), [mmt], [mmt])
                yield
                if hi == 0:
                    sc = 0 if d == 0 else hl - 1
                    S.emit("pool", lambda e, sc=sc: e.memset(mmt.ap[:, sc:sc + 1], 0.5), [mmt], [mmt])
                    yield
                S.emit("dve", lambda e, h0=h0, hl=hl: e.scalar_tensor_tensor(out=gx.ap[:, 0:hl], in0=gx.ap[:, 0:hl], scalar=1.0, in1=XC.ap[:, jc, h0:h0 + hl],
                                                                          op0=ALU.add, op1=ALU.mult), [gx, (XC, jc)], [gx])
                yield
                S.emit("pool", lambda e, hl=hl: e.tensor_tensor(out=gx.ap[:, 0:hl], in0=gx.ap[:, 0:hl], in1=mmt.ap[:, 0:hl], op=ALU.mult), [gx, mmt], [gx])
                for _ in range(5):
                    yield
                if hi == 1:
                    fi = 0 if d == 0 else hl - 1
                    prev = H.ap[:, HB0 - 1:HB0] if d == 0 else H.ap[:, HB0:HB0 + 1]
                    S.emit("dve", lambda e, fi=fi, prev=prev: e.scalar_tensor_tensor(out=gx.ap[:, fi:fi + 1], in0=ga.ap[:, fi:fi + 1], scalar=prev, in1=gx.ap[:, fi:fi + 1],
                                                                                  op0=ALU.mult, op1=ALU.add), [ga, gx, (H, 1 - half)], [gx])
                    yield
                if d == 0:
                    S.emit("dve", lambda e, h0=h0, hl=hl: e.tensor_tensor_scan(out=H.ap[:, h0:h0 + hl], data0=ga.ap[:, 0:hl], data1=gx.ap[:, 0:hl], initial=0.0,
                                                                            op0=ALU.mult, op1=ALU.add), [ga, gx], [(H, half)])
                else:
                    S.emit("dve", lambda e, h0=h0, hl=hl: e.tensor_tensor_scan(out=H.ap[:, h0:h0 + hl][:, ::-1], data0=ga.ap[:, 0:hl][:, ::-1], data1=gx.ap[:, 0:hl][:, ::-1],
                                                                            initial=0.0, op0=ALU.mult, op1=ALU.add), [ga, gx], [(H, half)])
                yield
            done[(nb, jc)] = done.get((nb, jc), 0) + 1

        def tail(nb, jc):
            c = 2 * nb + jc
            while done.get((nb, jc), 0) < 2:
                yield
            Hf, Hb = HFB[jc]
            S.dma("sp", lambda e: e.dma_start(out=GYt.ap, in_=GYd.ap[c]), [(GYd, c)], [GYt], "ld")
            yield
            S.emit("pool", lambda e: e.tensor_tensor(out=Hf.ap, in0=Hf.ap, in1=Hb.ap, op=ALU.add), [Hf, Hb], [Hf])
            for _ in range(16):
                yield
            S.emit("dve", lambda e: e.tensor_tensor(out=Zt.ap, in0=Hf.ap, in1=GYt.ap, op=ALU.mult), [Hf, GYt], [Zt])
            yield
            S.dma("sp", lambda e: e.dma_start(out=ZTd.ap[c], in_=Zt.ap), [Zt], [(ZTd, c)], "st")
            yield

        for nb in range(4):
            S.emit("pool", lambda e: e.memset(XRP.ap[:, :, 0:2], 0.0), [], [XRP])
            S.emit("pool", lambda e: e.memset(XRP.ap[:, :, L + 2:L + 4], 0.0), [], [XRP])
            for jc in range(2):
                c = 2 * nb + jc
                S.dma("sp", lambda e, jc=jc, c=c: e.dma_start(out=XRP.ap[:, jc, 2:L + 2], in_=XRd.ap[c]), [(XRd, c)], [(XRP, jc)], "ld")
                cw = lambda j, c=c: PAR.ap[:, P_CW + c * 4 + j:P_CW + c * 4 + j + 1]
                S.emit("dve", lambda e, jc=jc, c=c, cw=cw: e.tensor_scalar(out=XC.ap[:, jc, :], in0=XRP.ap[:, jc, 0:L], scalar1=cw(0),
                                                                          scalar2=PAR.ap[:, P_CB + c:P_CB + c + 1], op0=ALU.mult, op1=ALU.add),
                       [(XRP, jc), PAR], [(XC, jc)])
                for j in range(1, 4):
                    S.emit("dve", lambda e, jc=jc, j=j, cw=cw: e.scalar_tensor_tensor(out=XC.ap[:, jc, :], in0=XRP.ap[:, jc, j:j + L], scalar=cw(j),
                                                                                     in1=XC.ap[:, jc, :], op0=ALU.mult, op1=ALU.add),
                           [(XRP, jc), (XC, jc), PAR], [(XC, jc)])
                S.emit("act", lambda e, jc=jc: e.copy(out=XCB.ap[:, jc, :], in_=XC.ap[:, jc, :]), [(XC, jc)], [(XCB, jc)])
            zero_fill_xg(nb, 4)
            gens = []
            for jc in range(2):
                gens += [chain(nb, jc, 0), chain(nb, jc, 1), tail(nb, jc)]
            interleave(gens, 3, 6)
        A.reset(m)

    EPT = {}
    regs = {}

    def bcreg(e):
        if "bc" not in regs:
            regs["bc"] = e.to_reg(DUMP)
        return regs["bc"]

    def alloc_epilogue():
        LNG = A.alloc("lng", [D], F32)
        LNB = A.alloc("lnb", [D], F32)
        RBT = A.alloc("rbt", [NE], F32)
        RW = A.alloc("rw", [KC, NE], F32)
        Rt = [A.alloc("r%d" % i, [D], F32) for i in range(NB)]
        Yt = [A.alloc("y%d" % i, [D], F32) for i in range(NB)]
        XBt = [A.alloc("xb%d" % i, [D], BF16) for i in range(NB)]
        YTt = [A.alloc("ytt%d" % i, [KC, 128], F32) for i in range(NB)]
        SMALL = [A.alloc("sm%d" % i, [232 + 256], F32) for i in range(NB)]
        MSKB = [A.alloc("mskb%d" % i, [NE], BF16) for i in range(NB)]
        EPT["end"] = A.mark()
        EPT.update(LNG=LNG, LNB=LNB, RBT=RBT, RW=RW, Rt=Rt, Yt=Yt, XBt=XBt, YTt=YTt, SMALL=SMALL, MSKB=MSKB)

    ep_mark = A.mark()
    epi = [0]

    def load_ln_params(ln_idx, li=None):
        LNG, LNB, RBT, RW = EPT["LNG"], EPT["LNB"], EPT["RBT"], EPT["RW"]
        S.dma("sp", lambda e: e.dma_start(out=LNG.ap, in_=ROWSd.ap[2 * ln_idx:2 * ln_idx + 1, :].to_broadcast([128, D])), [], [LNG], "ld")
        S.dma("sp", lambda e: e.dma_start(out=LNB.ap, in_=ROWSd.ap[2 * ln_idx + 1:2 * ln_idx + 2, :].to_broadcast([128, D])), [], [LNB], "ld")
        if li is not None:
            S.dma("sp", lambda e: e.dma_start(out=RBT.ap, in_=RBd.ap[li:li + 1, :].to_broadcast([128, NE])), [], [RBT], "ld")
            S.dma("sp", lambda e: e.dma_start(out=RW.ap, in_=W_R.ap[li].rearrange("(k p) e -> p k e", p=128)), [], [RW], "ld")
            S.emit("pool", lambda e: e.memset(CNT.ap, 0.0), [], [CNT])

    def epilogue(ci, ps_pair, extra, hprev, hnext, route_li=None, h2t=None, out_rows=None):
        LNG, LNB, RBT, RW, Rt, Yt, XBt, YTt, SMALL, MSKB = (EPT[x] for x in ("LNG", "LNB", "RBT", "RW", "Rt", "Yt", "XBt", "YTt", "SMALL", "MSKB"))
        t0, n = CHUNKS[ci]
        k = epi[0] % NB
        epi[0] += 1
        R, Y, XB, SM = Rt[k], Yt[k], XBt[k], SMALL[k]
        YTt, MSKB = YTt[k], MSKB[k]
        ST = SM.ap[:, 0:12].rearrange("p (a b) -> p a b", a=2)
        MV = SM.ap[:, 12:14]
        RS = SM.ap[:, 14:15]
        NMX = SM.ap[:, 15:16]
        o = 16
        LG = SM.ap[:, o:o + 32]
        MSK = SM.ap[:, o + 32:o + 64]
        EX = SM.ap[:, o + 64:o + 96]
        POS = SM.ap[:, o + 96:o + 128]
        V1 = SM.ap[:, o + 128:o + 160]
        OH = SM.ap[:, o + 160:o + 192]
        MX = SM.ap[:, o + 192:o + 200]
        SS = SM.ap[:, o + 200:o + 201]
        DK = SM.ap[:, o + 208:o + 212]
        JNK = SM.ap[:, 232:488]
        S.dma("sp", lambda e: e.dma_start(out=R.ap[0:n], in_=hprev.ap[t0:t0 + n, :]), [(hprev, ci)], [R], "ld")
        yield
        for h in range(2):
            S.emit("dve", lambda e, h=h: e.scalar_tensor_tensor(out=R.ap[0:n, h * 512:(h + 1) * 512], in0=R.ap[0:n, h * 512:(h + 1) * 512], scalar=ALPHA,
                                                               in1=ps_pair[h].ap[0:n, :], op0=ALU.mult, op1=ALU.add), [R, ps_pair[h]], [R])
            yield
        ps_put(*ps_pair)
        if extra is not None:
            for kk in range(4):
                S.emit("dve", lambda e, kk=kk: e.scalar_tensor_tensor(out=R.ap[0:n], in0=extra.ap[0:n, kk, :], scalar=GK.ap[0:n, ci, kk:kk + 1], in1=R.ap[0:n],
                                                                     op0=ALU.mult, op1=ALU.add), [R, (extra, kk), (GK, ci)], [R])
                yield
        for h in range(2):
            S.emit("dve", lambda e, h=h: e.bn_stats(out=ST[0:n, h, :], in_=R.ap[0:n, h * 512:(h + 1) * 512]), [R], [(SM, "st%d" % h)])
            yield
        S.emit("dve", lambda e: e.bn_aggr(out=MV[0:n], in_=SM.ap[0:n, 0:12]), [(SM, "st0"), (SM, "st1")], [(SM, "mv")])
        yield
        S.emit("act", lambda e: e.activation(out=RS[0:n], in_=MV[0:n, 1:2], func=AF.Ln, bias=EPS, scale=1.0), [(SM, "mv")], [(SM, "rs")])
        yield
        S.emit("act", lambda e: e.activation(out=RS[0:n], in_=RS[0:n], func=AF.Exp, scale=-0.5), [(SM, "rs")], [(SM, "rs")])
        yield
        S.emit("dve", lambda e: e.scalar_tensor_tensor(out=Y.ap[0:n], in0=R.ap[0:n], scalar=MV[0:n, 0:1], in1=LNG.ap[0:n], op0=ALU.subtract, op1=ALU.mult),
               [R, (SM, "mv"), LNG], [Y])
        yield
        S.emit("dve", lambda e: e.scalar_tensor_tensor(out=Y.ap[0:n], in0=Y.ap[0:n], scalar=RS[0:n], in1=LNB.ap[0:n], op0=ALU.mult, op1=ALU.add),
               [Y, (SM, "rs"), LNB], [Y])
        yield
        if hnext is not None:
            S.dma("pool", lambda e: e.dma_start(out=hnext.ap[t0:t0 + n, :], in_=Y.ap[0:n]), [Y], [(hnext, ci)], "stp")
            yield
        if out_rows is not None:
            S.dma("pool", lambda e: e.dma_start(out=OUT.ap[out_rows:out_rows + n, :], in_=Y.ap[0:n]), [Y], [(OUT, ci)], "stp")
            yield
        if route_li is None and h2t is None:
            return
        pt = ((yield from ps_get()), (yield from ps_get()))
        for kk in range(KC):
            p = pt[kk // 4]
            S.emit("pe", lambda e, p=p, kk=kk: e.transpose(p.ap[:, (kk % 4) * 128:(kk % 4) * 128 + n], Y.ap[0:n, kk * 128:(kk + 1) * 128], IDF[0:n, 0:n]),
                   [Y, CF], [p], kk % 4 == 3)
            yield
        if h2t is not None:
            for hh in range(2):
                S.emit("act", lambda e, hh=hh: e.copy(out=h2t.ap[:, hh * 4:(hh + 1) * 4, t0:t0 + n],
                                                       in_=pt[hh].ap.rearrange("p (a b) -> p a b", a=4)[:, :, 0:n]), [pt[hh]], [(h2t, ci)])
                yield
        if route_li is None:
            ps_put(*pt)
            return
        li = route_li
        for hh in range(2):
            S.emit("act", lambda e, hh=hh: e.copy(out=YTt.ap[:, hh * 4:(hh + 1) * 4, 0:n], in_=pt[hh].ap.rearrange("p (a b) -> p a b", a=4)[:, :, 0:n]),
                   [pt[hh]], [(YTt, hh)])
            yield
        ps_put(*pt)
        S.emit("act", lambda e: e.copy(out=XB.ap[0:n], in_=Y.ap[0:n]), [Y], [XB])
        yield
        pl = yield from ps_get()
        for kk in range(KC):
            mm(pl.ap[0:n, 0:NE], YTt.ap[:, kk, 0:n], RW.ap[:, kk, :], kk == 0, kk == KC - 1, [YTt, RW], [pl], kk == KC - 1)
            yield
        S.emit("dve", lambda e: e.tensor_tensor(out=LG[0:n], in0=pl.ap[0:n, 0:NE], in1=RBT.ap[0:n], op=ALU.add), [pl, RBT], [(SM, "lg")])
        yield
        ps_put(pl)
        S.emit("dve", lambda e: e.max(out=MX[0:n], in_=LG[0:n]), [(SM, "lg")], [(SM, "mx")])
        yield
        S.emit("dve", lambda e: e.tensor_scalar(out=MSK[0:n], in0=LG[0:n], scalar1=MX[0:n, 3:4], scalar2=None, op0=ALU.is_ge), [(SM, "lg"), (SM, "mx")], [(SM, "msk")])
        yield
        S.emit("dve", lambda e: e.tensor_scalar(out=NMX[0:n], in0=MX[0:n, 0:1], scalar1=-1.0, scalar2=None, op0=ALU.mult), [(SM, "mx")], [(SM, "nmx")])
        yield
        S.emit("act", lambda e: e.activation(out=EX[0:n], in_=LG[0:n], func=AF.Exp, bias=NMX[0:n], scale=1.0), [(SM, "lg"), (SM, "nmx")], [(SM, "ex")])
        yield
        S.emit("dve", lambda e: e.tensor_tensor(out=EX[0:n], in0=EX[0:n], in1=MSK[0:n], op=ALU.mult), [(SM, "ex"), (SM, "msk")], [(SM, "ex")])
        yield
        S.emit("dve", lambda e: e.tensor_reduce(out=SS[0:n], in_=EX[0:n], axis=AX.X, op=ALU.add), [(SM, "ex")], [(SM, "ss")])
        yield
        S.emit("dve", lambda e: e.reciprocal(out=SS[0:n], in_=SS[0:n]), [(SM, "ss")], [(SM, "ss")])
        yield
        S.emit("dve", lambda e: e.tensor_scalar(out=GALL.ap[0:n, ci, :], in0=EX[0:n], scalar1=SS[0:n], scalar2=None, op0=ALU.mult),
               [(SM, "ex"), (SM, "ss")], [(GALL, ci)])
        yield
        S.emit("act", lambda e: e.copy(out=MSKB.ap[0:n], in_=MSK[0:n]), [(SM, "msk")], [MSKB])
        yield
        pp = yield from ps_get()
        mm(pp.ap[0:n, 0:NE], UTRI[0:n, 0:n], MSKB.ap[0:n], True, True, [MSKB, CB], [pp], False)
        yield
        mm(pp.ap[:, NE:2 * NE], ONESB[0:n, :], MSKB.ap[0:n], True, True, [MSKB, CB], [pp], True)
        yield
        S.emit("dve", lambda e: e.tensor_tensor(out=POS[0:n], in0=pp.ap[0:n, 0:NE], in1=CNT.ap[0:n], op=ALU.add), [pp, CNT], [(SM, "pos")])
        S.emit("dve", lambda e: e.tensor_tensor(out=CNT.ap, in0=pp.ap[:, NE:2 * NE], in1=CNT.ap, op=ALU.add), [pp, CNT, (SM, "pos")], [CNT])
        yield
        ps_put(pp)
        S.emit("dve", lambda e: e.tensor_tensor(out=V1[0:n], in0=POS[0:n], in1=EOFFM[0:n], op=ALU.add), [(SM, "pos"), CF], [(SM, "v1")])
        yield
        S.emit("dve", lambda e: e.tensor_scalar(out=POS[0:n], in0=POS[0:n], scalar1=float(CAP), scalar2=None, op0=ALU.is_lt), [(SM, "pos"), (SM, "v1")], [(SM, "pos")])
        yield
        S.emit("dve", lambda e: e.tensor_tensor(out=V1[0:n], in0=V1[0:n], in1=POS[0:n], op=ALU.mult), [(SM, "pos"), (SM, "v1")], [(SM, "v1")])
        yield
        for kk in range(4):
            j0 = JNK[:, 64 * kk:64 * kk + 32]
            j1 = JNK[:, 64 * kk + 32:64 * kk + 64]
            S.emit("dve", lambda e, kk=kk, j0=j0: e.scalar_tensor_tensor(out=j0[0:n], in0=LG[0:n], scalar=MX[0:n, kk:kk + 1], in1=V1[0:n], op0=ALU.is_equal, op1=ALU.mult,
                                                                      accum_out=DK[0:n, kk:kk + 1]), [(SM, "lg"), (SM, "mx"), (SM, "v1")], [(SM, "dk%d" % kk)])
            yield
            S.emit("dve", lambda e, kk=kk, j1=j1: e.scalar_tensor_tensor(out=j1[0:n], in0=LG[0:n], scalar=MX[0:n, kk:kk + 1], in1=GALL.ap[0:n, ci, :], op0=ALU.is_equal,
                                                                      op1=ALU.mult, accum_out=GK.ap[0:n, ci, kk:kk + 1]), [(SM, "lg"), (SM, "mx"), (GALL, ci)], [(GK, ci)])
            yield
        S.emit("dve", lambda e: e.tensor_scalar(out=DI.ap[0:n, ci, :], in0=DK[0:n], scalar1=float(DUMP), scalar2=None, op0=ALU.add), [(SM, "dk0"), (SM, "dk1"), (SM, "dk2"), (SM, "dk3")], [(DI, ci)])
        yield
        for kk in range(4):
            S.dma("pool", lambda e, kk=kk: e.indirect_dma_start(out=XG.ap, out_offset=bass.IndirectOffsetOnAxis(ap=DI.ap[0:n, ci, kk:kk + 1], axis=0),
                                                                in_=XB.ap[0:n], in_offset=None, bounds_check=bcreg(e), oob_is_err=False),
                  [XB, (DI, ci)], [(XG, "sc")], "ind", 8)
            yield


    def interleave(gens, width, stagger=0):
        gens = list(gens)
        active = []
        nxt = 0
        since = stagger
        while active or nxt < len(gens):
            if len(active) < width and nxt < len(gens) and (since >= stagger or not active):
                active.append(gens[nxt])
                nxt += 1
                since = 0
            since += 1
            for g in list(active):
                try:
                    next(g)
                except StopIteration:
                    active.remove(g)

    def phase_1c():
        m = A.mark()
        wo = A.alloc("wo", [KC, D], BF16)
        zg = [A.alloc("zg%d" % i, [KC, 512], BF16) for i in range(3)]
        for kc in range(KC):
            S.dma("pool", lambda e, kc=kc: e.dma_start(out=wo.ap[:, kc, :], in_=W_LO.ap[kc * 128:(kc + 1) * 128, :]), [], [(wo, kc)], "ldc", 8)
        load_ln_params(0, 0)
        def chunk_gen(ci, z, s_, n):
            pp = ((yield from ps_get()), (yield from ps_get()))
            for h in range(2):
                for kc in range(KC):
                    mm(pp[h].ap[0:n, :], z.ap[:, kc, s_ * 128:s_ * 128 + n], wo.ap[:, kc, h * 512:(h + 1) * 512], kc == 0, kc == KC - 1,
                       [z, (wo, kc)], [pp[h]], kc == KC - 1)
                yield
            yield from epilogue(ci, pp, None, H0, H1, route_li=0)

        gens = []
        ci = 0
        for gi, (g0, gn) in enumerate(GROUPS):
            z = zg[gi % 3]
            first = True
            for s_ in range(max(1, gn // 128)):
                def g_(ci=ci, z=z, s_=s_, n=min(128, gn), first=first, g0=g0, gn=gn):
                    if first:
                        S.dma("sp", lambda e: e.dma_start(out=z.ap[:, :, 0:gn], in_=ZTd.ap[:, :, g0:g0 + gn].rearrange("k p t -> p k t")), [ZTd], [z], "ld")
                    yield from chunk_gen(ci, z, s_, n)
                gens.append(g_())
                first = False
                ci += 1
        interleave(gens, NB, 30)
        A.reset(m)


    def phase_moe(li, hprev, hnext, ln_idx, chunk_ids, h2t=None, to_out=False):
        m = A.mark()
        A.reset(base_mark)
        WG = [A.alloc("wg%d" % i, [KC, 2 * D], BF16) for i in range(2)]
        WD = [A.alloc("wd%d" % i, [KC, D], BF16) for i in range(2)]
        XT = [A.alloc("xt%d" % i, [KC, CAP], BF16) for i in range(2)]
        ACTT = [A.alloc("actt%d" % i, [KC, CAP], BF16) for i in range(2)]
        XS = A.alloc("xs", [NSC, D], BF16)
        YS = [A.alloc("ys%d" % i, [D], F32) for i in range(2)]
        HN = CAP // 2
        NT = 3
        TT = [[A.alloc("t%d_%d" % (j, i), [HN], F32) for j in range(3)] for i in range(NT)]
        BG17 = A.alloc("bg17", [NE, 8], F32)
        bgu0 = (P_BGU0, P_BGU1)[li]
        SILU_C = 11.914 / (1.0 + float(np.exp(-11.914)))
        S.emit("dve", lambda en: en.tensor_scalar(out=BG17.ap, in0=PAR.ap[:, bgu0:bgu0 + 512].rearrange("p (e f) -> p e f", e=NE)[:, :, 0:8],
                                                  scalar1=1.702, scalar2=None, op0=ALU.mult), [PAR], [BG17])
        GUB = [PS[0:2], PS[2:4]]
        OTB = PS[4:8]
        oti = [0]

        def next_ot():
            p = OTB[oti[0] % 4]
            oti[0] += 1
            return p

        def load_wg(e):
            sl = e % 2
            for kc in range(KC):
                S.dma("pool", lambda en, kc=kc: en.dma_start(out=WG[sl].ap[:, kc, :], in_=W_GU.ap[li, e, kc * 128:(kc + 1) * 128, :]),
                      [], [(WG[sl], kc)], "ldw", 8)

        def load_wd(e):
            sl = e % 2
            for kc in range(KC):
                S.dma("pool", lambda en, kc=kc: en.dma_start(out=WD[sl].ap[:, kc, :], in_=W_DN.ap[li, e, kc * 128:(kc + 1) * 128, :]),
                      [], [(WD[sl], kc)], "ldw", 8)

        def load_xs(e):
            nf = CAP // 128
            S.dma("sp", lambda en: en.dma_start(out=XS.ap[:, 0:nf, :], in_=XG.ap[e * CAP:e * CAP + nf * 128, :].rearrange("(s p) d -> p s d", p=128)), [XG], [(XS, 0)], "ld")
            if CAP % 128:
                S.dma("sp", lambda en: en.dma_start(out=XS.ap[0:CAP % 128, nf, :], in_=XG.ap[e * CAP + nf * 128:(e + 1) * CAP, :]), [XG], [(XS, 1)], "ld")

        def transp(e):
            X = XT[e % 2]
            for k in range(KC):
                pt = next_ot()
                ptb = pt.ap.bitcast(BF16)
                for sc, (s0, sn) in enumerate(SLOTCH):
                    S.emit("pe", lambda en, ptb=ptb, sc=sc, k=k, s0=s0, sn=sn: en.transpose(ptb[:, s0:s0 + sn], XS.ap[0:sn, sc, k * 128:(k + 1) * 128], IDB[0:sn, 0:sn]),
                           [XS, CB], [pt], sc == NSC - 1)
                if k % 2 == 0:
                    S.emit("act", lambda en, ptb=ptb, k=k: en.copy(out=X.ap[:, k, :], in_=ptb[:, 0:CAP]), [pt], [(X, k)])
                else:
                    S.emit("dve", lambda en, ptb=ptb, k=k: en.tensor_copy(out=X.ap[:, k, :], in_=ptb[:, 0:CAP]), [pt], [(X, k)])

        tix = [0]

        def gate_up(e):
            sl = e % 2
            X, AC = XT[sl], ACTT[sl]
            for f in range(KC):
                for half in range(2):
                    hs = half * HN
                    SI, T3, SMt = TT[tix[0] % NT]
                    pg, pl = GUB[tix[0] % 2]
                    tix[0] += 1
                    for k in range(KC):
                        mm(pg.ap[:, 0:HN], WG[sl].ap[:, k, f * 128:(f + 1) * 128], X.ap[:, k, hs:hs + HN], k == 0, k == KC - 1,
                           [(WG[sl], k), (X, k)], [pg], k == KC - 1)
                    for k in range(KC):
                        mm(pl.ap[:, 0:HN], WG[sl].ap[:, k, D + f * 128:D + (f + 1) * 128], X.ap[:, k, hs:hs + HN], k == 0, k == KC - 1,
                           [(WG[sl], k), (X, k)], [pl], k == KC - 1)
                    bg = BG17.ap[:, e, f:f + 1]
                    bl = BL1.ap[:, li, e, f:f + 1]
                    S.emit("act", lambda en, SI=SI, pg=pg, bg=bg: en.activation(out=SI.ap, in_=pg.ap[:, 0:HN], func=AF.Silu, bias=bg, scale=1.702), [pg, BG17], [SI])
                    S.emit("dve", lambda en, T3=T3, pl=pl, bl=bl: en.tensor_scalar(out=T3.ap, in0=pl.ap[:, 0:HN], scalar1=bl, scalar2=-6.0, op0=ALU.add, op1=ALU.max),
                           [pl, BL1], [T3])
                    S.emit("dve", lambda en, SI=SI, SMt=SMt: en.tensor_scalar(out=SMt.ap, in0=SI.ap, scalar1=SILU_C, scalar2=1.0 / 1.702, op0=ALU.min, op1=ALU.mult),
                           [SI], [SMt])
                    S.emit("dve", lambda en, T3=T3, SMt=SMt, f=f, hs=hs: en.scalar_tensor_tensor(out=AC.ap[:, f, hs:hs + HN], in0=T3.ap, scalar=8.0, in1=SMt.ap,
                                                                                           op0=ALU.min, op1=ALU.mult), [T3, SMt], [(AC, (f, half))])

        def down(e):
            sl = e % 2
            AC = ACTT[sl]
            for sc, (s0, sn) in enumerate(SLOTCH):
                Y_ = YS[sc % 2]
                for dh in range(2):
                    pd = next_ot()
                    for f in range(KC):
                        mm(pd.ap[0:sn, :], AC.ap[:, f, s0:s0 + sn], WD[sl].ap[:, f, dh * 512:(dh + 1) * 512], f == 0, f == KC - 1,
                           [AC, (WD[sl], f)], [pd], f == KC - 1)
                    if dh == 0:
                        S.emit("act", lambda en, Y_=Y_, pd=pd, sn=sn: en.copy(out=Y_.ap[0:sn, 0:512], in_=pd.ap[0:sn, :]), [pd], [(Y_, 0)])
                    else:
                        S.emit("dve", lambda en, Y_=Y_, pd=pd, sn=sn: en.tensor_copy(out=Y_.ap[0:sn, 512:1024], in_=pd.ap[0:sn, :]), [pd], [(Y_, 1)])
                r0 = e * CAP + s0
                S.dma("sp", lambda en, Y_=Y_, r0=r0, sn=sn: en.dma_start(out=YG.ap[r0:r0 + sn, :], in_=Y_.ap[0:sn]), [Y_], [(YG, "y")], "st")

        load_wg(0)
        load_wd(0)
        load_xs(0)
        transp(0)
        load_xs(1)
        for e in range(NE):
            if e + 1 < NE:
                load_wg(e + 1)
                transp(e + 1)
                if e + 2 < NE:
                    load_xs(e + 2)
            gate_up(e)
            if e >= 1:
                down(e - 1)
            if e + 1 < NE:
                load_wd(e + 1)
        down(NE - 1)
        A.reset(m)
        pre = A.mark()
        if h2t:
            h2t = A.alloc("h2t", [KC, L], BF16)
        m = A.mark()
        YGa = [A.alloc("yga%d" % i, [4, D], F32) for i in range(NB)]
        BDN = A.alloc("bdn", [D], F32)
        GT = [A.alloc("gt%d" % i, [128], F32) for i in range(NB)]
        S.dma("sp", lambda en: en.dma_start(out=BDN.ap[0:NE], in_=B_DN.ap[li]), [], [BDN], "ld")
        load_ln_params(ln_idx, None)
        def comb_gen(it, ci):
            t0, n = CHUNKS[ci]
            Yg = YGa[it % NB]
            for kk in range(4):
                S.dma("pool", lambda en, kk=kk: en.indirect_dma_start(out=Yg.ap[0:n, kk, :], out_offset=None, in_=YG.ap,
                                                                   in_offset=bass.IndirectOffsetOnAxis(ap=DI.ap[0:n, ci, kk:kk + 1], axis=0),
                                                                   bounds_check=bcreg(en), oob_is_err=False),
                      [YG, (DI, ci)], [(Yg, kk)], "ind", 8)
            yield
            pg_ = yield from ps_get()
            S.emit("pe", lambda en: en.transpose(pg_.ap[0:NE, 0:n], GALL.ap[0:n, ci, :], IDF[0:n, 0:n]), [(GALL, ci), CF], [pg_], True)
            yield
            S.emit("act", lambda en: en.copy(out=GT[it % NB].ap[0:NE, 0:n], in_=pg_.ap[0:NE, 0:n]), [pg_], [GT[it % NB]])
            yield
            ps_put(pg_)
            pp = ((yield from ps_get()), (yield from ps_get()))
            for h in range(2):
                mm(pp[h].ap[0:n, :], GT[it % NB].ap[0:NE, 0:n], BDN.ap[0:NE, h * 512:(h + 1) * 512], True, True, [GT[it % NB], BDN], [pp[h]], True)
            yield
            yield from epilogue(ci, pp, Yg, hprev, hnext, route_li=None, h2t=(h2t or None), out_rows=((ci - 1) * 128 if to_out else None))

        interleave([comb_gen(it, ci) for it, ci in enumerate(chunk_ids)], NB, 10)
        A.reset(m)
        return h2t, pre


    def phase_na(H2T):
        m = A.mark()
        A.reset(base_mark)
        WQ = [A.alloc("wq%d" % i, [KC, 3, 128], BF16) for i in range(2)]
        QT0 = A.alloc("qt", [SEQ], BF16)
        KT0 = A.alloc("kt", [L], BF16)
        VA0 = A.alloc("va", [33, 2, 65], BF16)
        assert A.mark() <= EPT["end"]
        A.reset(m)
        QTs = [QT0, A.alloc("qt1", [SEQ], BF16)]
        KTs = [KT0, A.alloc("kt1", [L], BF16)]
        VAs = [VA0, A.alloc("va1", [33, 2, 65], BF16)]
        ATT = [A.alloc("att%d" % i, [32, 128], BF16) for i in range(1)]
        E2 = A.alloc("e2", [18, 64], F32)
        E2b = A.alloc("e2b", [18, 64], BF16)
        E2c = A.alloc("e2c", [18, 64], BF16)
        EB = [[A.alloc("eb%d_%d" % (i, p), [5, 128], BF16) for p in range(5)] for i in range(2)]
        PT = [A.alloc("pt%d" % i, [6, 128], BF16) for i in range(6)]
        RC = [A.alloc("rc%d" % i, [1], F32) for i in range(8)]
        EMB = A.alloc("emb", [2], F32)
        for VA in VAs:
            S.emit("pool", lambda e, VA=VA: e.memset(VA.ap[:, :, :, 64:65], 1.0), [], [VA])

        def rs(qr):
            return min(max(qr - 4, 0), 56)

        def pat_of(rp):
            return {0: 0, 1: 1, 30: 3, 31: 4}.get(rp, 2)

        pat_rp = [0, 1, 2, 30, 31]

        def load_wq(hp):
            for j in range(3):
                S.dma("pool", lambda e, j=j, hp=hp: e.dma_start(out=WQ[hp % 2].ap[:, :, j, :],
                                                                in_=W_QKV.ap[:, j * D + hp * 128:j * D + (hp + 1) * 128].rearrange("(k p) c -> p k c", p=128)),
                      [], [(WQ[hp % 2], j)], "ldc", 8)

        def qkv_gen(hp):
            W = WQ[hp % 2]
            QT, KT, VA = QTs[hp % 2], KTs[hp % 2], VAs[hp % 2]
            for g in range(8):
                ps = yield from ps_get()
                for kc in range(KC):
                    mm(ps.ap, W.ap[:, kc, 0, :], H2T.ap[:, kc, 16 + 512 * g:16 + 512 * (g + 1)], kc == 0, kc == KC - 1, [(W, 0), H2T], [ps], kc == KC - 1)
                S.emit("act", lambda e, ps=ps, g=g: e.mul(QT.ap[:, 512 * g:512 * (g + 1)], ps.ap, 0.125), [ps], [(QT, g)])
                ps_put(ps)
                yield
            for gi, (t0, n) in enumerate(GROUPS):
                ps = yield from ps_get()
                for kc in range(KC):
                    mm(ps.ap[:, 0:n], W.ap[:, kc, 1, :], H2T.ap[:, kc, t0:t0 + n], kc == 0, kc == KC - 1, [(W, 1), H2T], [ps], kc == KC - 1)
                S.emit("dve", lambda e, ps=ps, t0=t0, n=n: e.tensor_copy(out=KT.ap[:, t0:t0 + n], in_=ps.ap[:, 0:n]), [ps], [(KT, gi)])
                ps_put(ps)
                yield
            for ci, (t0, n) in enumerate(CHUNKS):
                ps = yield from ps_get()
                for kc in range(KC):
                    mm(ps.ap[0:n, 0:128], H2T.ap[:, kc, t0:t0 + n], W.ap[:, kc, 2, :], kc == 0, kc == KC - 1, [(W, 2), H2T], [ps], kc == KC - 1)
                if ci % 2 == 0:
                    S.emit("act", lambda e, ps=ps, ci=ci, n=n: e.copy(out=VA.ap[0:n, ci, :, 0:64], in_=ps.ap[0:n, 0:128].rearrange("p (a b) -> p a b", a=2)),
                           [ps], [(VA, ci)])
                else:
                    S.emit("dve", lambda e, ps=ps, ci=ci, n=n: e.tensor_copy(out=VA.ap[0:n, ci, :, 0:64], in_=ps.ap[0:n, 0:128].rearrange("p (a b) -> p a b", a=2)),
                           [ps], [(VA, ci)])
                ps_put(ps)
                yield

        load_wq(0)
        load_wq(1)
        for _ in qkv_gen(0):
            pass
        for hp in range(8):
            QT, KT, VA = QTs[hp % 2], KTs[hp % 2], VAs[hp % 2]
            if 1 <= hp and hp + 1 < 8:
                load_wq(hp + 1)
            AT_ = ATT[0]
            for hh in range(2):
                h = 2 * hp + hh
                r0 = 64 * hh
                EBh = EB[hh]
                for rho in range(2):
                    src = bass.AP(RPd.ap.tensor, h * 19 * 127 + rho * 127, [[1, 64], [127, 18], [1, 64]])
                    S.dma("sp", lambda e, rho=rho, src=src: e.dma_start(out=E2.ap[64 * rho:64 * rho + 64], in_=src), [], [(E2, rho)], "ld")
                S.emit("act", lambda e: e.activation(out=E2.ap, in_=E2.ap, func=AF.Exp), [E2], [E2])
                S.emit("dve", lambda e: e.tensor_tensor(out=E2c.ap, in0=E2.ap, in1=CMASK.unsqueeze(1).to_broadcast([128, 18, 64]), op=ALU.mult), [E2, CF], [E2c])
                for j in range(3):
                    pf = next_ps()
                    mm(pf.ap[:, 0:384], J2, E2c.ap.rearrange("p a b -> p (a b)")[:, 384 * j:384 * (j + 1)], True, True, [E2c, CB], [pf], True)
                    S.emit(("act", "dve")[j % 2], lambda e, pf=pf, j=j: (e.copy if j % 2 == 0 else e.tensor_copy)(
                        out=E2b.ap.rearrange("p a b -> p (a b)")[:, 384 * j:384 * (j + 1)], in_=pf.ap[:, 0:384]), [pf], [(E2b, j)])
                for p_i, rp in enumerate(pat_rp):
                    jp0 = min(max(rp - 2, 0), 27)
                    T = EBh[p_i]
                    q = 0
                    for jpi in range(5):
                        for rho in range(2):
                            qr = 2 * rp + rho
                            kr0 = 2 * (jp0 + jpi)
                            di = kr0 - qr + 9
                            v0 = rs(qr) <= kr0 <= rs(qr) + 7
                            v1 = rs(qr) <= kr0 + 1 <= rs(qr) + 7
                            dst = T.ap[:, jpi, 64 * rho:64 * rho + 64]
                            eng = ("pool", "dve")[q % 2]
                            q += 1
                            if v0 or v1:
                                assert 0 <= di <= 17, (rp, jpi, rho, di)
                                S.emit(eng, lambda e, dst=dst, di=di: e.tensor_copy(out=dst, in_=E2b.ap[:, di, :]), [E2b], [(T, (jpi, rho))])
                                if not v0:
                                    S.emit(eng, lambda e, dst=dst: e.memset(dst[0:64], 0.0), [], [(T, (jpi, rho))])
                                if not v1:
                                    S.emit(eng, lambda e, dst=dst: e.memset(dst[64:128], 0.0), [], [(T, (jpi, rho))])
                            else:
                                S.emit(eng, lambda e, dst=dst: e.memset(dst, 0.0), [], [(T, (jpi, rho))])
                S.emit("act", lambda e, h=h, hh=hh: e.activation(out=EMB.ap[0:16, hh:hh + 1], in_=PAR.ap[0:16, P_MB + h:P_MB + h + 1], func=AF.Exp), [PAR], [(EMB, hh)])
                S.emit("pool", lambda e, hh=hh, VA=VA: e.memset(VA.ap[0:16, 0, hh, 64:65], 1.0), [(VA, 0)], [(VA, 0)])
                S.emit("dve", lambda e, hh=hh, VA=VA: e.tensor_scalar(out=VA.ap[0:16, 0, hh, :], in0=VA.ap[0:16, 0, hh, :], scalar1=EMB.ap[0:16, hh:hh + 1], scalar2=None,
                                                               op0=ALU.mult), [(VA, 0), (EMB, hh)], [(VA, 0)])

            def na_gen(hh, rp, slot, AT_, QT=QT, KT=KT, VA=VA):
                r0 = 64 * hh
                jp0 = min(max(rp - 2, 0), 27)
                T = EB[hh][pat_of(rp)]
                P_, Rc = PT[slot], RC[slot]
                psA = yield from ps_get()
                psB = yield from ps_get()
                qs = QT.ap[r0:r0 + 64, 128 * rp:128 * (rp + 1)]
                for jpi in range(5):
                    k0 = 16 + 128 * (jp0 + jpi)
                    o = psA.ap[:, 128 * jpi:128 * (jpi + 1)] if jpi < 4 else psB.ap[:, 0:128]
                    mm(o, KT.ap[r0:r0 + 64, k0:k0 + 128], qs, True, True, [KT, QT], [psA if jpi < 4 else psB], jpi == 3)
                mm(psB.ap[:, 128:256], KT.ap[r0:r0 + 64, 0:128], qs, True, True, [KT, QT], [psB], True)
                yield
                S.emit("act", lambda e: e.activation(out=P_.ap[:, 0:4, :], in_=psA.ap.rearrange("p (a b) -> p a b", a=4), func=AF.Exp), [psA], [(P_, 0)])
                S.emit("act", lambda e: e.activation(out=P_.ap[:, 4:6, :], in_=psB.ap[:, 0:256].rearrange("p (a b) -> p a b", a=2), func=AF.Exp), [psB], [(P_, 1)])
                ps_put(psA, psB)
                yield
                S.emit("dve", lambda e: e.tensor_tensor(out=P_.ap[:, 0:5, :], in0=P_.ap[:, 0:5, :], in1=T.ap, op=ALU.mult), [P_, T], [P_])
                yield
                po = yield from ps_get()
                for jpi in range(5):
                    mm(po.ap[:, 0:65], P_.ap[:, jpi, :], VA.ap[:, 1 + jp0 + jpi, hh, :], jpi == 0, False, [P_, VA], [po], False)
                mm(po.ap[:, 0:65], P_.ap[0:16, 5, :], VA.ap[0:16, 0, hh, :], False, True, [P_, VA], [po], True)
                yield
                S.emit("dve", lambda e: e.reciprocal(out=Rc.ap, in_=po.ap[:, 64:65]), [po], [Rc])
                yield
                S.emit("dve", lambda e: e.tensor_scalar(out=AT_.ap[:, rp, 64 * hh:64 * hh + 64], in0=po.ap[:, 0:64], scalar1=Rc.ap,
                                                        scalar2=None, op0=ALU.mult), [po, Rc], [(AT_, (rp, hh))])
                ps_put(po)
                yield

            def na_chain(hh, par, AT_=AT_):
                for j, rp in enumerate(range(par, 32, 2)):
                    yield from na_gen(hh, rp, ((hh * 2 + par) * 2 + j % 2) % 6 if False else (hh * 2 + par), AT_)

            extra_g = [qkv_gen(hp + 1)] if hp + 1 < 8 else []
            interleave([na_chain(hh, par) for hh in range(2) for par in range(2)] + extra_g, 5, 1)
            S.dma("sp", lambda e, AT_=AT_, hp=hp: e.dma_start(out=ATTD.ap[:, hp * 128:(hp + 1) * 128].rearrange("(r p) c -> p r c", p=128), in_=AT_.ap),
                  [AT_], [(ATTD, hp)], "st")
        A.reset(m)
        m = A.mark()
        WNO = A.alloc("wno", [KC, D], BF16)
        ATt = [A.alloc("att_in%d" % i, [D], BF16) for i in range(NB)]
        ATk = [A.alloc("atk%d" % i, [KC, 128], BF16) for i in range(NB)]
        for kc in range(KC):
            S.dma("pool", lambda e, kc=kc: e.dma_start(out=WNO.ap[:, kc, :], in_=W_NO.ap[kc * 128:(kc + 1) * 128, :]), [], [(WNO, kc)], "ldc", 8)
        load_ln_params(2, 1)
        def no_gen(ci):
            a_in, a_k = ATt[ci % NB], ATk[ci % NB]
            S.dma("sp", lambda e: e.dma_start(out=a_in.ap, in_=ATTD.ap[(ci - 1) * 128:ci * 128, :]), [ATTD], [a_in], "ld")
            yield
            pt = yield from ps_get()
            ptb = pt.ap.bitcast(BF16)
            for k in range(KC):
                S.emit("pe", lambda e, k=k: e.transpose(ptb[:, k * 128:(k + 1) * 128], a_in.ap[:, k * 128:(k + 1) * 128], IDB), [a_in, CB], [pt], k == KC - 1)
            yield
            S.emit("act", lambda e: e.copy(out=a_k.ap, in_=ptb.rearrange("p (a b) -> p a b", a=KC)), [pt], [a_k])
            yield
            ps_put(pt)
            pp = ((yield from ps_get()), (yield from ps_get()))
            for h in range(2):
                for kc in range(KC):
                    mm(pp[h].ap, a_k.ap[:, kc, :], WNO.ap[:, kc, h * 512:(h + 1) * 512], kc == 0, kc == KC - 1, [a_k, (WNO, kc)], [pp[h]], kc == KC - 1)
                yield
            yield from epilogue(ci, pp, None, H2, H3, route_li=1)

        interleave([no_gen(ci) for ci in range(1, 33)], NB, 30)
        A.reset(m)

    def zero_fill_xg(part, nparts):
        nz = (DUMP + 1) // 1024
        for i in range(nz):
            if i % nparts == part:
                S.dma("sp", lambda e, i=i: e.dma_start(out=XG.ap[i * 1024:(i + 1) * 1024, :], in_=ZROWS.ap), [], [(XG, "sc")], "zf", 8)
        rem = DUMP + 1 - nz * 1024
        if rem and part == 0:
            S.dma("sp", lambda e: e.dma_start(out=XG.ap[nz * 1024:DUMP + 1, :], in_=ZROWS.ap[0:rem, :]), [], [(XG, "sc")], "zf", 8)

    zero_m = A.mark()
    ZR = A.alloc("zr", [D], F32)
    S.emit("pool", lambda e: e.memset(ZR.ap, 0.0), [], [ZR])
    S.dma("sp", lambda e: e.dma_start(out=YG.ap[DUMP:DUMP + 1, :], in_=ZR.ap[0:1, :]), [ZR], [(YG, "dump")], "st")
    A.reset(zero_m)

    phase_1a()
    phase_1b()
    if stop != "1b":
        alloc_epilogue()
        phase_1c()
        H2T_, pre_ = phase_moe(0, H1, H2, 1, list(range(33)), h2t=True)
        phase_na(H2T_)
        A.reset(pre_)
        phase_moe(1, H3, None, 3, list(range(1, 33)), to_out=True)

    outs = [OUT] if stop is None else [ZTd]
    if debug and stop is None:
        outs += [H1, H2, H3, ATTD]
    S.final_wait("sp", outs)
    S.check()
    with nc.Block() as block:
        S.build(block)
    es.close()
    return nc, in_names


def host_inputs(inp, b):
    f = np.float32
    x = np.asarray(inp["x"][b], f)
    h0 = np.ascontiguousarray(np.concatenate([np.asarray(inp["meta_tokens"], f), x], axis=0))
    d = {"h0": h0, "h0T": np.ascontiguousarray(h0.T)}
    return d


def shared_inputs(inp):
    f = np.float32
    par = np.zeros((128, NPAR), f)
    cw = np.asarray(inp["lru_conv_w"][0], f)
    par[:, P_CW:P_CW + 32] = cw.reshape(4, 8, 128).transpose(2, 1, 0).reshape(128, 32)
    par[:, P_CB:P_CB + 8] = np.asarray(inp["lru_conv_b"][0], f).reshape(8, 128).T
    for name, col in (("lru_ba", P_BA), ("lru_bx", P_BX), ("lru_lambda", P_LAM)):
        par[:, col:col + 16] = np.asarray(inp[name][0], f).reshape(2, 8, 128).transpose(2, 0, 1).reshape(128, 16)
    for li, col in ((0, P_BGU0), (1, P_BGU1)):
        par[:, col:col + 512] = np.asarray(inp["moe_b_gu"][li], f).reshape(NE, 16, 128).transpose(2, 0, 1).reshape(128, 512)
    par[0:16, P_MB:P_MB + 16] = np.asarray(inp["na_meta_bias"][0], f).T
    rows = np.stack([inp["ln_mix_g"][0], inp["ln_mix_b"][0], inp["ln_ffn_g"][0], inp["ln_ffn_b"][0],
                     inp["ln_mix_g"][1], inp["ln_mix_b"][1], inp["ln_ffn_g"][1], inp["ln_ffn_b"][1]]).astype(f)
    rpb = np.asarray(inp["na_rpb"][0], f)
    rp = np.zeros((16, 19, 127), f)
    rp[:, 2:17, 48:79] = rpb[:, :, ::-1]
    cstf = np.zeros((128, 224), f)
    cstf[:, 0:128] = np.eye(128, dtype=f)
    cstf[:, 128:160] = (np.arange(NE) * CAP - DUMP).astype(f)[None, :]
    cc = np.arange(64)
    cs = np.clip(cc - 8, 0, 48)
    cm = ((cc[:, None] >= cs[None, :]) & (cc[:, None] <= cs[None, :] + 15)).astype(f)
    cstf[:, 160:224] = np.concatenate([cm[::-1], cm[::-1]], axis=0)
    cstb = np.zeros((128, 512), f)
    jj = np.eye(64, dtype=f)[::-1]
    cstb[0:64, 384:448] = jj
    cstb[64:128, 448:512] = jj
    cstb[:, 0:128] = np.eye(128)
    cstb[:, 128:256] = np.triu(np.ones((128, 128)), 1)
    cstb[:, 256:384] = 1.0
    d = {"zrows": np.zeros((1024, D), ml_dtypes.bfloat16), "par": par, "rows": rows, "rb": np.asarray(inp["router_b"], f), "cstf": cstf, "cstb": cstb.astype(ml_dtypes.bfloat16),
         "lru_w_in": np.asarray(inp["lru_w_in"][0], f), "lru_wa": np.asarray(inp["lru_wa"][0], f), "lru_wx": np.asarray(inp["lru_wx"][0], f),
         "lru_w_out": np.asarray(inp["lru_w_out"][0], f), "na_w_qkv": np.asarray(inp["na_w_qkv"][0], f),
         "na_w_out": np.asarray(inp["na_w_out"][0], f), "rp": rp, "router_w": np.asarray(inp["router_w"], f),
         "moe_w_gu": np.asarray(inp["moe_w_gu"], f), "moe_w_down": np.asarray(inp["moe_w_down"], f),
         "moe_b_down": np.asarray(inp["moe_b_down"], f)}
    return d


def kernel(**inputs):
    nc, names = build_program()
    sh = shared_inputs(inputs)
    in_maps = []
    for b in range(8):
        m = dict(sh)
        m.update(host_inputs(inputs, b))
        in_maps.append({k: m[k] for k in names})
    res = run_bass_kernel_spmd(nc, in_maps, core_ids=list(range(8)))
    return np.stack([np.asarray(r["out"], np.float32) for r in res.results], axis=0)
```

```python
import contextlib
import numpy as np
import ml_dtypes
import concourse.bass as bass
import concourse.mybir as mybir
from concourse.bass_utils import run_bass_kernel_spmd

F32 = mybir.dt.float32
BF16 = mybir.dt.bfloat16
I32 = mybir.dt.int32
ALU = mybir.AluOpType
AF = mybir.ActivationFunctionType
AX = mybir.AxisListType

D = 1024
KC = 8
NMETA = 16
SEQ = 4096
L = NMETA + SEQ
NE = 32
CAP = 704
SLOTCH = [(i * 128, min(128, CAP - i * 128)) for i in range((CAP + 127) // 128)]
NSC = len(SLOTCH)
NB = 3
NBE = 4
DUMP = NE * CAP
ALPHA = 4.0 ** 0.25
EPS = 1e-5
GROUPS = [(0, 16)] + [(16 + 512 * g, 512) for g in range(8)]
CHUNKS = [(0, 16)] + [(16 + 128 * c, 128) for c in range(32)]

P_CW, P_CB, P_BA, P_BX, P_LAM, P_BGU0, P_BGU1, P_MB, NPAR = 0, 32, 40, 56, 72, 88, 600, 1112, 1128


class Buf:
    def __init__(self, name, space, rng=None):
        self.name, self.space, self.rng, self.aliases = name, space, rng, []


class Tile:
    def __init__(self, buf, ap):
        self.buf, self.ap = buf, ap


def _key(x):
    if isinstance(x, Tile):
        return (x.buf, None)
    t, i = x
    return (t.buf, i)


class Sched:
    CE = ("pe", "act", "dve", "pool")

    def __init__(self, nc, es):
        self.nc, self.es = nc, es
        self.q = {e: [] for e in self.CE + ("sp",)}
        self.cnt = {e: 0 for e in self.CE}
        self.semh = {e: es.enter_context(nc.semaphore("c_" + e)) for e in self.CE}
        self.seen = {e: {} for e in self.q}
        self.lastw, self.rd, self.keys_of, self.chans = {}, {}, {}, {}

    def _match(self, b, i):
        ks = self.keys_of.get(b, ())
        if i is None:
            return [(b, j) for j in ks]
        return [(b, j) for j in (i, None) if j in ks]

    def _deps(self, reads, writes):
        ev = {}

        def add(e):
            if e is not None and ev.get(e[0], 0) < e[1]:
                ev[e[0]] = e[1]

        for (b, i) in reads:
            for k in self._match(b, i):
                add(self.lastw.get(k))
        for (b, i) in writes:
            ks = self._match(b, i)
            for a in b.aliases:
                ks = ks + self._match(a, None)
            for k in ks:
                add(self.lastw.get(k))
                for sk, v in self.rd.get(k, {}).items():
                    add((sk, v))
        return ev

    def _record(self, reads, writes, e):
        for (b, i) in writes:
            if i is None:
                for k in self._match(b, None):
                    self.lastw[k] = e
                    self.rd[k] = {}
            self.keys_of.setdefault(b, set()).add(i)
            self.lastw[(b, i)] = e
            self.rd[(b, i)] = {}
        for (b, i) in reads:
            self.keys_of.setdefault(b, set()).add(i)
            d = self.rd.setdefault((b, i), {})
            if d.get(e[0], 0) < e[1]:
                d[e[0]] = e[1]

    def _waits(self, eng, ev):
        w = []
        for sk, v in ev.items():
            if eng == "pe" and sk == "pe":
                continue
            if self.seen[eng].get(sk, 0) >= v:
                continue
            self.seen[eng][sk] = v
            w.append((sk, v))
        return w

    def emit(self, eng, fn, reads=(), writes=(), inc=True):
        reads = [_key(x) for x in reads]
        writes = [_key(x) for x in writes]
        assert inc or eng == "pe"
        w = self._waits(eng, self._deps(reads, writes))
        if inc:
            self.cnt[eng] += 1
            e = (eng, self.cnt[eng])
        else:
            e = (eng, self.cnt[eng] + 1)
        self._record(reads, writes, e)
        self.q[eng].append((w, fn, eng if inc else None))

    def dma(self, q, fn, reads=(), writes=(), chan="d", K=4):
        reads = [_key(x) for x in reads]
        writes = [_key(x) for x in writes]
        st = self.chans.get(chan)
        if st is None:
            st = {"i": 0, "K": K}
            for s in range(K):
                self.semh[("dma", chan, s)] = self.es.enter_context(self.nc.semaphore("d_%s%d" % (chan, s)))
            self.chans[chan] = st
        i, K = st["i"], st["K"]
        st["i"] += 1
        sk = ("dma", chan, i % K)
        ev = self._deps(reads, writes)
        if i >= K:
            ev[sk] = max(ev.get(sk, 0), 16 * (i // K))
        w = self._waits(q, ev)
        self._record(reads, writes, (sk, 16 * (i // K + 1)))
        self.q[q].append((w, fn, sk))

    def final_wait(self, q, tiles):
        ev = self._deps([_key(t) for t in tiles], [])
        self.q[q].append((self._waits(q, ev), None, None))


    def check(self):
        sem = {}
        pos = {e: 0 for e in self.q}
        progress = True
        while progress:
            progress = False
            for e, lst in self.q.items():
                while pos[e] < len(lst):
                    waits, fn, inc = lst[pos[e]]
                    if any(sem.get(sk, 0) < v for sk, v in waits):
                        break
                    if fn is not None and inc is not None:
                        sem[inc] = sem.get(inc, 0) + (16 if isinstance(inc, tuple) else 1)
                    pos[e] += 1
                    progress = True
        stuck = {e: (pos[e], len(lst)) for e, lst in self.q.items() if pos[e] < len(lst)}
        if stuck:
            msg = []
            for e, (p, n) in stuck.items():
                waits = self.q[e][p][0]
                msg.append("%s stuck at %d/%d waiting %s" % (e, p, n, [(sk, v, sem.get(sk, 0)) for sk, v in waits if sem.get(sk, 0) < v]))
            raise RuntimeError("sync deadlock: " + "; ".join(msg))

    def build(self, block):
        for eng, deco in (("pe", block.tensor), ("act", block.scalar), ("dve", block.vector),
                          ("pool", block.gpsimd), ("sp", block.sync)):
            def body(e, lst=self.q[eng]):
                for waits, fn, inc in lst:
                    for sk, v in waits:
                        e.wait_ge(self.semh[sk], v)
                    if fn is None:
                        continue
                    ins = fn(e)
                    if inc is None:
                        continue
                    if isinstance(inc, tuple):
                        ins.then_inc(self.semh[inc], 16)
                    else:
                        ins.then_inc(self.semh[inc], 1)
            deco(body)


class Arena:
    def __init__(self, nc, es, nbytes):
        self.cap = nbytes
        self.t = es.enter_context(nc.sbuf_tensor("arena", [128, nbytes // 2], BF16))
        self.pos = 0
        self.bufs = []

    def alloc(self, name, free, dtype, parts=128):
        esz = 2 if dtype == BF16 else 4
        n = int(np.prod(free))
        nb = n * esz
        nba = (nb + 63) // 64 * 64
        off = self.pos
        self.pos += nba
        assert self.pos <= self.cap, ("SBUF arena overflow", name, self.pos)
        ap = self.t[0:parts, off // 2: off // 2 + nb // 2]
        if dtype != BF16:
            ap = ap.bitcast(dtype)
        if len(free) == 2:
            ap = ap.rearrange("p (a b) -> p a b", a=free[0])
        elif len(free) == 3:
            ap = ap.rearrange("p (a b c) -> p a b c", a=free[0], b=free[1])
        b = Buf(name, "sb", (off, off + nba))
        for o in self.bufs:
            if o.rng[0] < b.rng[1] and b.rng[0] < o.rng[1]:
                b.aliases.append(o)
                o.aliases.append(b)
        self.bufs.append(b)
        return Tile(b, ap)

    def mark(self):
        return self.pos

    def reset(self, m):
        self.pos = m


def build_program(debug=False, stop=None):
    nc = bass.Bass("TRN2", target_bir_lowering=False)
    es = contextlib.ExitStack()

    in_names = []

    def din(name, shape, dt=F32):
        in_names.append(name)
        return Tile(Buf(name, "dram"), nc.dram_tensor(name, list(shape), dt, kind="ExternalInput").ap())

    def dscr(name, shape, dt=F32, out=False):
        kind = "ExternalOutput" if (out or debug) else "Internal"
        return Tile(Buf(name, "dram"), nc.dram_tensor(name, list(shape), dt, kind=kind).ap())

    H0 = din("h0", [L, D])
    H0T = din("h0T", [D, L])
    PARd = din("par", [128, NPAR])
    ROWSd = din("rows", [8, D])
    RBd = din("rb", [2, NE])
    CSTF = din("cstf", [128, 224])
    CSTB = din("cstb", [128, 512], BF16)
    ZROWS = din("zrows", [1024, D], BF16)
    W_IN = din("lru_w_in", [D, 2 * D])
    W_A = din("lru_wa", [2, 4, 256, 256])
    W_X = din("lru_wx", [2, 4, 256, 256])
    W_LO = din("lru_w_out", [D, D])
    W_QKV = din("na_w_qkv", [D, 3 * D])
    W_NO = din("na_w_out", [D, D])
    RPd = din("rp", [16, 19, 127])
    W_R = din("router_w", [2, D, NE])
    W_GU = din("moe_w_gu", [2, NE, D, 2 * D])
    W_DN = din("moe_w_down", [2, NE, D, D])
    B_DN = din("moe_b_down", [2, NE, D])
    OUT = dscr("out", [SEQ, D], out=True)

    XRd = dscr("xr_s", [8, 128, L])
    GYd = dscr("gy_s", [8, 128, L], BF16)
    ZTd = dscr("zt_s", [8, 128, L], BF16)
    H1 = dscr("h1_s", [L, D])
    H2 = dscr("h2_s", [L, D])
    H3 = dscr("h3_s", [L, D])
    XG = dscr("xg_s", [DUMP + 1, D], BF16)
    YG = dscr("yg_s", [DUMP + 1, D])
    ATTD = dscr("att_s", [SEQ, D], BF16)

    S = Sched(nc, es)
    A = Arena(nc, es, 207 * 1024)
    PS = []
    for i in range(8):
        t = es.enter_context(nc.psum_tensor("ps%d" % i, [128, 512], F32))
        PS.append(Tile(Buf("ps%d" % i, "ps"), t[:]))
    psi = [0]

    def next_ps():
        p = PS[psi[0] % 8]
        psi[0] += 1
        return p

    ps_free = list(range(8))

    def ps_get():
        while not ps_free:
            yield
        return PS[ps_free.pop(0)]

    def ps_put(*ps):
        for p in ps:
            ps_free.append(PS.index(p))

    def mm(out, lhsT, rhs, start, stop, reads, writes, inc):
        S.emit("pe", lambda e: e.matmul(out, lhsT, rhs, start=start, stop=stop), reads, writes, inc)

    PAR = A.alloc("par", [NPAR], F32)
    CF = A.alloc("cstf", [224], F32)
    CB = A.alloc("cstb", [512], BF16)
    IDF = CF.ap[:, 0:128]
    EOFFM = CF.ap[:, 128:160]
    CMASK = CF.ap[:, 160:224]
    IDB = CB.ap[:, 0:128]
    UTRI = CB.ap[:, 128:256]
    ONESB = CB.ap[:, 256:384]
    J2 = CB.ap[:, 384:512]
    SP = A.alloc("sp", [32], F32)
    BL1 = A.alloc("bl1", [2, NE, 8], F32)
    HBIAS = A.alloc("hbias", [32], F32)
    SPH = A.alloc("sph", [16], F32)
    GALL = A.alloc("gall", [33, NE], F32)
    GK = A.alloc("gk", [33, 4], F32)
    DI = A.alloc("di", [33, 4], I32)
    CNT = A.alloc("cnt", [NE], F32)
    S.dma("sp", lambda e: e.dma_start(out=PAR.ap, in_=PARd.ap), [], [PAR], "ld")
    S.dma("sp", lambda e: e.dma_start(out=CF.ap, in_=CSTF.ap), [], [CF], "ld")
    S.dma("sp", lambda e: e.dma_start(out=CB.ap, in_=CSTB.ap), [], [CB], "ld")
    S.emit("act", lambda e: e.activation(out=SP.ap[:, 0:16], in_=PAR.ap[:, P_LAM:P_LAM + 16], func=AF.Exp, scale=-1.0), [PAR], [SP])
    S.emit("act", lambda e: e.activation(out=SP.ap[:, 0:16], in_=SP.ap[:, 0:16], func=AF.Ln, bias=1.0, scale=1.0), [SP], [SP])
    S.emit("dve", lambda e: e.tensor_scalar(out=SP.ap[:, 16:32], in0=SP.ap[:, 0:16], scalar1=-16.0, scalar2=None, op0=ALU.mult), [SP], [SP])
    S.emit("dve", lambda e: e.tensor_scalar(out=SP.ap[:, 0:16], in0=SP.ap[:, 0:16], scalar1=-8.0, scalar2=None, op0=ALU.mult), [SP], [SP])
    S.emit("dve", lambda e: e.tensor_scalar(out=SPH.ap, in0=SP.ap[:, 0:16], scalar1=0.5, scalar2=None, op0=ALU.mult), [SP], [SPH])
    S.emit("dve", lambda e: e.tensor_scalar(out=HBIAS.ap, in0=PAR.ap[:, P_BA:P_BA + 32], scalar1=0.5, scalar2=None, op0=ALU.mult), [PAR], [HBIAS])
    for li in range(2):
        c0 = (P_BGU0, P_BGU1)[li]
        src = PAR.ap[:, c0:c0 + 512].rearrange("p (e f) -> p e f", e=NE)[:, :, 8:16]
        S.emit("dve", lambda e, src=src, li=li: e.tensor_scalar(out=BL1.ap[:, li], in0=src, scalar1=1.0, scalar2=None, op0=ALU.add), [PAR], [BL1])
    base_mark = A.mark()

    def phase_1a():
        m = A.mark()
        h0T = A.alloc("h0T", [KC, L], BF16)
        win = A.alloc("win", [KC, 2 * D], BF16)
        ms = A.mark()
        stg = [A.alloc("stg%d" % i, [L], F32) for i in range(2)]
        A.reset(ms)
        stb = [A.alloc("stb%d" % i, [L], BF16) for i in range(2)]
        for kc in range(KC):
            for (a, b) in ((0, 2048), (2048, 4096), (4096, L)):
                S.dma("pool", lambda e, kc=kc, a=a, b=b: e.dma_start(out=h0T.ap[:, kc, a:b], in_=H0T.ap[kc * 128:(kc + 1) * 128, a:b]),
                      [], [(h0T, kc)], "ldc", 8)
            S.dma("pool", lambda e, kc=kc: e.dma_start(out=win.ap[:, kc, :], in_=W_IN.ap[kc * 128:(kc + 1) * 128, :]),
                  [], [(win, kc)], "ldc", 8)
        for fc in range(16):
            st = (stg if fc < 8 else stb)[fc % 2]
            for gi, (t0, n) in enumerate(GROUPS):
                ps = next_ps()
                for kc in range(KC):
                    mm(ps.ap[:, 0:n], win.ap[:, kc, fc * 128:(fc + 1) * 128], h0T.ap[:, kc, t0:t0 + n], kc == 0, kc == KC - 1,
                       [(win, kc), (h0T, kc)], [ps], kc == KC - 1)
                if fc < 8:
                    if gi % 2 == 0:
                        S.emit("act", lambda e, st=st, ps=ps, t0=t0, n=n: e.copy(out=st.ap[:, t0:t0 + n], in_=ps.ap[:, 0:n]), [ps], [(st, gi)])
                    else:
                        S.emit("dve", lambda e, st=st, ps=ps, t0=t0, n=n: e.tensor_copy(out=st.ap[:, t0:t0 + n], in_=ps.ap[:, 0:n]), [ps], [(st, gi)])
                else:
                    S.emit("act", lambda e, st=st, ps=ps, t0=t0, n=n: e.activation(out=st.ap[:, t0:t0 + n], in_=ps.ap[:, 0:n], func=AF.Gelu_apprx_tanh),
                           [ps], [(st, gi)])
            if fc < 8:
                S.dma("sp", lambda e, st=st, fc=fc: e.dma_start(out=XRd.ap[fc], in_=st.ap), [st], [(XRd, fc)], "st")
            else:
                S.dma("sp", lambda e, st=st, fc=fc: e.dma_start(out=GYd.ap[fc - 8], in_=st.ap), [st], [(GYd, fc - 8)], "st")
        A.reset(m)

    def phase_1b():
        m = A.mark()
        WA = A.alloc("wa", [16, 256], BF16)
        WX = A.alloc("wx", [16, 256], BF16)
        S.dma("pool", lambda e: e.dma_start(out=WA.ap, in_=W_A.ap.rearrange("d n (i p) j -> p (d n i) j", p=128)), [], [WA], "ldc", 8)
        S.dma("pool", lambda e: e.dma_start(out=WX.ap, in_=W_X.ap.rearrange("d n (i p) j -> p (d n i) j", p=128)), [], [WX], "ldc", 8)
        XC = A.alloc("xc", [2, L], F32)
        XCB = A.alloc("xcb", [2, L], BF16)
        HFB = [[A.alloc("h%d_%d" % (d, i), [L], F32) for d in range(2)] for i in range(2)]
        GYt = A.alloc("gyt", [L], BF16)
        Zt = A.alloc("zt", [L], BF16)
        HB0 = 2048
        HLMAX = L - HB0
        m2 = A.mark()
        XRP = A.alloc("xrp", [2, L + 4], F32)
        A.reset(m2)
        GA = [A.alloc("ga%d" % d, [HLMAX], F32) for d in range(2)]
        GX = [A.alloc("gx%d" % d, [HLMAX], F32) for d in range(2)]
        MMt = [A.alloc("mm%d" % d, [HLMAX], F32) for d in range(2)]
        HGROUPS = [[(512 * g, 512) for g in range(4)], [(512 * g, 512) for g in range(4)] + [(2048, L - HB0 - 2048)]]
        done = {}

        def chain(nb, jc, d):
            c = 2 * nb + jc
            H = HFB[jc][d]
            ga, gx, mmt = GA[d], GX[d], MMt[d]
            shcol = SPH.ap[:, d * 8 + c:d * 8 + c + 1]
            scol = SP.ap[:, d * 8 + c:d * 8 + c + 1]
            for hi, half in enumerate((0, 1) if d == 0 else (1, 0)):
                h0 = half * HB0
                hl = HB0 if half == 0 else L - HB0
                for gi, (g0, n) in enumerate(HGROUPS[half]):
                    t0 = h0 + g0
                    for (Wt, Gt, pcol) in ((WA, ga, 0), (WX, gx, 16)):
                        ps = yield from ps_get()
                        for ic in range(2):
                            mm(ps.ap[:, 0:n], Wt.ap[:, d * 8 + nb * 2 + ic, jc * 128:(jc + 1) * 128], XCB.ap[:, ic, t0:t0 + n], ic == 0, ic == 1,
                               [Wt, (XCB, ic)], [ps], ic == 1)
                        bcol = HBIAS.ap[:, pcol + d * 8 + c:pcol + d * 8 + c + 1]
                        S.emit("act", lambda e, Gt=Gt, ps=ps, g0=g0, n=n, bcol=bcol: e.activation(out=Gt.ap[:, g0:g0 + n], in_=ps.ap[:, 0:n], func=AF.Tanh,
                                                                                                 bias=bcol, scale=0.5), [ps, HBIAS], [(Gt, gi)])
                        ps_put(ps)
                        yield
                S.emit("act", lambda e, hl=hl: e.activation(out=mmt.ap[:, 0:hl], in_=ga.ap[:, 0:hl], func=AF.Exp, scale=scol, bias=scol), [ga, SP], [mmt])
                yield
                S.emit("act", lambda e, hl=hl: e.activation(out=ga.ap[:, 0:hl], in_=ga.ap[:, 0:hl], func=AF.Exp, scale=shcol, bias=shcol), [ga, SPH], [ga])
                yield
                S.emit("act", lambda e, hl=hl: e.activation(out=mmt.ap[:, 0:hl], in_=mmt.ap[:, 0:hl], func=AF.Sqrt, bias=0.25, scale=-0.25), [mmt], [mmt])
                yield
                if hi == 0:
                    sc = 0 if d == 0 else hl - 1
                    S.emit("pool", lambda e, sc=sc: e.memset(mmt.ap[:, sc:sc + 1], 0.5), [mmt], [mmt])
                    yield
                S.emit("dve", lambda e, h0=h0, hl=hl: e.scalar_tensor_tensor(out=gx.ap[:, 0:hl], in0=gx.ap[:, 0:hl], scalar=1.0, in1=XC.ap[:, jc, h0:h0 + hl],
                                                                          op0=ALU.add, op1=ALU.mult), [gx, (XC, jc)], [gx])
                yield
                S.emit("pool", lambda e, hl=hl: e.tensor_tensor(out=gx.ap[:, 0:hl], in0=gx.ap[:, 0:hl], in1=mmt.ap[:, 0:hl], op=ALU.mult), [gx, mmt], [gx])
                for _ in range(5):
                    yield
                if hi == 1:
                    fi = 0 if d == 0 else hl - 1
                    prev = H.ap[:, HB0 - 1:HB0] if d == 0 else H.ap[:, HB0:HB0 + 1]
                    S.emit("dve", lambda e, fi=fi, prev=prev: e.scalar_tensor_tensor(out=gx.ap[:, fi:fi + 1], in0=ga.ap[:, fi:fi + 1], scalar=prev, in1=gx.ap[:, fi:fi + 1],
                                                                                  op0=ALU.mult, op1=ALU.add), [ga, gx, (H, 1 - half)], [gx])
                    yield
                if d == 0:
                    S.emit("dve", lambda e, h0=h0, hl=hl: e.tensor_tensor_scan(out=H.ap[:, h0:h0 + hl], data0=ga.ap[:, 0:hl], data1=gx.ap[:, 0:hl], initial=0.0,
                                                                            op0=ALU.mult, op1=ALU.add), [ga, gx], [(H, half)])
                else:
                    S.emit("dve", lambda e, h0=h0, hl=hl: e.tensor_tensor_scan(out=H.ap[:, h0:h0 + hl][:, ::-1], data0=ga.ap[:, 0:hl][:, ::-1], data1=gx.ap[:, 0:hl][:, ::-1],
                                                                            initial=0.0, op0=ALU.mult, op1=ALU.add), [ga, gx], [(H, half)])
                yield
            done[(nb, jc)] = done.get((nb, jc), 0) + 1

        def tail(nb, jc):
            c = 2 * nb + jc
            while done.get((nb, jc), 0) < 2:
                yield
            Hf, Hb = HFB[jc]
            S.dma("sp", lambda e: e.dma_start(out=GYt.ap, in_=GYd.ap[c]), [(GYd, c)], [GYt], "ld")
            yield
            S.emit("pool", lambda e: e.tensor_tensor(out=Hf.ap, in0=Hf.ap, in1=Hb.ap, op=ALU.add), [Hf, Hb], [Hf])
            for _ in range(16):
                yield
            S.emit("dve", lambda e: e.tensor_tensor(out=Zt.ap, in0=Hf.ap, in1=GYt.ap, op=ALU.mult), [Hf, GYt], [Zt])
            yield
            S.dma("sp", lambda e: e.dma_start(out=ZTd.ap[c], in_=Zt.ap), [Zt], [(ZTd, c)], "st")
            yield

        for nb in range(4):
            S.emit("pool", lambda e: e.memset(XRP.ap[:, :, 0:2], 0.0), [], [XRP])
            S.emit("pool", lambda e: e.memset(XRP.ap[:, :, L + 2:L + 4], 0.0), [], [XRP])
            for jc in range(2):
                c = 2 * nb + jc
                S.dma("sp", lambda e, jc=jc, c=c: e.dma_start(out=XRP.ap[:, jc, 2:L + 2], in_=XRd.ap[c]), [(XRd, c)], [(XRP, jc)], "ld")
                cw = lambda j, c=c: PAR.ap[:, P_CW + c * 4 + j:P_CW + c * 4 + j + 1]
                S.emit("dve", lambda e, jc=jc, c=c, cw=cw: e.tensor_scalar(out=XC.ap[:, jc, :], in0=XRP.ap[:, jc, 0:L], scalar1=cw(0),
                                                                          scalar2=PAR.ap[:, P_CB + c:P_CB + c + 1], op0=ALU.mult, op1=ALU.add),
                       [(XRP, jc), PAR], [(XC, jc)])
                for j in range(1, 4):
                    S.emit("dve", lambda e, jc=jc, j=j, cw=cw: e.scalar_tensor_tensor(out=XC.ap[:, jc, :], in0=XRP.ap[:, jc, j:j + L], scalar=cw(j),
                                                                                     in1=XC.ap[:, jc, :], op0=ALU.mult, op1=ALU.add),
                           [(XRP, jc), (XC, jc), PAR], [(XC, jc)])
                S.emit("act", lambda e, jc=jc: e.copy(out=XCB.ap[:, jc, :], in_=XC.ap[:, jc, :]), [(XC, jc)], [(XCB, jc)])
            zero_fill_xg(nb, 4)
            gens = []
            for jc in range(2):
                gens += [chain(nb, jc, 0), chain(nb, jc, 1), tail(nb, jc)]
            interleave(gens, 3, 6)
        A.reset(m)

    EPT = {}
    regs = {}

    def bcreg(e):
        if "bc" not in regs:
            regs["bc"] = e.to_reg(DUMP)
        return regs["bc"]

    def alloc_epilogue():
        LNG = A.alloc("lng", [D], F32)
        LNB = A.alloc("lnb", [D], F32)
        RBT = A.alloc("rbt", [NE], F32)
        RW = A.alloc("rw", [KC, NE], F32)
        Rt = [A.alloc("r%d" % i, [D], F32) for i in range(NBE)]
        Yt = [A.alloc("y%d" % i, [D], F32) for i in range(NBE)]
        XBt = [A.alloc("xb%d" % i, [D], BF16) for i in range(NBE)]
        YTt = [A.alloc("ytt%d" % i, [KC, 128], F32) for i in range(NBE)]
        SMALL = [A.alloc("sm%d" % i, [232 + 256], F32) for i in range(NBE)]
        MSKB = [A.alloc("mskb%d" % i, [NE], BF16) for i in range(NBE)]
        EPT["end"] = A.mark()
        EPT.update(LNG=LNG, LNB=LNB, RBT=RBT, RW=RW, Rt=Rt, Yt=Yt, XBt=XBt, YTt=YTt, SMALL=SMALL, MSKB=MSKB)

    ep_mark = A.mark()
    epi = [0]

    def load_ln_params(ln_idx, li=None):
        LNG, LNB, RBT, RW = EPT["LNG"], EPT["LNB"], EPT["RBT"], EPT["RW"]
        S.dma("sp", lambda e: e.dma_start(out=LNG.ap, in_=ROWSd.ap[2 * ln_idx:2 * ln_idx + 1, :].to_broadcast([128, D])), [], [LNG], "ld")
        S.dma("sp", lambda e: e.dma_start(out=LNB.ap, in_=ROWSd.ap[2 * ln_idx + 1:2 * ln_idx + 2, :].to_broadcast([128, D])), [], [LNB], "ld")
        if li is not None:
            S.dma("sp", lambda e: e.dma_start(out=RBT.ap, in_=RBd.ap[li:li + 1, :].to_broadcast([128, NE])), [], [RBT], "ld")
            S.dma("sp", lambda e: e.dma_start(out=RW.ap, in_=W_R.ap[li].rearrange("(k p) e -> p k e", p=128)), [], [RW], "ld")
            S.emit("pool", lambda e: e.memset(CNT.ap, 0.0), [], [CNT])

    def epilogue(ci, ps_pair, extra, hprev, hnext, route_li=None, h2t=None, out_rows=None):
        LNG, LNB, RBT, RW, Rt, Yt, XBt, YTt, SMALL, MSKB = (EPT[x] for x in ("LNG", "LNB", "RBT", "RW", "Rt", "Yt", "XBt", "YTt", "SMALL", "MSKB"))
        t0, n = CHUNKS[ci]
        k = epi[0] % NBE
        epi[0] += 1
        R, Y, XB, SM = Rt[k], Yt[k], XBt[k], SMALL[k]
        YTt, MSKB = YTt[k], MSKB[k]
        ST = SM.ap[:, 0:12].rearrange("p (a b) -> p a b", a=2)
        MV = SM.ap[:, 12:14]
        RS = SM.ap[:, 14:15]
        NMX = SM.ap[:, 15:16]
        o = 16
        LG = SM.ap[:, o:o + 32]
        MSK = SM.ap[:, o + 32:o + 64]
        EX = SM.ap[:, o + 64:o + 96]
        POS = SM.ap[:, o + 96:o + 128]
        V1 = SM.ap[:, o + 128:o + 160]
        OH = SM.ap[:, o + 160:o + 192]
        MX = SM.ap[:, o + 192:o + 200]
        SS = SM.ap[:, o + 200:o + 201]
        DK = SM.ap[:, o + 208:o + 212]
        JNK = SM.ap[:, 232:488]
        S.dma("sp", lambda e: e.dma_start(out=R.ap[0:n], in_=hprev.ap[t0:t0 + n, :]), [(hprev, ci)], [R], "ld")
        yield
        for h in range(2):
            S.emit("dve", lambda e, h=h: e.scalar_tensor_tensor(out=R.ap[0:n, h * 512:(h + 1) * 512], in0=R.ap[0:n, h * 512:(h + 1) * 512], scalar=ALPHA,
                                                               in1=ps_pair[h].ap[0:n, :], op0=ALU.mult, op1=ALU.add), [R, ps_pair[h]], [R])
            yield
        ps_put(*ps_pair)
        if extra is not None:
            for kk in range(4):
                S.emit("dve", lambda e, kk=kk: e.scalar_tensor_tensor(out=R.ap[0:n], in0=extra.ap[0:n, kk, :], scalar=GK.ap[0:n, ci, kk:kk + 1], in1=R.ap[0:n],
                                                                     op0=ALU.mult, op1=ALU.add), [R, (extra, kk), (GK, ci)], [R])
                yield
        for h in range(2):
            S.emit("dve", lambda e, h=h: e.bn_stats(out=ST[0:n, h, :], in_=R.ap[0:n, h * 512:(h + 1) * 512]), [R], [(SM, "st%d" % h)])
            yield
        S.emit("dve", lambda e: e.bn_aggr(out=MV[0:n], in_=SM.ap[0:n, 0:12]), [(SM, "st0"), (SM, "st1")], [(SM, "mv")])
        yield
        S.emit("act", lambda e: e.activation(out=RS[0:n], in_=MV[0:n, 1:2], func=AF.Ln, bias=EPS, scale=1.0), [(SM, "mv")], [(SM, "rs")])
        yield
        S.emit("act", lambda e: e.activation(out=RS[0:n], in_=RS[0:n], func=AF.Exp, scale=-0.5), [(SM, "rs")], [(SM, "rs")])
        yield
        S.emit("dve", lambda e: e.scalar_tensor_tensor(out=Y.ap[0:n], in0=R.ap[0:n], scalar=MV[0:n, 0:1], in1=LNG.ap[0:n], op0=ALU.subtract, op1=ALU.mult),
               [R, (SM, "mv"), LNG], [Y])
        yield
        S.emit("dve", lambda e: e.scalar_tensor_tensor(out=Y.ap[0:n], in0=Y.ap[0:n], scalar=RS[0:n], in1=LNB.ap[0:n], op0=ALU.mult, op1=ALU.add),
               [Y, (SM, "rs"), LNB], [Y])
        yield
        if hnext is not None:
            S.dma("pool", lambda e: e.dma_start(out=hnext.ap[t0:t0 + n, :], in_=Y.ap[0:n]), [Y], [(hnext, ci)], "stp")
            yield
        if out_rows is not None:
            S.dma("pool", lambda e: e.dma_start(out=OUT.ap[out_rows:out_rows + n, :], in_=Y.ap[0:n]), [Y], [(OUT, ci)], "stp")
            yield
        if route_li is None and h2t is None:
            return
        pt = ((yield from ps_get()), (yield from ps_get()))
        for kk in range(KC):
            p = pt[kk // 4]
            S.emit("pe", lambda e, p=p, kk=kk: e.transpose(p.ap[:, (kk % 4) * 128:(kk % 4) * 128 + n], Y.ap[0:n, kk * 128:(kk + 1) * 128], IDF[0:n, 0:n]),
                   [Y, CF], [p], kk % 4 == 3)
            yield
        if h2t is not None:
            for hh in range(2):
                S.emit("act", lambda e, hh=hh: e.copy(out=h2t.ap[:, hh * 4:(hh + 1) * 4, t0:t0 + n],
                                                       in_=pt[hh].ap.rearrange("p (a b) -> p a b", a=4)[:, :, 0:n]), [pt[hh]], [(h2t, ci)])
                yield
        if route_li is None:
            ps_put(*pt)
            return
        li = route_li
        for hh in range(2):
            S.emit("act", lambda e, hh=hh: e.copy(out=YTt.ap[:, hh * 4:(hh + 1) * 4, 0:n], in_=pt[hh].ap.rearrange("p (a b) -> p a b", a=4)[:, :, 0:n]),
                   [pt[hh]], [(YTt, hh)])
            yield
        ps_put(*pt)
        S.emit("act", lambda e: e.copy(out=XB.ap[0:n], in_=Y.ap[0:n]), [Y], [XB])
        yield
        pl = yield from ps_get()
        for kk in range(KC):
            mm(pl.ap[0:n, 0:NE], YTt.ap[:, kk, 0:n], RW.ap[:, kk, :], kk == 0, kk == KC - 1, [YTt, RW], [pl], kk == KC - 1)
            yield
        S.emit("dve", lambda e: e.tensor_tensor(out=LG[0:n], in0=pl.ap[0:n, 0:NE], in1=RBT.ap[0:n], op=ALU.add), [pl, RBT], [(SM, "lg")])
        yield
        ps_put(pl)
        S.emit("dve", lambda e: e.max(out=MX[0:n], in_=LG[0:n]), [(SM, "lg")], [(SM, "mx")])
        yield
        S.emit("dve", lambda e: e.tensor_scalar(out=MSK[0:n], in0=LG[0:n], scalar1=MX[0:n, 3:4], scalar2=None, op0=ALU.is_ge), [(SM, "lg"), (SM, "mx")], [(SM, "msk")])
        yield
        S.emit("dve", lambda e: e.tensor_scalar(out=NMX[0:n], in0=MX[0:n, 0:1], scalar1=-1.0, scalar2=None, op0=ALU.mult), [(SM, "mx")], [(SM, "nmx")])
        yield
        S.emit("act", lambda e: e.activation(out=EX[0:n], in_=LG[0:n], func=AF.Exp, bias=NMX[0:n], scale=1.0), [(SM, "lg"), (SM, "nmx")], [(SM, "ex")])
        yield
        S.emit("dve", lambda e: e.tensor_tensor(out=EX[0:n], in0=EX[0:n], in1=MSK[0:n], op=ALU.mult), [(SM, "ex"), (SM, "msk")], [(SM, "ex")])
        yield
        S.emit("dve", lambda e: e.tensor_reduce(out=SS[0:n], in_=EX[0:n], axis=AX.X, op=ALU.add), [(SM, "ex")], [(SM, "ss")])
        yield
        S.emit("dve", lambda e: e.reciprocal(out=SS[0:n], in_=SS[0:n]), [(SM, "ss")], [(SM, "ss")])
        yield
        S.emit("dve", lambda e: e.tensor_scalar(out=GALL.ap[0:n, ci, :], in0=EX[0:n], scalar1=SS[0:n], scalar2=None, op0=ALU.mult),
               [(SM, "ex"), (SM, "ss")], [(GALL, ci)])
        yield
        S.emit("act", lambda e: e.copy(out=MSKB.ap[0:n], in_=MSK[0:n]), [(SM, "msk")], [MSKB])
        yield
        pp = yield from ps_get()
        mm(pp.ap[0:n, 0:NE], UTRI[0:n, 0:n], MSKB.ap[0:n], True, True, [MSKB, CB], [pp], False)
        yield
        mm(pp.ap[:, NE:2 * NE], ONESB[0:n, :], MSKB.ap[0:n], True, True, [MSKB, CB], [pp], True)
        yield
        S.emit("dve", lambda e: e.tensor_tensor(out=POS[0:n], in0=pp.ap[0:n, 0:NE], in1=CNT.ap[0:n], op=ALU.add), [pp, CNT], [(SM, "pos")])
        S.emit("dve", lambda e: e.tensor_tensor(out=CNT.ap, in0=pp.ap[:, NE:2 * NE], in1=CNT.ap, op=ALU.add), [pp, CNT, (SM, "pos")], [CNT])
        yield
        ps_put(pp)
        S.emit("dve", lambda e: e.tensor_tensor(out=V1[0:n], in0=POS[0:n], in1=EOFFM[0:n], op=ALU.add), [(SM, "pos"), CF], [(SM, "v1")])
        yield
        S.emit("dve", lambda e: e.tensor_scalar(out=POS[0:n], in0=POS[0:n], scalar1=float(CAP), scalar2=None, op0=ALU.is_lt), [(SM, "pos"), (SM, "v1")], [(SM, "pos")])
        yield
        S.emit("dve", lambda e: e.tensor_tensor(out=V1[0:n], in0=V1[0:n], in1=POS[0:n], op=ALU.mult), [(SM, "pos"), (SM, "v1")], [(SM, "v1")])
        yield
        for kk in range(4):
            j0 = JNK[:, 64 * kk:64 * kk + 32]
            j1 = JNK[:, 64 * kk + 32:64 * kk + 64]
            S.emit("dve", lambda e, kk=kk, j0=j0: e.scalar_tensor_tensor(out=j0[0:n], in0=LG[0:n], scalar=MX[0:n, kk:kk + 1], in1=V1[0:n], op0=ALU.is_equal, op1=ALU.mult,
                                                                      accum_out=DK[0:n, kk:kk + 1]), [(SM, "lg"), (SM, "mx"), (SM, "v1")], [(SM, "dk%d" % kk)])
            yield
            S.emit("dve", lambda e, kk=kk, j1=j1: e.scalar_tensor_tensor(out=j1[0:n], in0=LG[0:n], scalar=MX[0:n, kk:kk + 1], in1=GALL.ap[0:n, ci, :], op0=ALU.is_equal,
                                                                      op1=ALU.mult, accum_out=GK.ap[0:n, ci, kk:kk + 1]), [(SM, "lg"), (SM, "mx"), (GALL, ci)], [(GK, ci)])
            yield
        S.emit("dve", lambda e: e.tensor_scalar(out=DI.ap[0:n, ci, :], in0=DK[0:n], scalar1=float(DUMP), scalar2=None, op0=ALU.add), [(SM, "dk0"), (SM, "dk1"), (SM, "dk2"), (SM, "dk3")], [(DI, ci)])
        yield
        for kk in range(4):
            S.dma("pool", lambda e, kk=kk: e.indirect_dma_start(out=XG.ap, out_offset=bass.IndirectOffsetOnAxis(ap=DI.ap[0:n, ci, kk:kk + 1], axis=0),
                                                                in_=XB.ap[0:n], in_offset=None, bounds_check=bcreg(e), oob_is_err=False),
                  [XB, (DI, ci)], [(XG, "sc")], "ind", 8)
            yield


    def interleave(gens, width, stagger=0):
        gens = list(gens)
        active = []
        nxt = 0
        since = stagger
        while active or nxt < len(gens):
            if len(active) < width and nxt < len(gens) and (since >= stagger or not active):
                active.append(gens[nxt])
                nxt += 1
                since = 0
            since += 1
            for g in list(active):
                try:
                    next(g)
                except StopIteration:
                    active.remove(g)

    def phase_1c():
        m = A.mark()
        wo = A.alloc("wo", [KC, D], BF16)
        zg = [A.alloc("zg%d" % i, [KC, 512], BF16) for i in range(3)]
        for kc in range(KC):
            S.dma("pool", lambda e, kc=kc: e.dma_start(out=wo.ap[:, kc, :], in_=W_LO.ap[kc * 128:(kc + 1) * 128, :]), [], [(wo, kc)], "ldc", 8)
        load_ln_params(0, 0)
        def chunk_gen(ci, z, s_, n):
            pp = ((yield from ps_get()), (yield from ps_get()))
            for h in range(2):
                for kc in range(KC):
                    mm(pp[h].ap[0:n, :], z.ap[:, kc, s_ * 128:s_ * 128 + n], wo.ap[:, kc, h * 512:(h + 1) * 512], kc == 0, kc == KC - 1,
                       [z, (wo, kc)], [pp[h]], kc == KC - 1)
                yield
            yield from epilogue(ci, pp, None, H0, H1, route_li=0)

        gens = []
        ci = 0
        for gi, (g0, gn) in enumerate(GROUPS):
            z = zg[gi % 3]
            first = True
            for s_ in range(max(1, gn // 128)):
                def g_(ci=ci, z=z, s_=s_, n=min(128, gn), first=first, g0=g0, gn=gn):
                    if first:
                        S.dma("sp", lambda e: e.dma_start(out=z.ap[:, :, 0:gn], in_=ZTd.ap[:, :, g0:g0 + gn].rearrange("k p t -> p k t")), [ZTd], [z], "ld")
                    yield from chunk_gen(ci, z, s_, n)
                gens.append(g_())
                first = False
                ci += 1
        interleave(gens, NBE, 25)
        A.reset(m)


    def phase_moe(li, hprev, hnext, ln_idx, chunk_ids, h2t=None, to_out=False):
        m = A.mark()
        A.reset(base_mark)
        WG = [A.alloc("wg%d" % i, [KC, 2 * D], BF16) for i in range(2)]
        WD = [A.alloc("wd%d" % i, [KC, D], BF16) for i in range(2)]
        XT = [A.alloc("xt%d" % i, [KC, CAP], BF16) for i in range(2)]
        ACTT = [A.alloc("actt%d" % i, [KC, CAP], BF16) for i in range(2)]
        XS = A.alloc("xs", [NSC, D], BF16)
        YS = [A.alloc("ys%d" % i, [D], F32) for i in range(2)]
        HN = CAP // 2
        NT = 3
        TT = [[A.alloc("t%d_%d" % (j, i), [HN], F32) for j in range(3)] for i in range(NT)]
        BG17 = A.alloc("bg17", [NE, 8], F32)
        bgu0 = (P_BGU0, P_BGU1)[li]
        SILU_C = 11.914 / (1.0 + float(np.exp(-11.914)))
        S.emit("dve", lambda en: en.tensor_scalar(out=BG17.ap, in0=PAR.ap[:, bgu0:bgu0 + 512].rearrange("p (e f) -> p e f", e=NE)[:, :, 0:8],
                                                  scalar1=1.702, scalar2=None, op0=ALU.mult), [PAR], [BG17])
        GUB = [PS[0:2], PS[2:4]]
        OTB = PS[4:8]
        oti = [0]

        def next_ot():
            p = OTB[oti[0] % 4]
            oti[0] += 1
            return p

        def load_wg(e):
            sl = e % 2
            for kc in range(KC):
                S.dma("pool", lambda en, kc=kc: en.dma_start(out=WG[sl].ap[:, kc, :], in_=W_GU.ap[li, e, kc * 128:(kc + 1) * 128, :]),
                      [], [(WG[sl], kc)], "ldw", 8)

        def load_wd(e):
            sl = e % 2
            for kc in range(KC):
                S.dma("pool", lambda en, kc=kc: en.dma_start(out=WD[sl].ap[:, kc, :], in_=W_DN.ap[li, e, kc * 128:(kc + 1) * 128, :]),
                      [], [(WD[sl], kc)], "ldw", 8)

        def load_xs(e):
            nf = CAP // 128
            S.dma("sp", lambda en: en.dma_start(out=XS.ap[:, 0:nf, :], in_=XG.ap[e * CAP:e * CAP + nf * 128, :].rearrange("(s p) d -> p s d", p=128)), [XG], [(XS, 0)], "ld")
            if CAP % 128:
                S.dma("sp", lambda en: en.dma_start(out=XS.ap[0:CAP % 128, nf, :], in_=XG.ap[e * CAP + nf * 128:(e + 1) * CAP, :]), [XG], [(XS, 1)], "ld")

        def transp(e):
            X = XT[e % 2]
            for k in range(KC):
                pt = next_ot()
                ptb = pt.ap.bitcast(BF16)
                for sc, (s0, sn) in enumerate(SLOTCH):
                    S.emit("pe", lambda en, ptb=ptb, sc=sc, k=k, s0=s0, sn=sn: en.transpose(ptb[:, s0:s0 + sn], XS.ap[0:sn, sc, k * 128:(k + 1) * 128], IDB[0:sn, 0:sn]),
                           [XS, CB], [pt], sc == NSC - 1)
                if k % 2 == 0:
                    S.emit("act", lambda en, ptb=ptb, k=k: en.copy(out=X.ap[:, k, :], in_=ptb[:, 0:CAP]), [pt], [(X, k)])
                else:
                    S.emit("dve", lambda en, ptb=ptb, k=k: en.tensor_copy(out=X.ap[:, k, :], in_=ptb[:, 0:CAP]), [pt], [(X, k)])

        tix = [0]

        def gate_up(e):
            sl = e % 2
            X, AC = XT[sl], ACTT[sl]
            for f in range(KC):
                for half in range(2):
                    hs = half * HN
                    SI, T3, SMt = TT[tix[0] % NT]
                    pg, pl = GUB[tix[0] % 2]
                    tix[0] += 1
                    for k in range(KC):
                        mm(pg.ap[:, 0:HN], WG[sl].ap[:, k, f * 128:(f + 1) * 128], X.ap[:, k, hs:hs + HN], k == 0, k == KC - 1,
                           [(WG[sl], k), (X, k)], [pg], k == KC - 1)
                    for k in range(KC):
                        mm(pl.ap[:, 0:HN], WG[sl].ap[:, k, D + f * 128:D + (f + 1) * 128], X.ap[:, k, hs:hs + HN], k == 0, k == KC - 1,
                           [(WG[sl], k), (X, k)], [pl], k == KC - 1)
                    bg = BG17.ap[:, e, f:f + 1]
                    bl = BL1.ap[:, li, e, f:f + 1]
                    S.emit("act", lambda en, SI=SI, pg=pg, bg=bg: en.activation(out=SI.ap, in_=pg.ap[:, 0:HN], func=AF.Silu, bias=bg, scale=1.702), [pg, BG17], [SI])
                    S.emit("dve", lambda en, T3=T3, pl=pl, bl=bl: en.tensor_scalar(out=T3.ap, in0=pl.ap[:, 0:HN], scalar1=bl, scalar2=-6.0, op0=ALU.add, op1=ALU.max),
                           [pl, BL1], [T3])
                    S.emit("dve", lambda en, SI=SI, SMt=SMt: en.tensor_scalar(out=SMt.ap, in0=SI.ap, scalar1=SILU_C, scalar2=1.0 / 1.702, op0=ALU.min, op1=ALU.mult),
                           [SI], [SMt])
                    S.emit("dve", lambda en, T3=T3, SMt=SMt, f=f, hs=hs: en.scalar_tensor_tensor(out=AC.ap[:, f, hs:hs + HN], in0=T3.ap, scalar=8.0, in1=SMt.ap,
                                                                                           op0=ALU.min, op1=ALU.mult), [T3, SMt], [(AC, (f, half))])

        def down(e):
            sl = e % 2
            AC = ACTT[sl]
            for sc, (s0, sn) in enumerate(SLOTCH):
                Y_ = YS[sc % 2]
                for dh in range(2):
                    pd = next_ot()
                    for f in range(KC):
                        mm(pd.ap[0:sn, :], AC.ap[:, f, s0:s0 + sn], WD[sl].ap[:, f, dh * 512:(dh + 1) * 512], f == 0, f == KC - 1,
                           [AC, (WD[sl], f)], [pd], f == KC - 1)
                    if dh == 0:
                        S.emit("act", lambda en, Y_=Y_, pd=pd, sn=sn: en.copy(out=Y_.ap[0:sn, 0:512], in_=pd.ap[0:sn, :]), [pd], [(Y_, 0)])
                    else:
                        S.emit("dve", lambda en, Y_=Y_, pd=pd, sn=sn: en.tensor_copy(out=Y_.ap[0:sn, 512:1024], in_=pd.ap[0:sn, :]), [pd], [(Y_, 1)])
                r0 = e * CAP + s0
                S.dma("sp", lambda en, Y_=Y_, r0=r0, sn=sn: en.dma_start(out=YG.ap[r0:r0 + sn, :], in_=Y_.ap[0:sn]), [Y_], [(YG, "y")], "st")

        load_wg(0)
        load_wd(0)
        load_xs(0)
        transp(0)
        load_xs(1)
        for e in range(NE):
            if e + 1 < NE:
                load_wg(e + 1)
                transp(e + 1)
                if e + 2 < NE:
                    load_xs(e + 2)
            gate_up(e)
            if e >= 1:
                down(e - 1)
            if e + 1 < NE:
                load_wd(e + 1)
        down(NE - 1)
        A.reset(m)
        pre = A.mark()
        if h2t:
            h2t = A.alloc("h2t", [KC, L], BF16)
        m = A.mark()
        YGa = [A.alloc("yga%d" % i, [4, D], F32) for i in range(NB)]
        BDN = A.alloc("bdn", [D], F32)
        GT = [A.alloc("gt%d" % i, [128], F32) for i in range(NB)]
        S.dma("sp", lambda en: en.dma_start(out=BDN.ap[0:NE], in_=B_DN.ap[li]), [], [BDN], "ld")
        load_ln_params(ln_idx, None)
        def comb_gen(it, ci):
            t0, n = CHUNKS[ci]
            Yg = YGa[it % NB]
            for kk in range(4):
                S.dma("pool", lambda en, kk=kk: en.indirect_dma_start(out=Yg.ap[0:n, kk, :], out_offset=None, in_=YG.ap,
                                                                   in_offset=bass.IndirectOffsetOnAxis(ap=DI.ap[0:n, ci, kk:kk + 1], axis=0),
                                                                   bounds_check=bcreg(en), oob_is_err=False),
                      [YG, (DI, ci)], [(Yg, kk)], "ind", 8)
            yield
            pg_ = yield from ps_get()
            S.emit("pe", lambda en: en.transpose(pg_.ap[0:NE, 0:n], GALL.ap[0:n, ci, :], IDF[0:n, 0:n]), [(GALL, ci), CF], [pg_], True)
            yield
            S.emit("act", lambda en: en.copy(out=GT[it % NB].ap[0:NE, 0:n], in_=pg_.ap[0:NE, 0:n]), [pg_], [GT[it % NB]])
            yield
            ps_put(pg_)
            pp = ((yield from ps_get()), (yield from ps_get()))
            for h in range(2):
                mm(pp[h].ap[0:n, :], GT[it % NB].ap[0:NE, 0:n], BDN.ap[0:NE, h * 512:(h + 1) * 512], True, True, [GT[it % NB], BDN], [pp[h]], True)
            yield
            yield from epilogue(ci, pp, Yg, hprev, hnext, route_li=None, h2t=(h2t or None), out_rows=((ci - 1) * 128 if to_out else None))

        interleave([comb_gen(it, ci) for it, ci in enumerate(chunk_ids)], NB, 10)
        A.reset(m)
        return h2t, pre


    def phase_na(H2T):
        m = A.mark()
        A.reset(base_mark)
        WQ = [A.alloc("wq%d" % i, [KC, 3, 128], BF16) for i in range(2)]
        QT = A.alloc("qt", [SEQ], BF16)
        KT = A.alloc("kt", [L], BF16)
        VA = A.alloc("va", [33, 2, 65], BF16)
        assert A.mark() <= EPT["end"]
        A.reset(m)
        ATT = [A.alloc("att%d" % i, [32, 128], BF16) for i in range(2)]
        E2 = A.alloc("e2", [18, 64], F32)
        E2b = A.alloc("e2b", [18, 64], BF16)
        E2c = A.alloc("e2c", [18, 64], BF16)
        EB = [[A.alloc("eb%d_%d" % (i, p), [5, 128], BF16) for p in range(5)] for i in range(2)]
        PT = [A.alloc("pt%d" % i, [6, 128], BF16) for i in range(8)]
        RC = [A.alloc("rc%d" % i, [1], F32) for i in range(8)]
        EMB = A.alloc("emb", [2], F32)
        S.emit("pool", lambda e: e.memset(VA.ap[:, :, :, 64:65], 1.0), [], [VA])

        def rs(qr):
            return min(max(qr - 4, 0), 56)

        def pat_of(rp):
            return {0: 0, 1: 1, 30: 3, 31: 4}.get(rp, 2)

        pat_rp = [0, 1, 2, 30, 31]

        def load_wq(hp):
            for j in range(3):
                S.dma("pool", lambda e, j=j, hp=hp: e.dma_start(out=WQ[hp % 2].ap[:, :, j, :],
                                                                in_=W_QKV.ap[:, j * D + hp * 128:j * D + (hp + 1) * 128].rearrange("(k p) c -> p k c", p=128)),
                      [], [(WQ[hp % 2], j)], "ldc", 8)

        load_wq(0)
        it = 0
        for hp in range(8):
            W = WQ[hp % 2]
            if hp + 1 < 8:
                load_wq(hp + 1)
            for g in range(8):
                ps = next_ps()
                for kc in range(KC):
                    mm(ps.ap, W.ap[:, kc, 0, :], H2T.ap[:, kc, 16 + 512 * g:16 + 512 * (g + 1)], kc == 0, kc == KC - 1, [(W, 0), H2T], [ps], kc == KC - 1)
                S.emit("act", lambda e, ps=ps, g=g: e.mul(QT.ap[:, 512 * g:512 * (g + 1)], ps.ap, 0.125), [ps], [(QT, g)])
            for gi, (t0, n) in enumerate(GROUPS):
                ps = next_ps()
                for kc in range(KC):
                    mm(ps.ap[:, 0:n], W.ap[:, kc, 1, :], H2T.ap[:, kc, t0:t0 + n], kc == 0, kc == KC - 1, [(W, 1), H2T], [ps], kc == KC - 1)
                S.emit("dve", lambda e, ps=ps, t0=t0, n=n: e.tensor_copy(out=KT.ap[:, t0:t0 + n], in_=ps.ap[:, 0:n]), [ps], [(KT, gi)])
            for ci, (t0, n) in enumerate(CHUNKS):
                ps = next_ps()
                for kc in range(KC):
                    mm(ps.ap[0:n, 0:128], H2T.ap[:, kc, t0:t0 + n], W.ap[:, kc, 2, :], kc == 0, kc == KC - 1, [(W, 2), H2T], [ps], kc == KC - 1)
                eng = ("act", "dve")[ci % 2]
                if eng == "act":
                    S.emit("act", lambda e, ps=ps, ci=ci, n=n: e.copy(out=VA.ap[0:n, ci, :, 0:64], in_=ps.ap[0:n, 0:128].rearrange("p (a b) -> p a b", a=2)),
                           [ps], [(VA, ci)])
                else:
                    S.emit("dve", lambda e, ps=ps, ci=ci, n=n: e.tensor_copy(out=VA.ap[0:n, ci, :, 0:64], in_=ps.ap[0:n, 0:128].rearrange("p (a b) -> p a b", a=2)),
                           [ps], [(VA, ci)])
            AT_ = ATT[hp % 2]
            for hh in range(2):
                h = 2 * hp + hh
                r0 = 64 * hh
                EBh = EB[hh]
                for rho in range(2):
                    src = bass.AP(RPd.ap.tensor, h * 19 * 127 + rho * 127, [[1, 64], [127, 18], [1, 64]])
                    S.dma("sp", lambda e, rho=rho, src=src: e.dma_start(out=E2.ap[64 * rho:64 * rho + 64], in_=src), [], [(E2, rho)], "ld")
                S.emit("act", lambda e: e.activation(out=E2.ap, in_=E2.ap, func=AF.Exp), [E2], [E2])
                S.emit("dve", lambda e: e.tensor_tensor(out=E2c.ap, in0=E2.ap, in1=CMASK.unsqueeze(1).to_broadcast([128, 18, 64]), op=ALU.mult), [E2, CF], [E2c])
                for j in range(3):
                    pf = next_ps()
                    mm(pf.ap[:, 0:384], J2, E2c.ap.rearrange("p a b -> p (a b)")[:, 384 * j:384 * (j + 1)], True, True, [E2c, CB], [pf], True)
                    S.emit(("act", "dve")[j % 2], lambda e, pf=pf, j=j: (e.copy if j % 2 == 0 else e.tensor_copy)(
                        out=E2b.ap.rearrange("p a b -> p (a b)")[:, 384 * j:384 * (j + 1)], in_=pf.ap[:, 0:384]), [pf], [(E2b, j)])
                for p_i, rp in enumerate(pat_rp):
                    jp0 = min(max(rp - 2, 0), 27)
                    T = EBh[p_i]
                    q = 0
                    for jpi in range(5):
                        for rho in range(2):
                            qr = 2 * rp + rho
                            kr0 = 2 * (jp0 + jpi)
                            di = kr0 - qr + 9
                            v0 = rs(qr) <= kr0 <= rs(qr) + 7
                            v1 = rs(qr) <= kr0 + 1 <= rs(qr) + 7
                            dst = T.ap[:, jpi, 64 * rho:64 * rho + 64]
                            eng = ("pool", "dve")[q % 2]
                            q += 1
                            if v0 or v1:
                                assert 0 <= di <= 17, (rp, jpi, rho, di)
                                S.emit(eng, lambda e, dst=dst, di=di: e.tensor_copy(out=dst, in_=E2b.ap[:, di, :]), [E2b], [(T, (jpi, rho))])
                                if not v0:
                                    S.emit(eng, lambda e, dst=dst: e.memset(dst[0:64], 0.0), [], [(T, (jpi, rho))])
                                if not v1:
                                    S.emit(eng, lambda e, dst=dst: e.memset(dst[64:128], 0.0), [], [(T, (jpi, rho))])
                            else:
                                S.emit(eng, lambda e, dst=dst: e.memset(dst, 0.0), [], [(T, (jpi, rho))])
                S.emit("act", lambda e, h=h, hh=hh: e.activation(out=EMB.ap[0:16, hh:hh + 1], in_=PAR.ap[0:16, P_MB + h:P_MB + h + 1], func=AF.Exp), [PAR], [(EMB, hh)])
                S.emit("pool", lambda e, hh=hh: e.memset(VA.ap[0:16, 0, hh, 64:65], 1.0), [(VA, 0)], [(VA, 0)])
                S.emit("dve", lambda e, hh=hh: e.tensor_scalar(out=VA.ap[0:16, 0, hh, :], in0=VA.ap[0:16, 0, hh, :], scalar1=EMB.ap[0:16, hh:hh + 1], scalar2=None,
                                                               op0=ALU.mult), [(VA, 0), (EMB, hh)], [(VA, 0)])

            def na_gen(hh, rp, slot, AT_):
                r0 = 64 * hh
                jp0 = min(max(rp - 2, 0), 27)
                T = EB[hh][pat_of(rp)]
                P_, Rc = PT[slot], RC[slot]
                psA = yield from ps_get()
                psB = yield from ps_get()
                qs = QT.ap[r0:r0 + 64, 128 * rp:128 * (rp + 1)]
                for jpi in range(5):
                    k0 = 16 + 128 * (jp0 + jpi)
                    o = psA.ap[:, 128 * jpi:128 * (jpi + 1)] if jpi < 4 else psB.ap[:, 0:128]
                    mm(o, KT.ap[r0:r0 + 64, k0:k0 + 128], qs, True, True, [KT, QT], [psA if jpi < 4 else psB], jpi == 3)
                mm(psB.ap[:, 128:256], KT.ap[r0:r0 + 64, 0:128], qs, True, True, [KT, QT], [psB], True)
                yield
                S.emit("act", lambda e: e.activation(out=P_.ap[:, 0:4, :], in_=psA.ap.rearrange("p (a b) -> p a b", a=4), func=AF.Exp), [psA], [(P_, 0)])
                S.emit("act", lambda e: e.activation(out=P_.ap[:, 4:6, :], in_=psB.ap[:, 0:256].rearrange("p (a b) -> p a b", a=2), func=AF.Exp), [psB], [(P_, 1)])
                ps_put(psA, psB)
                yield
                S.emit("dve", lambda e: e.tensor_tensor(out=P_.ap[:, 0:5, :], in0=P_.ap[:, 0:5, :], in1=T.ap, op=ALU.mult), [P_, T], [P_])
                yield
                po = yield from ps_get()
                for jpi in range(5):
                    mm(po.ap[:, 0:65], P_.ap[:, jpi, :], VA.ap[:, 1 + jp0 + jpi, hh, :], jpi == 0, False, [P_, VA], [po], False)
                mm(po.ap[:, 0:65], P_.ap[0:16, 5, :], VA.ap[0:16, 0, hh, :], False, True, [P_, VA], [po], True)
                yield
                S.emit("dve", lambda e: e.reciprocal(out=Rc.ap, in_=po.ap[:, 64:65]), [po], [Rc])
                yield
                S.emit("dve", lambda e: e.tensor_scalar(out=AT_.ap[:, rp, 64 * hh:64 * hh + 64], in0=po.ap[:, 0:64], scalar1=Rc.ap,
                                                        scalar2=None, op0=ALU.mult), [po, Rc], [(AT_, (rp, hh))])
                ps_put(po)
                yield

            def na_chain(hh, par, AT_=AT_):
                for j, rp in enumerate(range(par, 32, 2)):
                    yield from na_gen(hh, rp, (hh * 2 + par) * 2 + j % 2, AT_)

            interleave([na_chain(hh, par) for hh in range(2) for par in range(2)], 4, 1)
            S.dma("sp", lambda e, AT_=AT_, hp=hp: e.dma_start(out=ATTD.ap[:, hp * 128:(hp + 1) * 128].rearrange("(r p) c -> p r c", p=128), in_=AT_.ap),
                  [AT_], [(ATTD, hp)], "st")
        A.reset(m)
        m = A.mark()
        WNO = A.alloc("wno", [KC, D], BF16)
        ATt = [A.alloc("att_in%d" % i, [D], BF16) for i in range(NBE)]
        ATk = [A.alloc("atk%d" % i, [KC, 128], BF16) for i in range(NBE)]
        for kc in range(KC):
            S.dma("pool", lambda e, kc=kc: e.dma_start(out=WNO.ap[:, kc, :], in_=W_NO.ap[kc * 128:(kc + 1) * 128, :]), [], [(WNO, kc)], "ldc", 8)
        load_ln_params(2, 1)
        def no_gen(ci):
            a_in, a_k = ATt[ci % NBE], ATk[ci % NBE]
            S.dma("sp", lambda e: e.dma_start(out=a_in.ap, in_=ATTD.ap[(ci - 1) * 128:ci * 128, :]), [ATTD], [a_in], "ld")
            yield
            pt = yield from ps_get()
            ptb = pt.ap.bitcast(BF16)
            for k in range(KC):
                S.emit("pe", lambda e, k=k: e.transpose(ptb[:, k * 128:(k + 1) * 128], a_in.ap[:, k * 128:(k + 1) * 128], IDB), [a_in, CB], [pt], k == KC - 1)
            yield
            S.emit("act", lambda e: e.copy(out=a_k.ap, in_=ptb.rearrange("p (a b) -> p a b", a=KC)), [pt], [a_k])
            yield
            ps_put(pt)
            pp = ((yield from ps_get()), (yield from ps_get()))
            for h in range(2):
                for kc in range(KC):
                    mm(pp[h].ap, a_k.ap[:, kc, :], WNO.ap[:, kc, h * 512:(h + 1) * 512], kc == 0, kc == KC - 1, [a_k, (WNO, kc)], [pp[h]], kc == KC - 1)
                yield
            yield from epilogue(ci, pp, None, H2, H3, route_li=1)

        interleave([no_gen(ci) for ci in range(1, 33)], NBE, 25)
        A.reset(m)

    def zero_fill_xg(part, nparts):
        nz = (DUMP + 1) // 1024
        for i in range(nz):
            if i % nparts == part:
                S.dma("sp", lambda e, i=i: e.dma_start(out=XG.ap[i * 1024:(i + 1) * 1024, :], in_=ZROWS.ap), [], [(XG, "sc")], "zf", 8)
        rem = DUMP + 1 - nz * 1024
        if rem and part == 0:
            S.dma("sp", lambda e: e.dma_start(out=XG.ap[nz * 1024:DUMP + 1, :], in_=ZROWS.ap[0:rem, :]), [], [(XG, "sc")], "zf", 8)

    zero_m = A.mark()
    ZR = A.alloc("zr", [D], F32)
    S.emit("pool", lambda e: e.memset(ZR.ap, 0.0), [], [ZR])
    S.dma("sp", lambda e: e.dma_start(out=YG.ap[DUMP:DUMP + 1, :], in_=ZR.ap[0:1, :]), [ZR], [(YG, "dump")], "st")
    A.reset(zero_m)

    phase_1a()
    phase_1b()
    if stop != "1b":
        alloc_epilogue()
        phase_1c()
        H2T_, pre_ = phase_moe(0, H1, H2, 1, list(range(33)), h2t=True)
        phase_na(H2T_)
        A.reset(pre_)
        phase_moe(1, H3, None, 3, list(range(1, 33)), to_out=True)

    outs = [OUT] if stop is None else [ZTd]
    if debug and stop is None:
        outs += [H1, H2, H3, ATTD]
    S.final_wait("sp", outs)
    S.check()
    with nc.Block() as block:
        S.build(block)
    es.close()
    return nc, in_names


def host_inputs(inp, b):
    f = np.float32
    x = np.asarray(inp["x"][b], f)
    h0 = np.ascontiguousarray(np.concatenate([np.asarray(inp["meta_tokens"], f), x], axis=0))
    d = {"h0": h0, "h0T": np.ascontiguousarray(h0.T)}
    return d


def shared_inputs(inp):
    f = np.float32
    par = np.zeros((128, NPAR), f)
    cw = np.asarray(inp["lru_conv_w"][0], f)
    par[:, P_CW:P_CW + 32] = cw.reshape(4, 8, 128).transpose(2, 1, 0).reshape(128, 32)
    par[:, P_CB:P_CB + 8] = np.asarray(inp["lru_conv_b"][0], f).reshape(8, 128).T
    for name, col in (("lru_ba", P_BA), ("lru_bx", P_BX), ("lru_lambda", P_LAM)):
        par[:, col:col + 16] = np.asarray(inp[name][0], f).reshape(2, 8, 128).transpose(2, 0, 1).reshape(128, 16)
    for li, col in ((0, P_BGU0), (1, P_BGU1)):
        par[:, col:col + 512] = np.asarray(inp["moe_b_gu"][li], f).reshape(NE, 16, 128).transpose(2, 0, 1).reshape(128, 512)
    par[0:16, P_MB:P_MB + 16] = np.asarray(inp["na_meta_bias"][0], f).T
    rows = np.stack([inp["ln_mix_g"][0], inp["ln_mix_b"][0], inp["ln_ffn_g"][0], inp["ln_ffn_b"][0],
                     inp["ln_mix_g"][1], inp["ln_mix_b"][1], inp["ln_ffn_g"][1], inp["ln_ffn_b"][1]]).astype(f)
    rpb = np.asarray(inp["na_rpb"][0], f)
    rp = np.zeros((16, 19, 127), f)
    rp[:, 2:17, 48:79] = rpb[:, :, ::-1]
    cstf = np.zeros((128, 224), f)
    cstf[:, 0:128] = np.eye(128, dtype=f)
    cstf[:, 128:160] = (np.arange(NE) * CAP - DUMP).astype(f)[None, :]
    cc = np.arange(64)
    cs = np.clip(cc - 8, 0, 48)
    cm = ((cc[:, None] >= cs[None, :]) & (cc[:, None] <= cs[None, :] + 15)).astype(f)
    cstf[:, 160:224] = np.concatenate([cm[::-1], cm[::-1]], axis=0)
    cstb = np.zeros((128, 512), f)
    jj = np.eye(64, dtype=f)[::-1]
    cstb[0:64, 384:448] = jj
    cstb[64:128, 448:512] = jj
    cstb[:, 0:128] = np.eye(128)
    cstb[:, 128:256] = np.triu(np.ones((128, 128)), 1)
    cstb[:, 256:384] = 1.0
    d = {"zrows": np.zeros((1024, D), ml_dtypes.bfloat16), "par": par, "rows": rows, "rb": np.asarray(inp["router_b"], f), "cstf": cstf, "cstb": cstb.astype(ml_dtypes.bfloat16),
         "lru_w_in": np.asarray(inp["lru_w_in"][0], f), "lru_wa": np.asarray(inp["lru_wa"][0], f), "lru_wx": np.asarray(inp["lru_wx"][0], f),
         "lru_w_out": np.asarray(inp["lru_w_out"][0], f), "na_w_qkv": np.asarray(inp["na_w_qkv"][0], f),
         "na_w_out": np.asarray(inp["na_w_out"][0], f), "rp": rp, "router_w": np.asarray(inp["router_w"], f),
         "moe_w_gu": np.asarray(inp["moe_w_gu"], f), "moe_w_down": np.asarray(inp["moe_w_down"], f),
         "moe_b_down": np.asarray(inp["moe_b_down"], f)}
    return d


def kernel(**inputs):
    nc, names = build_program()
    sh = shared_inputs(inputs)
    in_maps = []
    for b in range(8):
        m = dict(sh)
        m.update(host_inputs(inputs, b))
        in_maps.append({k: m[k] for k in names})
    res = run_bass_kernel_spmd(nc, in_maps, core_ids=list(range(8)))
    return np.stack([np.asarray(r["out"], np.float32) for r in res.results], axis=0)
```
